# Optimizing a Trainium2 kernel written in Bass

```python
import jax, jax.numpy as jnp
from jax import lax
import numpy as np


D_MODEL = 2048
BATCH = 2
SEQ = 4096
DEPTH = 4

N_META = 16
GLA_HEADS = 4
GLA_DK = D_MODEL // 2
GLA_DV = D_MODEL
GLA_DK_HEAD = GLA_DK // GLA_HEADS
GLA_DV_HEAD = GLA_DV // GLA_HEADS
GLA_LOWRANK = 16
GLA_TAU = 16.0
GLA_CHUNK = 64
CONV_CH = D_MODEL
CONV_WIDTH = 31
D_FF = 5632
N_EXPERTS = 8
TOP_K = 2
MOE_BLOCK = 256
N_DENSE = (DEPTH + 1) // 2
N_MOE = DEPTH // 2
DEEPNORM_ALPHA = (2.0 * DEPTH) ** 0.25
DEEPNORM_BETA = (8.0 * DEPTH) ** -0.25
LN_EPS = 1e-5
IN_WIDTHS = (GLA_DK, GLA_DK, GLA_DV, GLA_DV, GLA_LOWRANK, 2 * CONV_CH, D_MODEL, D_MODEL)
IN_SPLITS = tuple(sum(IN_WIDTHS[:i + 1]) for i in range(len(IN_WIDTHS) - 1))
D_IN = sum(IN_WIDTHS)

kernel_name = 'hybrid_gla_conformer_moe_deepnorm'


def layer_norm(x, g, b):
    xf = x.astype(jnp.float32)
    mu = xf.mean(-1, keepdims=True)
    var = jnp.square(xf - mu).mean(-1, keepdims=True)
    return ((xf - mu) * lax.rsqrt(var + LN_EPS) * g.astype(jnp.float32) + b.astype(jnp.float32)).astype(x.dtype)


def rms_norm(x, g):
    xf = x.astype(jnp.float32)
    return xf * lax.rsqrt(jnp.square(xf).mean(-1, keepdims=True) + LN_EPS) * g.astype(jnp.float32)


def gla_branch(q, k, v, r, a_low, w_alpha_up, b_alpha, norm_g, w_o):
    Bt, L, _ = q.shape
    log_a = jax.nn.log_sigmoid((a_low @ w_alpha_up + b_alpha).astype(jnp.float32)) / GLA_TAU
    pad = (-N_META) % GLA_CHUNK
    n_chunks = (L + pad) // GLA_CHUNK

    def to_chunks(t):
        t = jnp.pad(t.astype(jnp.float32), ((0, 0), (pad, 0), (0, 0)))
        t = t.reshape(Bt, n_chunks, GLA_CHUNK, GLA_HEADS, -1)
        return t.transpose(0, 3, 1, 2, 4)

    qc = to_chunks(q) * (GLA_DK_HEAD ** -0.5)
    kc = to_chunks(k)
    vc = to_chunks(v)
    b = jnp.cumsum(to_chunks(log_a), axis=3)
    b_last = b[:, :, :, -1:, :]
    q_dec = qc * jnp.exp(b)
    k_dec = kc * jnp.exp(-b)
    k_to_end = kc * jnp.exp(b_last - b)
    causal = jnp.tril(jnp.ones((GLA_CHUNK, GLA_CHUNK), dtype=bool))
    scores = jnp.where(causal, jnp.einsum('bhnid,bhnjd->bhnij', q_dec, k_dec), 0.0)
    o_intra = jnp.einsum('bhnij,bhnjv->bhniv', scores, vc)

    def chunk_step(S, inp):
        qd, kd, vch, decay = inp
        o = jnp.einsum('bhid,bhdv->bhiv', qd, S)
        S_new = decay[..., None] * S + jnp.einsum('bhjd,bhjv->bhdv', kd, vch)
        return S_new, o

    xs = (jnp.moveaxis(q_dec, 2, 0), jnp.moveaxis(k_to_end, 2, 0), jnp.moveaxis(vc, 2, 0),
          jnp.moveaxis(jnp.exp(b_last[:, :, :, 0, :]), 2, 0))
    S0 = jnp.zeros((Bt, GLA_HEADS, GLA_DK_HEAD, GLA_DV_HEAD), jnp.float32)
    _, o_inter = lax.scan(chunk_step, S0, xs)
    o = o_intra + jnp.moveaxis(o_inter, 0, 2)
    o = o.transpose(0, 2, 3, 1, 4).reshape(Bt, n_chunks * GLA_CHUNK, GLA_HEADS, GLA_DV_HEAD)[:, pad:]
    o = rms_norm(o, norm_g.reshape(GLA_HEADS, GLA_DV_HEAD)).reshape(Bt, L, GLA_DV).astype(v.dtype)
    return (jax.nn.silu(r) * o) @ w_o


def conformer_conv_branch(u_glu, conv_w, conv_b, norm_g, norm_b, w_o):
    a, g = jnp.split(u_glu, 2, axis=-1)
    u = a * jax.nn.sigmoid(g)
    u = jnp.pad(u, ((0, 0), (CONV_WIDTH - 1, 0), (0, 0)))
    y = lax.conv_general_dilated(u, conv_w[:, None, :], window_strides=(1,), padding='VALID',
                                 dimension_numbers=('NWC', 'WIO', 'NWC'),
                                 feature_group_count=CONV_CH) + conv_b
    y = jax.nn.silu(layer_norm(y, norm_g, norm_b))
    return y @ w_o


def hybrid_mixer(h, w_in, w_alpha_up, b_alpha, gla_norm_g, w_gla_o, conv_w, conv_b,
                 conv_norm_g, conv_norm_b, w_conv_o, w_out):
    p = h @ w_in
    q, k, v, r, a_low, glu, ga, gb = jnp.split(p, IN_SPLITS, axis=-1)
    y_a = gla_branch(q, k, v, r, a_low, w_alpha_up, b_alpha, gla_norm_g, w_gla_o)
    y_b = conformer_conv_branch(glu, conv_w, conv_b, conv_norm_g, conv_norm_b, w_conv_o)
    m = jax.nn.sigmoid(ga) * y_a + jax.nn.sigmoid(gb) * y_b
    return m @ w_out


def swiglu(h, w1, w3, w2):
    return (jax.nn.silu(h @ w1) * (h @ w3)) @ w2


def moe_swiglu(h, router_w, router_b, w1, w3, w2):
    Bt, L, D = h.shape
    xt = h.reshape(Bt * L, D)
    n_tok = Bt * L
    n_assign = n_tok * TOP_K
    logits = xt.astype(jnp.float32) @ router_w.astype(jnp.float32) + router_b.astype(jnp.float32)
    top_logit, top_exp = lax.top_k(logits, TOP_K)
    top_gate = jax.nn.softmax(top_logit, axis=-1)
    exp_flat = top_exp.reshape(n_assign)
    tok_flat = jnp.repeat(jnp.arange(n_tok, dtype=jnp.int32), TOP_K)
    gate_flat = top_gate.reshape(n_assign)
    order = jnp.argsort(exp_flat)
    exp_sorted = exp_flat[order]
    counts = jnp.bincount(exp_flat, length=N_EXPERTS)
    padded = (counts + MOE_BLOCK - 1) // MOE_BLOCK * MOE_BLOCK
    start = jnp.cumsum(counts) - counts
    pad_end = jnp.cumsum(padded)
    pad_start = pad_end - padded
    dest = pad_start[exp_sorted] + (jnp.arange(n_assign) - start[exp_sorted])
    n_blocks = -(-n_assign // MOE_BLOCK) + N_EXPERTS
    n_slots = n_blocks * MOE_BLOCK
    slot_tok = jnp.zeros((n_slots,), jnp.int32).at[dest].set(tok_flat[order])
    slot_gate = jnp.zeros((n_slots,), jnp.float32).at[dest].set(gate_flat[order])
    block_exp = jnp.minimum(jnp.searchsorted(pad_end, jnp.arange(n_blocks) * MOE_BLOCK, side='right'),
                            N_EXPERTS - 1)

    def expert_block(args):
        tok, gate, e = args
        xb = xt[tok]
        hb = jax.nn.silu(xb @ w1[e]) * (xb @ w3[e])
        return (hb @ w2[e]) * gate[:, None].astype(xb.dtype)

    y = lax.map(expert_block, (slot_tok.reshape(n_blocks, MOE_BLOCK),
                               slot_gate.reshape(n_blocks, MOE_BLOCK), block_exp))
    out = jnp.zeros_like(xt).at[slot_tok].add(y.reshape(n_slots, D))
    return out.reshape(Bt, L, D)


def setup_inputs(seed: int = 0) -> dict:
    key = jax.random.key(seed)
    ks = jax.random.split(key, 32)
    d = D_MODEL

    def nrm(k, shape, scale):
        return jax.random.normal(k, shape, jnp.float32) * scale

    def gain(k, shape):
        return 1.0 + nrm(k, shape, 0.02)

    return {
        'x': nrm(ks[0], (BATCH, SEQ, d), 1.0),
        'meta_tokens': nrm(ks[1], (N_META, d), 1.0),
        'ln_in_g': gain(ks[2], (d,)),
        'ln_in_b': nrm(ks[3], (d,), 0.02),
        'w_in': nrm(ks[4], (DEPTH, d, D_IN), d ** -0.5),
        'w_alpha_up': nrm(ks[5], (DEPTH, GLA_LOWRANK, GLA_DK), GLA_LOWRANK ** -0.5),
        'b_alpha': nrm(ks[6], (DEPTH, GLA_DK), 0.1),
        'gla_norm_g': gain(ks[7], (DEPTH, GLA_DV)),
        'w_gla_o': nrm(ks[8], (DEPTH, GLA_DV, d), GLA_DV ** -0.5),
        'conv_w': nrm(ks[9], (DEPTH, CONV_WIDTH, CONV_CH), CONV_WIDTH ** -0.5),
        'conv_b': nrm(ks[10], (DEPTH, CONV_CH), 0.02),
        'conv_norm_g': gain(ks[11], (DEPTH, CONV_CH)),
        'conv_norm_b': nrm(ks[12], (DEPTH, CONV_CH), 0.02),
        'w_conv_o': nrm(ks[13], (DEPTH, CONV_CH, d), CONV_CH ** -0.5),
        'w_out': nrm(ks[14], (DEPTH, d, d), DEEPNORM_BETA * d ** -0.5),
        'ln_mix_g': gain(ks[15], (DEPTH, d)),
        'ln_mix_b': nrm(ks[16], (DEPTH, d), 0.02),
        'ffn_w1': nrm(ks[17], (N_DENSE, d, D_FF), d ** -0.5),
        'ffn_w3': nrm(ks[18], (N_DENSE, d, D_FF), d ** -0.5),
        'ffn_w2': nrm(ks[19], (N_DENSE, D_FF, d), DEEPNORM_BETA * D_FF ** -0.5),
        'router_w': nrm(ks[20], (N_MOE, d, N_EXPERTS), d ** -0.5),
        'router_b': nrm(ks[21], (N_MOE, N_EXPERTS), 0.01),
        'moe_w1': nrm(ks[22], (N_MOE, N_EXPERTS, d, D_FF), d ** -0.5),
        'moe_w3': nrm(ks[23], (N_MOE, N_EXPERTS, d, D_FF), d ** -0.5),
        'moe_w2': nrm(ks[24], (N_MOE, N_EXPERTS, D_FF, d), DEEPNORM_BETA * D_FF ** -0.5),
        'ln_ffn_g': gain(ks[25], (DEPTH, d)),
        'ln_ffn_b': nrm(ks[26], (DEPTH, d), 0.02),
    }


def reference(x, meta_tokens, ln_in_g, ln_in_b, w_in, w_alpha_up, b_alpha, gla_norm_g, w_gla_o,
              conv_w, conv_b, conv_norm_g, conv_norm_b, w_conv_o, w_out, ln_mix_g, ln_mix_b,
              ffn_w1, ffn_w3, ffn_w2, router_w, router_b, moe_w1, moe_w3, moe_w2,
              ln_ffn_g, ln_ffn_b):
    Bt = x.shape[0]
    meta = jnp.broadcast_to(meta_tokens[None].astype(x.dtype), (Bt, N_META, D_MODEL))
    h = jnp.concatenate([meta, x], axis=1)
    h = layer_norm(h, ln_in_g, ln_in_b)
    for i in range(DEPTH):
        mix = hybrid_mixer(h, w_in[i], w_alpha_up[i], b_alpha[i], gla_norm_g[i], w_gla_o[i],
                           conv_w[i], conv_b[i], conv_norm_g[i], conv_norm_b[i], w_conv_o[i], w_out[i])
        h = layer_norm(DEEPNORM_ALPHA * h + mix, ln_mix_g[i], ln_mix_b[i])
        j = i // 2
        if i % 2 == 0:
            f = swiglu(h, ffn_w1[j], ffn_w3[j], ffn_w2[j])
        else:
            f = moe_swiglu(h, router_w[j], router_b[j], moe_w1[j], moe_w3[j], moe_w2[j])
        h = layer_norm(DEEPNORM_ALPHA * h + f, ln_ffn_g[i], ln_ffn_b[i])
    return h[:, N_META:]
```

```python
import numpy as np
import concourse.bass as bass
import concourse.mybir as mybir
from concourse.bass_utils import run_bass_kernel_spmd

F32 = mybir.dt.float32
BF16 = mybir.dt.bfloat16
AF = mybir.ActivationFunctionType
ALU = mybir.AluOpType

CFG_FULL = dict(D=2048, SEQ=4096, DEPTH=4, DFF=5632, NE=8, NMETA=16, LOWR=16, CW=31)
PR, PC = 256, 2048
PIECE = PR * PC
GROUPS = [[0, 1, 2, 3], [4, 5, 6, 7]]


class Op:
    __slots__ = ("eng", "fn", "deps", "kind", "inc", "sem", "val", "idx")


class Sched:
    def __init__(self):
        self.ops = []
        self.lastw = {}
        self.readers = {}
        self.last_barrier = None
        self.last_eng = {}
        self.recent_sp = []

    def op(self, eng, fn, reads=(), writes=(), kind="c"):
        o = Op()
        o.eng, o.fn, o.kind, o.inc, o.idx = eng, fn, kind, False, len(self.ops)
        deps = set()
        for b in reads:
            w = self.lastw.get(b)
            if w is not None:
                deps.add(w)
        for b in writes:
            w = self.lastw.get(b)
            if w is not None:
                deps.add(w)
            for r in self.readers.get(b, ()):
                deps.add(r)
        deps.discard(o.idx)
        if self.last_barrier is not None and eng != "pool":
            deps.add(self.last_barrier)
        o.deps = deps
        if kind == "dma" and eng == "sp":
            self.recent_sp = (self.recent_sp + [o.idx])[-8:]
        elif kind == "c":
            self.last_eng[eng] = o.idx
        for b in reads:
            lst = self.readers.setdefault(b, [])
            if kind == "c":
                lst[:] = [r for r in lst if not (self.ops[r].eng == eng and self.ops[r].kind == "c")]
            lst.append(o.idx)
        for b in writes:
            self.lastw[b] = o.idx
            self.readers[b] = []
        self.ops.append(o)
        return o

    def barrier(self, fn):
        o = self.op("dve", fn, [], [])
        o.deps |= set(self.last_eng.values()) | set(self.recent_sp)
        o.deps.discard(o.idx)
        self.last_barrier = o.idx
        return o

    def emit(self, nc, sems, dma_sems, cc_sem):
        ops = self.ops
        for o in ops:
            for d in o.deps:
                p = ops[d]
                if p.eng == "pe" and o.eng == "pe" and p.kind == "c":
                    continue
                p.inc = True
        cnt = {e: 0 for e in sems}
        dcnt = {}
        rr = {"sp": 0, "pool": 0, "act": 0}
        ccn = 0
        prev_on_sem = {}
        for o in ops:
            if o.kind == "dma":
                lst = dma_sems[o.eng]
                s = lst[rr[o.eng] % len(lst)]
                rr[o.eng] += 1
                pv = prev_on_sem.get(id(s))
                if pv is not None:
                    o.deps.add(pv)
                prev_on_sem[id(s)] = o.idx
                dcnt[id(s)] = dcnt.get(id(s), 0) + 16
                o.sem, o.val, o.inc = s, dcnt[id(s)], True
            elif o.kind == "cc":
                ccn += 1
                o.sem, o.val, o.inc = cc_sem, ccn, True
            elif o.inc:
                cnt[o.eng] += 1
                o.sem, o.val = sems[o.eng], cnt[o.eng]
        per = {"pe": [], "act": [], "dve": [], "pool": [], "sp": []}
        waited = {e: {} for e in per}
        for o in ops:
            ws = {}
            for d in o.deps:
                p = ops[d]
                if not p.inc:
                    continue
                if p.eng == "pe" and o.eng == "pe" and p.kind == "c":
                    continue
                k = id(p.sem)
                if waited[o.eng].get(k, 0) >= p.val:
                    continue
                if k not in ws or ws[k][1] < p.val:
                    ws[k] = (p.sem, p.val)
            for k, (s, v) in ws.items():
                waited[o.eng][k] = v
            per[o.eng].append((list(ws.values()), o))
        engs = {"pe": "tensor", "act": "scalar", "dve": "vector", "pool": "gpsimd", "sp": "sync"}
        with nc.Block() as block:
            for en, lst in per.items():
                def body(e, lst=lst):
                    for ws, o in lst:
                        for s, v in ws:
                            e.wait_ge(s, v)
                        ins = o.fn(e)
                        if o.inc:
                            if o.kind == "dma":
                                ins.then_inc(o.sem, 16)
                            elif o.kind == "cc":
                                ins.then_inc(o.sem)
                            else:
                                ins.then_inc(o.sem, 1)
                    if en in ("sp", "pool"):
                        for s in dma_sems[en]:
                            v = dcnt.get(id(s), 0)
                            if v:
                                e.wait_ge(s, v)
                getattr(block, engs[en])(body)


def n_pieces(numel):
    return -(-numel // (4 * PIECE))


def shard_weight(w, r):
    flat = np.ascontiguousarray(w, dtype=np.float32).reshape(-1)
    npc = n_pieces(flat.size)
    pad = npc * 4 * PIECE - flat.size
    if pad:
        flat = np.concatenate([flat, np.zeros(pad, np.float32)])
    return np.ascontiguousarray(flat.reshape(npc, 4, PR, PC)[:, r])


def build(cfg):
    D, SEQ, DEPTH, DFF, NE = cfg["D"], cfg["SEQ"], cfg["DEPTH"], cfg["DFF"], cfg["NE"]
    NM, LOWR, CW = cfg["NMETA"], cfg["LOWR"], cfg["CW"]
    STOP = cfg.get("STOP")
    KC = D // 128
    DK = D // 2
    NH = 4
    DKH = DK // NH
    DVH = D // NH
    KH = DKH // 128
    VH = DVH // 128
    FC = DFF // 128
    TL = SEQ // 4
    T = NM + TL
    DIN = DK + DK + D + D + LOWR + 2 * D + D + D
    oQ, oK, oV, oR, oA = 0, DK, 2 * DK, 2 * DK + D, 2 * DK + 2 * D
    oG = oA + LOWR
    oGA = oG + 2 * D
    oGB = oGA + D
    cQ, cK, cV = 0, DK // 128, 2 * DK // 128
    cR = cV + KC
    cG = cR + KC
    cGA = cG + 2 * KC
    cGB = cGA + KC
    cA = cGB + KC
    DINP = (cA + 1) * 128
    ALPHA = (2.0 * DEPTH) ** 0.25
    QSCALE = DKH ** -0.5
    EPS = 1e-5
    HALO = CW - 1
    UW = HALO + NM + HALO + TL
    R1 = LOWR + 1
    NMOE = max(1, DEPTH // 2)
    TB = [(0, NM)] + [(NM + i, min(512, TL - i)) for i in range(0, TL, 512)]
    NB = len(TB)
    TILES = [(0, NM)] + [(NM + i, 128) for i in range(0, TL, 128)]

    def upos(t0):
        return HALO + t0 if t0 < NM else HALO + NM + HALO + (t0 - NM)

    nc = bass.Bass("TRN2", target_bir_lowering=False, num_devices=8)
    S = Sched()

    def dt_in(name, shape):
        return nc.dram_tensor(name, list(shape), F32, kind="ExternalInput")

    xT = dt_in("xT", [128, KC, T])
    vecs = dt_in("vecs", [128, 2 + DEPTH * 9, KC])
    convw = dt_in("convw", [DEPTH, 128, KC, CW])
    gnorm = dt_in("gnorm", [128, DEPTH, KC])
    alup = dt_in("alup", [DEPTH, NH, R1, DKH])
    rtr = dt_in("rtr", [NMOE, 128, KC, NE])
    rtb = dt_in("rtb", [NMOE, 1, NE])
    consts = dt_in("consts", [128, 5, 128])
    flags = dt_in("flags", [128, 8])
    out = nc.dram_tensor("out", [128, KC, TL], F32, kind="ExternalOutput")

    wspec = []
    for l in range(DEPTH):
        wspec += [(f"w_in{l}", D, DINP, l), (f"w_glao{l}", D, D, l), (f"w_convo{l}", D, D, l), (f"w_out{l}", D, D, l)]
        if l % 2 == 0:
            wspec += [(f"ffn1_{l}", D, DFF, l), (f"ffn3_{l}", D, DFF, l), (f"ffn2_{l}", DFF, D, l)]
        else:
            for e in range(NE):
                wspec += [(f"moe1_{l}_{e}", D, DFF, l), (f"moe3_{l}_{e}", D, DFF, l), (f"moe2_{l}_{e}", DFF, D, l)]
    win, wbn, wg, wview = {}, {}, {}, {}
    for name, K, N, _ in wspec:
        npc = n_pieces(K * N)
        win[name] = dt_in(name, [npc, PR, PC])
        wbn[name] = nc.dram_tensor(name + "_b", [npc, PR, PC], BF16)
        wg[name] = nc.dram_tensor(name + "_g", [npc, 4 * PR, PC], BF16)
        wview[name] = wg[name].ap().rearrange("a b c -> (a b c)")[0:K * N].rearrange("(j p k c) -> j p k c", p=128, k=K // 128, c=128)

    hres = nc.dram_tensor("hres", [128, KC, T], F32)
    zres = nc.dram_tensor("zres", [128, KC, T], F32)
    xsrc = nc.dram_tensor("xsrc", [NH + 1, 128, 1024], F32)
    xdst = nc.dram_tensor("xdst", [NH + 1, 512, 1024], F32)
    usrc = nc.dram_tensor("usrc", [128, 1024], F32)
    udst = nc.dram_tensor("udst", [512, 1024], F32)

    import contextlib
    es = contextlib.ExitStack()
    sb = lambda name, shape, dt=F32: es.enter_context(nc.sbuf_tensor(name, list(shape), dt))
    with es:
        hb = sb("hb", [128, KC, T], BF16)
        BIGN = KC * (2 * T + UW)
        assert FC * T <= BIGN
        big = sb("big", [128, BIGN], BF16)
        RA = big[:, 0:KC * T].rearrange("p (c t) -> p c t", t=T)
        Ue = big[:, KC * T:KC * T + KC * UW].rearrange("p (c t) -> p c t", t=UW)
        RM = big[:, KC * (T + UW):BIGN].rearrange("p (c t) -> p c t", t=T)
        HH = big[:, 0:FC * T].rearrange("p (c t) -> p c t", t=T)
        stat = sb("stat", [128, 2, T])
        vec = sb("vec", [128, 2 + DEPTH * 9, KC])
        cw = sb("cw", [128, KC, CW])
        gn = sb("gn", [128, DEPTH, KC])
        cst = sb("cst", [128, 5, 128])
        idb = sb("idb", [128, 128], BF16)
        fl = sb("fl", [128, 8])
        small = sb("small", [128, 80])
        KG = max(KC, 11)
        wt = [sb(f"wt{i}", [128, KG, 128], BF16) for i in range(4)]
        stg = [sb(f"stg{i}", [128, T]) for i in range(3)]
        tmp = [sb(f"tmp{i}", [128, 512]) for i in range(4)]
        hal = sb("hal", [128, KC * HALO])
        NF = max(6 * DKH + 2 * KH * DVH, KC * HALO + NM + HALO + TL, KC * NE + 3 * T + 64)
        scrF = sb("scrF", [128, NF])
        NHH = 3 * DKH + DVH + 128 + KH * DVH
        scrH = sb("scrH", [128, NHH], BF16)
        ps = [es.enter_context(nc.psum_tensor(f"ps{i}", [128, 512], F32)) for i in range(8)]
        sems = {e: es.enter_context(nc.semaphore(f"s_{e}")) for e in ("pe", "act", "dve")}
        dma_sems = {q: [es.enter_context(nc.semaphore(f"d_{q}{i}")) for i in range(8)] for q in ("sp", "pool")}
        dma_sems["act"] = []
        cc_sem = es.enter_context(nc.semaphore("cc"))

        o_ = 0
        auh = scrF[:, o_:o_ + DKH]; o_ += DKH
        e1 = scrF[:, o_:o_ + DKH]; o_ += DKH
        ltok = scrF[:, o_:o_ + DKH]; o_ += DKH
        ef = scrF[:, o_:o_ + DKH]; o_ += DKH
        eB = scrF[:, o_:o_ + DKH].rearrange("p (k n) -> p k n", n=128); o_ += DKH
        enB = scrF[:, o_:o_ + DKH].rearrange("p (k n) -> p k n", n=128); o_ += DKH
        Sstf = scrF[:, o_:o_ + KH * DVH]
        Sst = Sstf.rearrange("p (k n) -> p k n", n=DVH); o_ += KH * DVH
        Sx = scrF[:, o_:o_ + KH * DVH]; o_ += KH * DVH
        halg = scrF[:, 0:KC * HALO]
        cacc = scrF[:, KC * HALO:KC * HALO + NM + HALO + TL]
        rtw = scrF[:, 0:KC * NE].rearrange("p (k n) -> p k n", n=NE)
        GT = scrF[:, KC * NE:KC * NE + T]
        Gsel = scrF[:, KC * NE + T:KC * NE + 2 * T]
        Gb = scrF[:, KC * NE + 2 * T:KC * NE + 3 * T]
        lg = scrF[:, KC * NE + 3 * T:KC * NE + 3 * T + 64]
        o_ = 0
        kte = scrH[:, o_:o_ + DKH]; o_ += DKH
        vt = scrH[:, o_:o_ + DVH]; o_ += DVH
        qd = scrH[:, o_:o_ + DKH].rearrange("p (k n) -> p k n", n=128); o_ += DKH
        kd = scrH[:, o_:o_ + DKH].rearrange("p (k n) -> p k n", n=128); o_ += DKH
        sT = scrH[:, o_:o_ + 128]; o_ += 128
        Sb = scrH[:, o_:o_ + KH * DVH].rearrange("p (k n) -> p k n", n=DVH); o_ += KH * DVH
        wt_mix = list(range(len(wt)))
        for i in range(2):
            if FC * T + (i + 1) * KG * 128 <= BIGN:
                wt.append(big[:, FC * T + i * KG * 128:FC * T + (i + 1) * KG * 128].rearrange("p (k c) -> p k c", c=128))
        if NHH >= KG * 128:
            wt.append(scrH[:, 0:KG * 128].rearrange("p (k c) -> p k c", c=128))
        wt_ffn = list(range(len(wt)))
        wt_act = [wt_mix]
        alT = stat[:, 0, :]
        erun = small[:, 0:KH]
        acoef = small[:, 8:8 + KH]
        dsg = small[:, 16:16 + NH * KH]
        dall = small[:, 32:32 + 3 * 8].rearrange("p (j n) -> p j n", n=8) if NH * KH <= 8 else None
        sm = small[:, 56:64]

        psi = [0]

        def next_ps():
            psi[0] = (psi[0] + 1) % 8
            return psi[0]

        tmi = [0]

        def next_tmp():
            tmi[0] = (tmi[0] + 1) % 4
            return tmi[0]

        def dma(q, out_ap, in_ap, reads, writes):
            S.op(q, lambda e: e.dma_start(out=out_ap, in_=in_ap), reads, writes, kind="dma")

        def mmr(out_ap, pi, lhsT, rhs, start, stop, reads):
            S.op("pe", lambda e: e.matmul(out_ap, lhsT, rhs, start=start, stop=stop), reads, [("ps", pi)])

        def mm(pi, sl, lhsT, rhs, start, stop, reads):
            mmr(ps[pi][sl[0], sl[1]], pi, lhsT, rhs, start, stop, reads)

        def act(out_ap, in_ap, func, reads, writes, bias=None, scale=None):
            kw = {}
            if bias is not None:
                kw["bias"] = bias
            if scale is not None:
                kw["scale"] = scale
            S.op("act", lambda e: e.activation(out=out_ap, in_=in_ap, func=func, **kw), reads, writes)

        def tt(out_ap, a, b, op, reads, writes, eng="dve"):
            S.op(eng, lambda e: e.tensor_tensor(out=out_ap, in0=a, in1=b, op=op), reads, writes)

        def ts(out_ap, a, s1, s2, op0, op1, reads, writes, eng="dve"):
            if s2 is None:
                S.op(eng, lambda e: e.tensor_scalar(out=out_ap, in0=a, scalar1=s1, scalar2=None, op0=op0), reads, writes)
            else:
                S.op(eng, lambda e: e.tensor_scalar(out=out_ap, in0=a, scalar1=s1, scalar2=s2, op0=op0, op1=op1), reads, writes)

        def stt(out_ap, a, s, b, op0, op1, reads, writes, eng="dve"):
            S.op(eng, lambda e: e.scalar_tensor_tensor(out=out_ap, in0=a, scalar=s, in1=b, op0=op0, op1=op1), reads, writes)

        def cp(out_ap, in_ap, reads, writes, eng="dve"):
            if eng == "act":
                S.op(eng, lambda e: e.copy(out=out_ap, in_=in_ap), reads, writes)
            else:
                S.op(eng, lambda e: e.tensor_copy(out=out_ap, in_=in_ap), reads, writes)

        def memset(ap, v, writes, eng="dve"):
            S.op(eng, lambda e: e.memset(ap, v), [], writes)

        def recip(out_ap, in_ap, reads, writes):
            S.op("dve", lambda e: e.reciprocal(out=out_ap, in_=in_ap), reads, writes)

        def barrier():
            S.barrier(lambda e: e.memset(small[:, 72:73], 0.0))

        dma("sp", vec[:], vecs.ap(), [], ["vec"])
        dma("sp", gn[:], gnorm.ap(), [], ["gn"])
        dma("sp", cst[:], consts.ap(), [], ["cst"])
        dma("sp", fl[:], flags.ap(), [], ["fl"])
        cp(idb[:], cst[:, 0, :], ["cst"], ["idb"])
        IDN, TRI, UPP, ONES, CM = 0, 1, 2, 3, 4

        gq = [name for name, _, _, _ in wspec]
        gpos = [0]

        def gather_through(pred):
            last = max([i for i, n in enumerate(gq) if pred(n)], default=-1)
            while gpos[0] <= last:
                name = gq[gpos[0]]
                gpos[0] += 1
                for p in range(win[name].shape[0]):
                    dma("pool", wbn[name][p], win[name][p], [], [("wb", name, p)])
                    S.op("pool", lambda e, a=wbn[name][p], b=wg[name][p]: e.collective_compute(
                        "AllGather", ALU.bypass, replica_groups=GROUPS, ins=[a], outs=[b]),
                        [("wb", name, p)], [("wg", name)], kind="cc")

        wl_of = {name: wl for name, _, _, wl in wspec}

        def first_part(n, lmax):
            if wl_of[n] < lmax:
                return True
            if wl_of[n] > lmax:
                return False
            return not (n.startswith("moe") and int(n.split("_")[2]) >= NE // 2)

        def gather_after_exchange(l):
            if l >= DEPTH - 2:
                gather_through(lambda n: True)
            elif (l + 1) % 2 == 1:
                gather_through(lambda n: first_part(n, l + 1))
            else:
                gather_through(lambda n: first_part(n, min(l + 3, DEPTH - 1)))

        wbi = [0]

        def load_w(name, k0, nk, j, ncol=128):
            i = wt_act[0][wbi[0] % len(wt_act[0])]
            wbi[0] += 1
            dma("sp", wt[i][:, 0:nk, 0:ncol], wview[name][j, :, k0:k0 + nk, 0:ncol], [("wg", name)], [("wt", i)])
            return i

        HBK = [("hb", j) for j in range(KC)]

        def proj(name, jc, ncol, blocks, epi, rhs=None, rkeys=None, rpos=None):
            rhs = hb if rhs is None else rhs
            rkeys = HBK if rkeys is None else rkeys
            rpos = rpos or (lambda t0: t0)
            wi = load_w(name, 0, KC, jc, ncol)
            for (t0, tn) in blocks:
                pi = next_ps()
                r0 = rpos(t0)
                for k in range(KC):
                    mm(pi, (slice(0, ncol), slice(0, tn)), wt[wi][:, k, 0:ncol], rhs[:, k, r0:r0 + tn], k == 0, k == KC - 1,
                       [("wt", wi)] + rkeys)
                epi(t0, tn, pi)

        def ln_accum(pss, pqq, j, zap, zkey):
            for bi, (t0, tn) in enumerate(TB):
                ti = next_tmp()
                act(tmp[ti][:, 0:tn], zap[:, t0:t0 + tn], AF.Square, [zkey], [("tmp", ti)])
                mm(pss[bi], (slice(0, 128), slice(0, tn)), cst[:, ONES, :], zap[:, t0:t0 + tn], j == 0, j == KC - 1, [zkey, "cst"])
                mm(pqq[bi], (slice(0, 128), slice(0, tn)), cst[:, ONES, :], tmp[ti][:, 0:tn], j == 0, j == KC - 1, [("tmp", ti), "cst"])

        def ln_finish(pss, pqq, n):
            for bi, (t0, tn) in enumerate(TB):
                sl = slice(t0, t0 + tn)
                ta, tb_ = next_tmp(), next_tmp()
                ts(stat[:, 0, sl], ps[pss[bi]][:, 0:tn], 1.0 / n, None, ALU.mult, None, [("ps", pss[bi])], [("stat", 0, bi)])
                ts(tmp[ta][:, 0:tn], ps[pqq[bi]][:, 0:tn], 1.0 / n, None, ALU.mult, None, [("ps", pqq[bi])], [("tmp", ta)])
                tt(tmp[tb_][:, 0:tn], stat[:, 0, sl], stat[:, 0, sl], ALU.mult, [("stat", 0, bi)], [("tmp", tb_)])
                tt(tmp[ta][:, 0:tn], tmp[ta][:, 0:tn], tmp[tb_][:, 0:tn], ALU.subtract, [("tmp", ta), ("tmp", tb_)], [("tmp", ta)])
                ts(tmp[ta][:, 0:tn], tmp[ta][:, 0:tn], EPS, None, ALU.add, None, [("tmp", ta)], [("tmp", ta)])
                act(tmp[tb_][:, 0:tn], tmp[ta][:, 0:tn], AF.Sqrt, [("tmp", ta)], [("tmp", tb_)])
                recip(stat[:, 1, sl], tmp[tb_][:, 0:tn], [("tmp", tb_)], [("stat", 1, bi)])

        SK = [("stat", r, b) for r in (0, 1) for b in range(NB)]

        def ln_from_dram(src):
            pss, pqq = [next_ps() for _ in TB], [next_ps() for _ in TB]
            for j in range(KC):
                si = j % 3
                dma("sp", stg[si][:], src[:, j, :], [("zres", j)], [("stg", si)])
                ln_accum(pss, pqq, j, stg[si], ("stg", si))
            ln_finish(pss, pqq, D)

        def ln_apply(src_dram, gi, bi_, write_out=None):
            for j in range(KC):
                si = j % 3
                dma("sp", stg[si][:], src_dram[:, j, :], [("zres", j)], [("stg", si)])
                tt(stg[si][:], stg[si][:], stat[:, 0, :], ALU.subtract, [("stg", si)] + SK, [("stg", si)])
                tt(stg[si][:], stg[si][:], stat[:, 1, :], ALU.mult, [("stg", si)] + SK, [("stg", si)])
                ts(stg[si][:], stg[si][:], vec[:, gi, j:j + 1], vec[:, bi_, j:j + 1], ALU.mult, ALU.add, [("stg", si), "vec"], [("stg", si)])
                cp(hb[:, j, :], stg[si][:], [("stg", si)], [("hb", j)], eng="act")
                dma("sp", hres[:, j, :], stg[si][:], [("stg", si)], [("hres", j)])
                if write_out is not None:
                    dma("sp", write_out[:, j, :], stg[si][:, NM:T], [("stg", si)], [("out", j)])

        def ffn(w1n, w3n, w2n, first, gated):
            for f in range(FC):
                wa = load_w(w1n, 0, KC, f)
                wgk = load_w(w3n, 0, KC, f)
                for (t0, tn) in TB:
                    pa, pg = next_ps(), next_ps()
                    for k in range(KC):
                        mm(pa, (slice(0, 128), slice(0, tn)), wt[wa][:, k, 0:128], hb[:, k, t0:t0 + tn], k == 0, k == KC - 1, [("wt", wa)] + HBK)
                    for k in range(KC):
                        mm(pg, (slice(0, 128), slice(0, tn)), wt[wgk][:, k, 0:128], hb[:, k, t0:t0 + tn], k == 0, k == KC - 1, [("wt", wgk)] + HBK)
                    ti = next_tmp()
                    act(tmp[ti][:, 0:tn], ps[pa][:, 0:tn], AF.Silu, [("ps", pa)], [("tmp", ti)])
                    tt(HH[:, f, t0:t0 + tn], ps[pg][:, 0:tn], tmp[ti][:, 0:tn], ALU.mult, [("ps", pg), ("tmp", ti)], [("H", f)])
            HK = [("H", f) for f in range(FC)]
            for j in range(KC):
                grp = []
                for k0 in range(0, FC, KG):
                    nk = min(KG, FC - k0)
                    grp.append((k0, nk, load_w(w2n, k0, nk, j)))
                si = j % 3
                if first:
                    dma("sp", stg[si][:], hres[:, j, :], [("hres", j)], [("stg", si)])
                    ts(stg[si][:], stg[si][:], ALPHA, None, ALU.mult, None, [("stg", si)], [("stg", si)])
                else:
                    dma("sp", stg[si][:], zres[:, j, :], [("zres", j)], [("stg", si)])
                for (t0, tn) in TB:
                    pa = next_ps()
                    for (k0, nk, wi) in grp:
                        for k in range(nk):
                            mm(pa, (slice(0, 128), slice(0, tn)), wt[wi][:, k, 0:128], HH[:, k0 + k, t0:t0 + tn],
                               k0 + k == 0, k0 + k == FC - 1, [("wt", wi)] + HK)
                    if gated:
                        ti = next_tmp()
                        tt(tmp[ti][:, 0:tn], ps[pa][:, 0:tn], Gb[:, t0:t0 + tn], ALU.mult, [("ps", pa), "Gb"], [("tmp", ti)])
                        tt(stg[si][:, t0:t0 + tn], stg[si][:, t0:t0 + tn], tmp[ti][:, 0:tn], ALU.add, [("stg", si), ("tmp", ti)], [("stg", si)])
                    else:
                        tt(stg[si][:, t0:t0 + tn], stg[si][:, t0:t0 + tn], ps[pa][:, 0:tn], ALU.add, [("stg", si), ("ps", pa)], [("stg", si)])
                dma("sp", zres[:, j, :], stg[si][:], [("stg", si)], [("zres", j)])

        def router(li):
            dma("sp", rtw[:], rtr[li], [], ["rtw"])
            dma("sp", sm[0:1, 0:NE], rtb[li], [], ["rtbias"])
            for (t0, nt) in TILES:
                pl = next_ps()
                for g0 in range(0, KC, 4):
                    gi = next_tmp()
                    ng = min(4, KC - g0)
                    hv = tmp[gi][:, 0:ng * 128].rearrange("p (k n) -> p k n", n=128)
                    dma("sp", hv[:, :, 0:nt], hres[:, g0:g0 + ng, t0:t0 + nt], [("hres", j) for j in range(g0, g0 + ng)], [("tmp", gi)])
                    for k in range(ng):
                        mm(pl, (slice(0, nt), slice(0, NE)), hv[:, k, 0:nt], rtw[:, g0 + k, :], g0 + k == 0, False, [("tmp", gi), "rtw"])
                mm(pl, (slice(0, nt), slice(0, NE)), cst[0:1, ONES, 0:nt], sm[0:1, 0:NE], False, True, ["cst", "rtbias"])
                L0 = lg[0:nt, 0:NE]
                L1 = lg[0:nt, 8:8 + NE]
                K1 = lg[0:nt, 16:16 + NE]
                K2 = lg[0:nt, 24:24 + NE]
                GG = lg[0:nt, 32:32 + NE]
                m1, m2, dd, g1, g2 = (lg[0:nt, 40 + i:41 + i] for i in range(5))
                LK = ["lg"]
                cp(L0, ps[pl][0:nt, 0:NE], [("ps", pl)], LK)
                S.op("dve", lambda e, o=m1, i=L0: e.reduce_max(out=o, in_=i, axis=mybir.AxisListType.X), LK, LK)
                ts(K1, L0, m1, None, ALU.is_equal, None, LK, LK)
                stt(L1, K1, -1e30, L0, ALU.mult, ALU.add, LK, LK)
                S.op("dve", lambda e, o=m2, i=L1: e.reduce_max(out=o, in_=i, axis=mybir.AxisListType.X), LK, LK)
                ts(K2, L1, m2, None, ALU.is_equal, None, LK, LK)
                tt(dd, m2, m1, ALU.subtract, LK, LK)
                act(dd, dd, AF.Exp, LK, LK)
                ts(g1, dd, 1.0, None, ALU.add, None, LK, LK)
                recip(g1, g1, LK, LK)
                tt(g2, dd, g1, ALU.mult, LK, LK)
                ts(GG, K1, g1, None, ALU.mult, None, LK, LK)
                stt(GG, K2, g2, GG, ALU.mult, ALU.add, LK, LK)
                pt = next_ps()
                mm(pt, (slice(0, NE), slice(0, nt)), GG, cst[0:nt, IDN, 0:nt], True, True, LK + ["cst"])
                cp(GT[0:NE, t0:t0 + nt], ps[pt][0:NE, 0:nt], [("ps", pt)], ["GT"], eng="act")

        def gate_bcast(e):
            ts(Gsel[0:NE, :], GT[0:NE, :], cst[0:NE, IDN, e:e + 1], None, ALU.mult, None, ["GT", "cst"], ["Gsel"])
            for (t0, tn) in TB:
                pi = next_ps()
                mm(pi, (slice(0, 128), slice(0, tn)), cst[0:NE, ONES, :], Gsel[0:NE, t0:t0 + tn], True, True, ["Gsel", "cst"])
                cp(Gb[:, t0:t0 + tn], ps[pi][:, 0:tn], [("ps", pi)], ["Gb"], eng="act")

        def gla_head(l, h, wn):
            RAK = [("RA", h * VH + v) for v in range(VH)]
            QK = [("RM", h * KH + k) for k in range(KH)]
            KK = [("RM", NH * KH + k) for k in range(KH)]
            dma("sp", auh[0:R1, 0:DKH], alup[l, h], [], ["auh"])
            for kh in range(KH):
                proj(wn, cQ + h * KH + kh, 128, TB,
                     lambda t0, tn, pi, kh=kh: cp(RM[:, h * KH + kh, t0:t0 + tn], ps[pi][:, 0:tn], [("ps", pi)], [("RM", h * KH + kh)], eng="act"))
                proj(wn, cK + h * KH + kh, 128, TB,
                     lambda t0, tn, pi, kh=kh: cp(RM[:, NH * KH + kh, t0:t0 + tn], ps[pi][:, 0:tn], [("ps", pi)], [("RM", NH * KH + kh)]))
            for vv in range(VH):
                proj(wn, cV + h * VH + vv, 128, TB,
                     lambda t0, tn, pi, vv=vv: cp(RA[:, h * VH + vv, t0:t0 + tn], ps[pi][:, 0:tn], [("ps", pi)], [("RA", h * VH + vv)],
                                                  eng="act" if vv % 2 else "dve"))
            memset(Sst[:, :, :], 0.0, ["Sst"])
            memset(Sb[:, :, :], 0.0, ["Sb"])
            memset(erun, 1.0, ["erun"])
            for (t0, nt) in TILES:
                pre = t0 < NM
                chunks = [(0, nt)] if nt <= 64 else [(0, 64), (64, 64)]
                pa_ = next_ps()
                mm(pa_, (slice(0, nt), slice(0, DKH)), alT[0:R1, t0:t0 + nt], auh[0:R1, 0:DKH], True, True, ["alT", "auh"])
                act(e1[0:nt, :], ps[pa_][0:nt, 0:DKH], AF.Exp, [("ps", pa_)], ["e1"], scale=-1.0)
                act(ltok[0:nt, :], e1[0:nt, :], AF.Ln, ["e1"], ["ltok"], bias=1.0)
                pe_ = next_ps()
                mm(pe_, (slice(0, nt), slice(0, DKH)), cst[0:nt, UPP, 0:nt], ltok[0:nt, :], True, True, ["ltok", "cst"])
                act(ef[0:nt, :], ps[pe_][0:nt, 0:DKH], AF.Exp, [("ps", pe_)], ["ef"])
                pb_ = next_ps()
                for kh in range(KH):
                    mm(pb_, (slice(0, 128), slice(kh * 128, kh * 128 + nt)), ltok[0:nt, kh * 128:(kh + 1) * 128], cst[0:nt, TRI, 0:nt],
                       True, True, ["ltok", "cst"])
                pbv = ps[pb_][:, 0:KH * 128].rearrange("p (k n) -> p k n", n=128)[:, :, 0:nt]
                act(eB[:, :, 0:nt], pbv, AF.Exp, [("ps", pb_)], ["eB"])
                act(enB[:, :, 0:nt], pbv, AF.Exp, [("ps", pb_)], ["enB"], scale=-1.0)
                pk_ = next_ps()
                for kh in range(KH):
                    mm(pk_, (slice(0, nt), slice(kh * 128, (kh + 1) * 128)), RM[:, NH * KH + kh, t0:t0 + nt], idb[:, :], True, True, KK + ["idb"])
                tt(kte[0:nt, :], ps[pk_][0:nt, 0:DKH], ef[0:nt, :], ALU.mult, [("ps", pk_), "ef"], ["kte"])
                if pre:
                    ts(kte[0:nt, :], kte[0:nt, :], fl[0:nt, 0:1], None, ALU.mult, None, ["kte", "fl"], ["kte"])
                pv_ = next_ps()
                for vv in range(VH):
                    mm(pv_, (slice(0, nt), slice(vv * 128, (vv + 1) * 128)), RA[:, h * VH + vv, t0:t0 + nt], idb[:, :], True, True, RAK + ["idb"])
                cp(vt[0:nt, :], ps[pv_][0:nt, 0:DVH], [("ps", pv_)], ["vt"], eng="act")
                stt(qd[:, :, 0:nt], RM[:, h * KH:(h + 1) * KH, t0:t0 + nt], QSCALE, eB[:, :, 0:nt], ALU.mult, ALU.mult, QK + ["eB"], ["qd"])
                tt(kd[:, :, 0:nt], RM[:, NH * KH:NH * KH + KH, t0:t0 + nt], enB[:, :, 0:nt], ALU.mult, KK + ["enB"], ["kd"])
                ps_ = next_ps()
                for kh in range(KH):
                    mm(ps_, (slice(0, nt), slice(0, nt)), kd[:, kh, 0:nt], qd[:, kh, 0:nt], kh == 0, kh == KH - 1, ["kd", "qd"])
                tt(sT[0:nt, 0:nt], ps[ps_][0:nt, 0:nt], cst[0:nt, CM, 0:nt], ALU.mult, [("ps", ps_), "cst"], ["sT"])
                for (c0, cn) in chunks:
                    cs = slice(c0, c0 + cn)
                    po = next_ps()
                    for vv in range(VH):
                        osl = (slice(0, 128), slice(vv * 64, vv * 64 + cn))
                        mm(po, osl, vt[cs, vv * 128:(vv + 1) * 128], sT[cs, cs], True, False, ["vt", "sT"])
                        for kh in range(KH):
                            mm(po, osl, Sb[:, kh, vv * 128:(vv + 1) * 128], qd[:, kh, cs], False, kh == KH - 1, ["Sb", "qd"])
                    for kh in range(KH):
                        ts(RM[:, h * KH + kh, t0 + c0:t0 + c0 + cn], qd[:, kh, cs], erun[:, kh:kh + 1], None, ALU.mult, None,
                           ["qd", "erun"], [("RM", h * KH + kh)])
                    pov = ps[po][:, 0:VH * 64].rearrange("p (v n) -> p v n", n=64)[:, :, 0:cn]
                    cp(RA[:, h * VH:(h + 1) * VH, t0 + c0:t0 + c0 + cn], pov, [("ps", po)], RAK, eng="act")
                    for kh in range(KH):
                        pS = next_ps()
                        mm(pS, (slice(0, 128), slice(0, DVH)), kte[cs, kh * 128:(kh + 1) * 128], vt[cs, :], True, True, ["kte", "vt"])
                        dec = eB[:, kh, c0 + cn - 1:c0 + cn]
                        stt(Sst[:, kh, :], Sst[:, kh, :], dec, ps[pS][:, 0:DVH], ALU.mult, ALU.add, ["Sst", "eB", ("ps", pS)], ["Sst"])
                        cp(Sb[:, kh, :], Sst[:, kh, :], ["Sst"], ["Sb"], eng="act")
                        if not pre:
                            tt(erun[:, kh:kh + 1], erun[:, kh:kh + 1], dec, ALU.mult, ["erun", "eB"], ["erun"])
            dma("sp", xsrc[h][:, 0:KH * DVH], Sstf, ["Sst"], [("xsrc", h)])
            cp(dsg[:, h * KH:(h + 1) * KH], erun, ["erun"], ["dsg"])
            S.op("pool", lambda e, a=xsrc[h], b=xdst[h]: e.collective_compute(
                "AllGather", ALU.bypass, replica_groups=GROUPS, ins=[a], outs=[b]), [("xsrc", h)], [("xdst", h)], kind="cc")

        def gla_finish_head(l, h, wn):
            RAK = [("RA", h * VH + v) for v in range(VH)]
            memset(Sst[:, :, :], 0.0, ["Sst"])
            for j in range(3):
                dma("sp", Sx[:, :], xdst[h][j * 128:(j + 1) * 128, 0:KH * DVH], [("xdst", h)], ["Sx"])
                ts(acoef, dall[:, j, h * KH:(h + 1) * KH], -1.0, fl[:, 1 + j:2 + j], ALU.add, ALU.mult, ["dall", "fl"], ["acoef"])
                ts(acoef, acoef, 1.0, None, ALU.add, None, ["acoef"], ["acoef"])
                ts(Sx[:, :], Sx[:, :], fl[:, 1 + j:2 + j], None, ALU.mult, None, ["Sx", "fl"], ["Sx"])
                for kh in range(KH):
                    stt(Sst[:, kh, :], Sst[:, kh, :], acoef[:, kh:kh + 1], Sx[:, kh * DVH:(kh + 1) * DVH], ALU.mult, ALU.add,
                        ["Sst", "acoef", "Sx"], ["Sst"])
            cp(Sb[:, :, :], Sst[:, :, :], ["Sst"], ["Sb"], eng="act")
            for vv in range(VH):
                for (t0, tn) in TB:
                    pi = next_ps()
                    for kh in range(KH):
                        mm(pi, (slice(0, 128), slice(0, tn)), Sb[:, kh, vv * 128:(vv + 1) * 128], RM[:, h * KH + kh, t0:t0 + tn],
                           kh == 0, kh == KH - 1, ["Sb", ("RM", h * KH + kh)])
                    tt(RA[:, h * VH + vv, t0:t0 + tn], RA[:, h * VH + vv, t0:t0 + tn], ps[pi][:, 0:tn], ALU.add,
                       [("ps", pi), ("RA", h * VH + vv)], [("RA", h * VH + vv)])
            for bi, (t0, tn) in enumerate(TB):
                pr = next_ps()
                for vv in range(VH):
                    ti = next_tmp()
                    act(tmp[ti][:, 0:tn], RA[:, h * VH + vv, t0:t0 + tn], AF.Square, [("RA", h * VH + vv)], [("tmp", ti)])
                    mm(pr, (slice(0, 128), slice(0, tn)), cst[:, ONES, :], tmp[ti][:, 0:tn], vv == 0, vv == VH - 1, [("tmp", ti), "cst"])
                ta, tb_ = next_tmp(), next_tmp()
                ts(tmp[ta][:, 0:tn], ps[pr][:, 0:tn], 1.0 / DVH, EPS, ALU.mult, ALU.add, [("ps", pr)], [("tmp", ta)])
                act(tmp[tb_][:, 0:tn], tmp[ta][:, 0:tn], AF.Sqrt, [("tmp", ta)], [("tmp", tb_)])
                recip(stat[:, 1, t0:t0 + tn], tmp[tb_][:, 0:tn], [("tmp", tb_)], [("stat", 1, bi)])
            for vv in range(VH):
                jj = h * VH + vv

                def epi_r(t0, tn, pi, jj=jj):
                    ti = next_tmp()
                    act(tmp[ti][:, 0:tn], ps[pi][:, 0:tn], AF.Silu, [("ps", pi)], [("tmp", ti)])
                    tt(tmp[ti][:, 0:tn], tmp[ti][:, 0:tn], stat[:, 1, t0:t0 + tn], ALU.mult, [("tmp", ti)] + SK, [("tmp", ti)])
                    stt(RA[:, jj, t0:t0 + tn], RA[:, jj, t0:t0 + tn], gn[:, l, jj:jj + 1], tmp[ti][:, 0:tn], ALU.mult, ALU.mult,
                        [("RA", jj), "gn", ("tmp", ti)], [("RA", jj)])
                proj(wn, cR + jj, 128, TB, epi_r)

        gather_through(lambda n: wl_of[n] == 0)
        pss, pqq = [next_ps() for _ in TB], [next_ps() for _ in TB]
        for j in range(KC):
            si = j % 3
            dma("sp", stg[si][:], xT[:, j, :], [], [("stg", si)])
            ln_accum(pss, pqq, j, stg[si], ("stg", si))
            dma("sp", zres[:, j, :], stg[si][:], [("stg", si)], [("zres", j)])
        ln_finish(pss, pqq, D)
        ln_apply(zres, 0, 1)

        for l in range(DEPTH):
            vb = 2 + l * 9
            V_CB, V_CNG, V_CNB, V_MG, V_MB, V_FG, V_FB = [vb + i for i in range(7)]
            wn = f"w_in{l}"
            barrier()
            wt_act[0] = wt_mix
            dma("sp", cw[:], convw[l], [], ["cw"])
            if STOP != "noGLA":
                memset(alT[0:32, :], 1.0, ["alT"])
                proj(wn, cA, LOWR, TB, lambda t0, tn, pi: cp(alT[0:LOWR, t0:t0 + tn], ps[pi][0:LOWR, 0:tn], [("ps", pi)], ["alT"]))
                for h in range(NH):
                    gla_head(l, h, wn)
                dma("sp", xsrc[NH][:, 0:NH * KH], dsg, ["dsg"], [("xsrc", NH)])
                S.op("pool", lambda e, a=xsrc[NH], b=xdst[NH]: e.collective_compute(
                    "AllGather", ALU.bypass, replica_groups=GROUPS, ins=[a], outs=[b]), [("xsrc", NH)], [("xdst", NH)], kind="cc")
            memset(Ue[:, :, 0:HALO], 0.0, [("Ue", j) for j in range(KC)])
            for j in range(KC):
                wa = load_w(wn, 0, KC, cG + j)
                wgk = load_w(wn, 0, KC, cG + KC + j)
                for (t0, tn) in TB:
                    pa, pg = next_ps(), next_ps()
                    for k in range(KC):
                        mm(pa, (slice(0, 128), slice(0, tn)), wt[wa][:, k, 0:128], hb[:, k, t0:t0 + tn], k == 0, k == KC - 1, [("wt", wa)] + HBK)
                    for k in range(KC):
                        mm(pg, (slice(0, 128), slice(0, tn)), wt[wgk][:, k, 0:128], hb[:, k, t0:t0 + tn], k == 0, k == KC - 1, [("wt", wgk)] + HBK)
                    ti = next_tmp()
                    act(tmp[ti][:, 0:tn], ps[pg][:, 0:tn], AF.Sigmoid, [("ps", pg)], [("tmp", ti)])
                    u0 = upos(t0)
                    tt(Ue[:, j, u0:u0 + tn], ps[pa][:, 0:tn], tmp[ti][:, 0:tn], ALU.mult, [("ps", pa), ("tmp", ti)], [("Ue", j)])
            for j in range(KC):
                cp(hal[:, j * HALO:(j + 1) * HALO], Ue[:, j, UW - HALO:UW], [("Ue", j)], ["hal"])
            dma("sp", usrc[:, 0:KC * HALO], hal[:], ["hal"], ["usrc"])
            S.op("pool", lambda e: e.collective_compute("AllGather", ALU.bypass, replica_groups=GROUPS,
                                                        ins=[usrc.ap()], outs=[udst.ap()]), ["usrc"], ["udst"], kind="cc")
            gather_after_exchange(l)
            if STOP != "noGLA":
                for j in range(3):
                    dma("sp", dall[:, j, 0:NH * KH], xdst[NH][j * 128:(j + 1) * 128, 0:NH * KH], [("xdst", NH)], ["dall"])
                for h in range(NH):
                    gla_finish_head(l, h, wn)
            barrier()
            ts(hal[:], hal[:], 0.0, None, ALU.mult, None, ["hal", "usrc"], ["hal"])
            for jj in range(3):
                dma("sp", halg[:, :], udst[jj * 128:(jj + 1) * 128, 0:KC * HALO], ["udst"], ["halg"])
                stt(hal[:], halg[:, :], fl[:, 4 + jj:5 + jj], hal[:], ALU.mult, ALU.add, ["halg", "fl", "hal"], ["hal"])
            W = NM + HALO + TL
            pss, pqq = [next_ps() for _ in TB], [next_ps() for _ in TB]
            for j in range(KC):
                hj = hal[:, j * HALO:(j + 1) * HALO]
                stt(hj[:, HALO - NM:HALO], Ue[:, j, HALO:HALO + NM], fl[:, 0:1], hj[:, HALO - NM:HALO], ALU.mult, ALU.add,
                    [("Ue", j), "fl", "hal"], ["hal"])
                cp(Ue[:, j, HALO + NM:HALO + NM + HALO], hj, ["hal"], [("Ue", j)])
                ts(cacc[:, :], Ue[:, j, 0:W], cw[:, j, 0:1], vec[:, V_CB, j:j + 1], ALU.mult, ALU.add, [("Ue", j), "cw", "vec"], ["cacc"])
                for k in range(1, CW):
                    stt(cacc[:, :], Ue[:, j, k:k + W], cw[:, j, k:k + 1], cacc[:, :], ALU.mult, ALU.add, [("Ue", j), "cw", "cacc"], ["cacc"])
                si = j % 3
                cp(stg[si][:, 0:NM], cacc[:, 0:NM], ["cacc"], [("stg", si)])
                cp(stg[si][:, NM:T], cacc[:, NM + HALO:W], ["cacc"], [("stg", si)], eng="act")
                ln_accum(pss, pqq, j, stg[si], ("stg", si))
                cp(Ue[:, j, HALO:HALO + NM], stg[si][:, 0:NM], [("stg", si)], [("Ue", j)])
                cp(Ue[:, j, HALO + NM + HALO:UW], stg[si][:, NM:T], [("stg", si)], [("Ue", j)], eng="act")
            ln_finish(pss, pqq, D)
            for j in range(KC):
                si = j % 3
                cp(stg[si][:, 0:NM], Ue[:, j, HALO:HALO + NM], [("Ue", j)], [("stg", si)])
                cp(stg[si][:, NM:T], Ue[:, j, HALO + NM + HALO:UW], [("Ue", j)], [("stg", si)])
                tt(stg[si][:], stg[si][:], stat[:, 0, :], ALU.subtract, [("stg", si)] + SK, [("stg", si)])
                tt(stg[si][:], stg[si][:], stat[:, 1, :], ALU.mult, [("stg", si)] + SK, [("stg", si)])
                ts(stg[si][:], stg[si][:], vec[:, V_CNG, j:j + 1], vec[:, V_CNB, j:j + 1], ALU.mult, ALU.add, [("stg", si), "vec"], [("stg", si)])
                act(Ue[:, j, HALO:HALO + NM], stg[si][:, 0:NM], AF.Silu, [("stg", si)], [("Ue", j)])
                act(Ue[:, j, HALO + NM + HALO:UW], stg[si][:, NM:T], AF.Silu, [("stg", si)], [("Ue", j)])
            UK = [("Ue", j) for j in range(KC)]
            for j in range(KC):
                wa = load_w(f"w_convo{l}", 0, KC, j)
                wgk = load_w(wn, 0, KC, cGB + j)
                for (t0, tn) in TB:
                    pa, pg = next_ps(), next_ps()
                    u0 = upos(t0)
                    for k in range(KC):
                        mm(pa, (slice(0, 128), slice(0, tn)), wt[wa][:, k, 0:128], Ue[:, k, u0:u0 + tn], k == 0, k == KC - 1, [("wt", wa)] + UK)
                    for k in range(KC):
                        mm(pg, (slice(0, 128), slice(0, tn)), wt[wgk][:, k, 0:128], hb[:, k, t0:t0 + tn], k == 0, k == KC - 1, [("wt", wgk)] + HBK)
                    ti = next_tmp()
                    act(tmp[ti][:, 0:tn], ps[pg][:, 0:tn], AF.Sigmoid, [("ps", pg)], [("tmp", ti)])
                    tt(RM[:, j, t0:t0 + tn], ps[pa][:, 0:tn], tmp[ti][:, 0:tn], ALU.mult, [("ps", pa), ("tmp", ti)], [("RM", j)])
            if STOP != "noGLA":
                RAALL = [("RA", j) for j in range(KC)]
                for j in range(KC):
                    wa = load_w(f"w_glao{l}", 0, KC, j)
                    wgk = load_w(wn, 0, KC, cGA + j)
                    for (t0, tn) in TB:
                        pa, pg = next_ps(), next_ps()
                        for k in range(KC):
                            mm(pa, (slice(0, 128), slice(0, tn)), wt[wa][:, k, 0:128], RA[:, k, t0:t0 + tn], k == 0, k == KC - 1, [("wt", wa)] + RAALL)
                        for k in range(KC):
                            mm(pg, (slice(0, 128), slice(0, tn)), wt[wgk][:, k, 0:128], hb[:, k, t0:t0 + tn], k == 0, k == KC - 1, [("wt", wgk)] + HBK)
                        ti = next_tmp()
                        act(tmp[ti][:, 0:tn], ps[pg][:, 0:tn], AF.Sigmoid, [("ps", pg)], [("tmp", ti)])
                        tt(tmp[ti][:, 0:tn], ps[pa][:, 0:tn], tmp[ti][:, 0:tn], ALU.mult, [("ps", pa), ("tmp", ti)], [("tmp", ti)])
                        tt(RM[:, j, t0:t0 + tn], RM[:, j, t0:t0 + tn], tmp[ti][:, 0:tn], ALU.add, [("RM", j), ("tmp", ti)], [("RM", j)])
            MK = [("RM", j) for j in range(KC)]
            for j in range(KC):
                wa = load_w(f"w_out{l}", 0, KC, j)
                si = j % 3
                dma("sp", stg[si][:], hres[:, j, :], [("hres", j)], [("stg", si)])
                for (t0, tn) in TB:
                    pa = next_ps()
                    for k in range(KC):
                        mm(pa, (slice(0, 128), slice(0, tn)), wt[wa][:, k, 0:128], RM[:, k, t0:t0 + tn], k == 0, k == KC - 1, [("wt", wa)] + MK)
                    stt(stg[si][:, t0:t0 + tn], stg[si][:, t0:t0 + tn], ALPHA, ps[pa][:, 0:tn], ALU.mult, ALU.add, [("stg", si), ("ps", pa)], [("stg", si)])
                dma("sp", zres[:, j, :], stg[si][:], [("stg", si)], [("zres", j)])
            ln_from_dram(zres)
            ln_apply(zres, V_MG, V_MB)
            barrier()
            wt_act[0] = wt_ffn
            last_out = out if l == DEPTH - 1 else None
            if l % 2 == 0:
                ffn(f"ffn1_{l}", f"ffn3_{l}", f"ffn2_{l}", True, False)
            else:
                router(l // 2)
                for e in range(NE):
                    gate_bcast(e)
                    ffn(f"moe1_{l}_{e}", f"moe3_{l}_{e}", f"moe2_{l}_{e}", e == 0, True)
            ln_from_dram(zres)
            ln_apply(zres, V_FG, V_FB, write_out=last_out)
        S.emit(nc, sems, dma_sems, cc_sem)
    return nc


def make_inputs(cfg, inp):
    D, SEQ, DEPTH, DFF, NE, NM, LOWR, CW = (cfg[k] for k in ("D", "SEQ", "DEPTH", "DFF", "NE", "NMETA", "LOWR", "CW"))
    KC = D // 128
    TL = SEQ // 4
    T = NM + TL
    f32 = lambda a: np.ascontiguousarray(np.asarray(a), dtype=np.float32)
    fm = lambda v: f32(v).reshape(KC, 128).T
    x = f32(inp["x"])
    meta = f32(inp["meta_tokens"])
    nv = 2 + DEPTH * 9
    vecs = np.zeros((128, nv, KC), np.float32)
    vecs[:, 0] = fm(inp["ln_in_g"]); vecs[:, 1] = fm(inp["ln_in_b"])
    for l in range(DEPTH):
        b = 2 + l * 9
        for i, k in enumerate(("conv_b", "conv_norm_g", "conv_norm_b", "ln_mix_g", "ln_mix_b", "ln_ffn_g", "ln_ffn_b")):
            vecs[:, b + i] = fm(f32(inp[k])[l])
    convw = np.ascontiguousarray(f32(inp["conv_w"]).reshape(DEPTH, CW, KC, 128).transpose(0, 3, 2, 1))
    gnorm = np.ascontiguousarray(f32(inp["gla_norm_g"]).reshape(DEPTH, KC, 128).transpose(2, 0, 1))
    NH = 4
    DK = D // 2
    DKH = DK // NH
    al = np.concatenate([f32(inp["w_alpha_up"]), f32(inp["b_alpha"])[:, None, :]], 1)
    alup = np.ascontiguousarray(al.reshape(DEPTH, LOWR + 1, NH, DKH).transpose(0, 2, 1, 3))
    NMOE = max(1, DEPTH // 2)
    rtr = np.zeros((NMOE, 128, KC, NE), np.float32)
    rtb = np.zeros((NMOE, 1, NE), np.float32)
    if DEPTH // 2:
        rtr[:] = f32(inp["router_w"]).reshape(DEPTH // 2, KC, 128, NE).transpose(0, 2, 1, 3)
        rtb[:, 0] = f32(inp["router_b"])
    consts = np.zeros((128, 5, 128), np.float32)
    ii = np.arange(128)
    same = (ii[:, None] // 64) == (ii[None, :] // 64)
    consts[:, 0] = np.eye(128)
    consts[:, 1] = (same & (ii[:, None] <= ii[None, :])) * (-1.0 / 16.0)
    consts[:, 2] = (same & (ii[:, None] > ii[None, :])) * (-1.0 / 16.0)
    consts[:, 3] = 1.0
    consts[:, 4] = (same & (ii[:, None] <= ii[None, :])) * 1.0
    shared = dict(vecs=vecs, convw=convw, gnorm=gnorm, alup=alup, rtr=rtr, rtb=rtb, consts=consts)
    wsrc = {}
    for l in range(DEPTH):
        wsrc[f"w_in{l}"] = inp["w_in"][l]; wsrc[f"w_glao{l}"] = inp["w_gla_o"][l]
        wsrc[f"w_convo{l}"] = inp["w_conv_o"][l]; wsrc[f"w_out{l}"] = inp["w_out"][l]
        if l % 2 == 0:
            wsrc[f"ffn1_{l}"] = inp["ffn_w1"][l // 2]; wsrc[f"ffn3_{l}"] = inp["ffn_w3"][l // 2]; wsrc[f"ffn2_{l}"] = inp["ffn_w2"][l // 2]
        else:
            for e in range(NE):
                wsrc[f"moe1_{l}_{e}"] = inp["moe_w1"][l // 2][e]; wsrc[f"moe3_{l}_{e}"] = inp["moe_w3"][l // 2][e]
                wsrc[f"moe2_{l}_{e}"] = inp["moe_w2"][l // 2][e]
    DK_ = D // 2
    oA_ = 2 * DK_ + 2 * D

    def tile_major(n, w):
        w = f32(w)
        if n.startswith("w_in"):
            w = np.concatenate([w[:, :oA_], w[:, oA_ + LOWR:], w[:, oA_:oA_ + LOWR], np.zeros((w.shape[0], 128 - LOWR), np.float32)], 1)
        K_, N_ = w.shape
        return np.ascontiguousarray(w.reshape(K_ // 128, 128, N_ // 128, 128).transpose(2, 1, 0, 3))

    wsh = [dict() for _ in range(4)]
    for n, w in wsrc.items():
        wtm = tile_major(n, w)
        for r in range(4):
            wsh[r][n] = shard_weight(wtm, r)
        del wtm
    maps = []
    for core in range(8):
        b, c = core // 4, core % 4
        tok = np.concatenate([meta, x[b, c * TL:(c + 1) * TL]], 0)
        xT = np.ascontiguousarray(tok.T.reshape(KC, 128, T).transpose(1, 0, 2))
        fl = np.zeros((128, 8), np.float32)
        fl[:, 0] = 1.0 if c == 0 else 0.0
        for j in range(3):
            fl[:, 1 + j] = 1.0 if j < c else 0.0
            fl[:, 4 + j] = 1.0 if j == c - 1 else 0.0
        m = dict(shared)
        m.update(xT=xT, flags=fl)
        m.update(wsh[c])
        maps.append(m)
    return maps


def run(cfg, inp):
    nc = build(cfg)
    maps = make_inputs(cfg, inp)
    res = run_bass_kernel_spmd(nc, maps, core_ids=list(range(8)))
    D, SEQ = cfg["D"], cfg["SEQ"]
    TL = SEQ // 4
    B = 2
    o = np.zeros((B, SEQ, D), np.float32)
    for core in range(8):
        b, c = core // 4, core % 4
        y = res.results[core]["out"]
        o[b, c * TL:(c + 1) * TL] = y.transpose(2, 1, 0).reshape(TL, D)
    return o


def kernel(**inputs):
    return run(dict(CFG_FULL), inputs)
```

```python
import numpy as np
import concourse.bass as bass
import concourse.mybir as mybir
from concourse.bass_utils import run_bass_kernel_spmd

F32 = mybir.dt.float32
BF16 = mybir.dt.bfloat16
AF = mybir.ActivationFunctionType
ALU = mybir.AluOpType

CFG_FULL = dict(D=2048, SEQ=4096, DEPTH=4, DFF=5632, NE=8, NMETA=16, LOWR=16, CW=31)
PR, PC = 256, 2048
PIECE = PR * PC
GROUPS = [[0, 1, 2, 3], [4, 5, 6, 7]]
GQOS = {"dma_qos": "P2"}


class Op:
    __slots__ = ("eng", "fn", "deps", "kind", "inc", "sem", "val", "idx")


class Sched:
    def __init__(self):
        self.ops = []
        self.lastw = {}
        self.readers = {}
        self.last_barrier = None
        self.last_eng = {}
        self.recent_sp = []

    def op(self, eng, fn, reads=(), writes=(), kind="c"):
        o = Op()
        o.eng, o.fn, o.kind, o.inc, o.idx = eng, fn, kind, False, len(self.ops)
        deps = set()
        for b in reads:
            w = self.lastw.get(b)
            if w is not None:
                deps.add(w)
        for b in writes:
            w = self.lastw.get(b)
            if w is not None:
                deps.add(w)
            for r in self.readers.get(b, ()):
                deps.add(r)
        deps.discard(o.idx)
        if self.last_barrier is not None and eng != "pool":
            deps.add(self.last_barrier)
        o.deps = deps
        if kind == "dma" and eng in ("sp", "act"):
            self.recent_sp = (self.recent_sp + [o.idx])[-16:]
        elif kind == "c":
            self.last_eng[eng] = o.idx
        for b in reads:
            lst = self.readers.setdefault(b, [])
            if kind == "c":
                lst[:] = [r for r in lst if not (self.ops[r].eng == eng and self.ops[r].kind == "c")]
            lst.append(o.idx)
        for b in writes:
            self.lastw[b] = o.idx
            self.readers[b] = []
        self.ops.append(o)
        return o

    def barrier(self, fn):
        o = self.op("dve", fn, [], [])
        o.deps |= set(self.last_eng.values()) | set(self.recent_sp)
        o.deps.discard(o.idx)
        self.last_barrier = o.idx
        return o

    def emit(self, nc, sems, dma_sems, cc_sem):
        ops = self.ops
        for o in ops:
            for d in o.deps:
                p = ops[d]
                if p.eng == "pe" and o.eng == "pe" and p.kind == "c":
                    continue
                p.inc = True
        cnt = {e: 0 for e in sems}
        dcnt = {}
        rr = {"sp": 0, "pool": 0, "act": 0}
        ccn = 0
        prev_on_sem = {}
        for o in ops:
            if o.kind == "dma":
                lst = dma_sems[o.eng]
                s = lst[rr[o.eng] % len(lst)]
                rr[o.eng] += 1
                pv = prev_on_sem.get(id(s))
                if pv is not None:
                    o.deps.add(pv)
                prev_on_sem[id(s)] = o.idx
                dcnt[id(s)] = dcnt.get(id(s), 0) + 16
                o.sem, o.val, o.inc = s, dcnt[id(s)], True
            elif o.kind == "cc":
                ccn += 1
                o.sem, o.val, o.inc = cc_sem, ccn, True
            elif o.inc:
                cnt[o.eng] += 1
                o.sem, o.val = sems[o.eng], cnt[o.eng]
        per = {"pe": [], "act": [], "dve": [], "pool": [], "sp": []}
        waited = {e: {} for e in per}
        for o in ops:
            ws = {}
            for d in o.deps:
                p = ops[d]
                if not p.inc:
                    continue
                if p.eng == "pe" and o.eng == "pe" and p.kind == "c":
                    continue
                k = id(p.sem)
                if waited[o.eng].get(k, 0) >= p.val:
                    continue
                if k not in ws or ws[k][1] < p.val:
                    ws[k] = (p.sem, p.val)
            for k, (s, v) in ws.items():
                waited[o.eng][k] = v
            per[o.eng].append((list(ws.values()), o))
        engs = {"pe": "tensor", "act": "scalar", "dve": "vector", "pool": "gpsimd", "sp": "sync"}
        with nc.Block() as block:
            for en, lst in per.items():
                def body(e, lst=lst):
                    for ws, o in lst:
                        for s, v in ws:
                            e.wait_ge(s, v)
                        ins = o.fn(e)
                        if o.inc:
                            if o.kind == "dma":
                                ins.then_inc(o.sem, 16)
                            elif o.kind == "cc":
                                ins.then_inc(o.sem)
                            else:
                                ins.then_inc(o.sem, 1)
                    if en in ("sp", "pool", "act"):
                        for s in dma_sems[en]:
                            v = dcnt.get(id(s), 0)
                            if v:
                                e.wait_ge(s, v)
                getattr(block, engs[en])(body)


def n_pieces(numel):
    return -(-numel // (4 * PIECE))


def shard_weight(w, r):
    flat = np.ascontiguousarray(w, dtype=np.float32).reshape(-1)
    npc = n_pieces(flat.size)
    pad = npc * 4 * PIECE - flat.size
    if pad:
        flat = np.concatenate([flat, np.zeros(pad, np.float32)])
    return np.ascontiguousarray(flat.reshape(npc, 4, PR, PC)[:, r])


def build(cfg):
    D, SEQ, DEPTH, DFF, NE = cfg["D"], cfg["SEQ"], cfg["DEPTH"], cfg["DFF"], cfg["NE"]
    NM, LOWR, CW = cfg["NMETA"], cfg["LOWR"], cfg["CW"]
    STOP = cfg.get("STOP")
    KC = D // 128
    DK = D // 2
    NH = 4
    DKH = DK // NH
    DVH = D // NH
    KH = DKH // 128
    VH = DVH // 128
    FC = DFF // 128
    TL = SEQ // 4
    T = NM + TL
    DIN = DK + DK + D + D + LOWR + 2 * D + D + D
    oQ, oK, oV, oR, oA = 0, DK, 2 * DK, 2 * DK + D, 2 * DK + 2 * D
    oG = oA + LOWR
    oGA = oG + 2 * D
    oGB = oGA + D
    cQ, cK, cV = 0, DK // 128, 2 * DK // 128
    cR = cV + KC
    cG = cR + KC
    cGA = cG + 2 * KC
    cGB = cGA + KC
    cA = cGB + KC
    DINP = (cA + 1) * 128
    ALPHA = (2.0 * DEPTH) ** 0.25
    QSCALE = DKH ** -0.5
    EPS = 1e-5
    HALO = CW - 1
    UW = HALO + NM + HALO + TL
    R1 = LOWR + 1
    NMOE = max(1, DEPTH // 2)
    TB = [(0, NM)] + [(NM + i, min(512, TL - i)) for i in range(0, TL, 512)]
    NB = len(TB)
    TILES = [(0, NM)] + [(NM + i, 128) for i in range(0, TL, 128)]

    def upos(t0):
        return HALO + t0 if t0 < NM else HALO + NM + HALO + (t0 - NM)

    nc = bass.Bass("TRN2", target_bir_lowering=False, num_devices=8)
    S = Sched()

    def dt_in(name, shape):
        return nc.dram_tensor(name, list(shape), F32, kind="ExternalInput")

    xT = dt_in("xT", [128, KC, T])
    vecs = dt_in("vecs", [128, 2 + DEPTH * 9, KC])
    convw = dt_in("convw", [DEPTH, 128, KC, CW])
    gnorm = dt_in("gnorm", [128, DEPTH, KC])
    alup = dt_in("alup", [DEPTH, NH, R1, DKH])
    rtr = dt_in("rtr", [NMOE, 128, KC, NE])
    rtb = dt_in("rtb", [NMOE, 1, NE])
    consts = dt_in("consts", [128, 5, 128])
    flags = dt_in("flags", [128, 8])
    out = nc.dram_tensor("out", [128, KC, TL], F32, kind="ExternalOutput")

    wspec = []
    for l in range(DEPTH):
        wspec += [(f"w_in{l}", D, DINP, l), (f"w_glao{l}", D, D, l), (f"w_convo{l}", D, D, l), (f"w_out{l}", D, D, l)]
        if l % 2 == 0:
            wspec += [(f"ffn1_{l}", D, DFF, l), (f"ffn3_{l}", D, DFF, l), (f"ffn2_{l}", DFF, D, l)]
        else:
            for e in range(NE):
                wspec += [(f"moe1_{l}_{e}", D, DFF, l), (f"moe3_{l}_{e}", D, DFF, l), (f"moe2_{l}_{e}", DFF, D, l)]
    win, wbn, wg, wview = {}, {}, {}, {}
    for name, K, N, _ in wspec:
        npc = n_pieces(K * N)
        win[name] = dt_in(name, [npc, PR, PC])
        wbn[name] = nc.dram_tensor(name + "_b", [npc, PR, PC], BF16)
        wg[name] = nc.dram_tensor(name + "_g", [npc, 4 * PR, PC], BF16)
        wview[name] = wg[name].ap().rearrange("a b c -> (a b c)")[0:K * N].rearrange("(j p k c) -> j p k c", p=128, k=K // 128, c=128)

    hres = nc.dram_tensor("hres", [128, KC, T], F32)
    zres = nc.dram_tensor("zres", [128, KC, T], F32)
    xsrc = nc.dram_tensor("xsrc", [NH + 1, 128, 1024], F32)
    xdst = nc.dram_tensor("xdst", [NH + 1, 512, 1024], F32)
    usrc = nc.dram_tensor("usrc", [128, 1024], F32)
    udst = nc.dram_tensor("udst", [512, 1024], F32)

    import contextlib
    es = contextlib.ExitStack()
    sb = lambda name, shape, dt=F32: es.enter_context(nc.sbuf_tensor(name, list(shape), dt))
    with es:
        hb = sb("hb", [128, KC, T], BF16)
        BIGN = KC * (2 * T + UW)
        assert FC * T <= BIGN
        big = sb("big", [128, BIGN], BF16)
        RA = big[:, 0:KC * T].rearrange("p (c t) -> p c t", t=T)
        Ue = big[:, KC * T:KC * T + KC * UW].rearrange("p (c t) -> p c t", t=UW)
        RM = big[:, KC * (T + UW):BIGN].rearrange("p (c t) -> p c t", t=T)
        HH = big[:, 0:FC * T].rearrange("p (c t) -> p c t", t=T)
        stat = sb("stat", [128, 2, T])
        vec = sb("vec", [128, 2 + DEPTH * 9, KC])
        cw = sb("cw", [128, KC, CW])
        gn = sb("gn", [128, DEPTH, KC])
        cst = sb("cst", [128, 5, 128])
        idb = sb("idb", [128, 128], BF16)
        fl = sb("fl", [128, 8])
        small = sb("small", [128, 80])
        KG = max(KC, 11)
        wt = [sb(f"wt{i}", [128, KG, 128], BF16) for i in range(4)]
        stg = [sb(f"stg{i}", [128, T]) for i in range(3)]
        tmp = [sb(f"tmp{i}", [128, 512]) for i in range(4)]
        hal = sb("hal", [128, KC * HALO])
        NF = max(6 * DKH + 2 * KH * DVH, KC * HALO + NM + HALO + TL, KC * NE + 3 * T + 64)
        scrF = sb("scrF", [128, NF])
        NHH = 3 * DKH + DVH + 128 + KH * DVH
        scrH = sb("scrH", [128, NHH], BF16)
        ps = [es.enter_context(nc.psum_tensor(f"ps{i}", [128, 512], F32)) for i in range(8)]
        sems = {e: es.enter_context(nc.semaphore(f"s_{e}")) for e in ("pe", "act", "dve")}
        dma_sems = {q: [es.enter_context(nc.semaphore(f"d_{q}{i}")) for i in range(8)] for q in ("sp", "pool")}
        dma_sems["act"] = [es.enter_context(nc.semaphore(f"d_act{i}")) for i in range(4)]
        cc_sem = es.enter_context(nc.semaphore("cc"))

        o_ = 0
        auh = scrF[:, o_:o_ + DKH]; o_ += DKH
        e1 = scrF[:, o_:o_ + DKH]; o_ += DKH
        ltok = scrF[:, o_:o_ + DKH]; o_ += DKH
        ef = scrF[:, o_:o_ + DKH]; o_ += DKH
        eB = scrF[:, o_:o_ + DKH].rearrange("p (k n) -> p k n", n=128); o_ += DKH
        enB = scrF[:, o_:o_ + DKH].rearrange("p (k n) -> p k n", n=128); o_ += DKH
        Sstf = scrF[:, o_:o_ + KH * DVH]
        Sst = Sstf.rearrange("p (k n) -> p k n", n=DVH); o_ += KH * DVH
        Sx = scrF[:, o_:o_ + KH * DVH]; o_ += KH * DVH
        halg = scrF[:, 0:KC * HALO]
        cacc = scrF[:, KC * HALO:KC * HALO + NM + HALO + TL]
        rtw = scrF[:, 0:KC * NE].rearrange("p (k n) -> p k n", n=NE)
        GT = scrF[:, KC * NE:KC * NE + T]
        Gsel = scrF[:, KC * NE + T:KC * NE + 2 * T]
        Gb = scrF[:, KC * NE + 2 * T:KC * NE + 3 * T]
        lg = scrF[:, KC * NE + 3 * T:KC * NE + 3 * T + 64]
        o_ = 0
        kte = scrH[:, o_:o_ + DKH]; o_ += DKH
        vt = scrH[:, o_:o_ + DVH]; o_ += DVH
        qd = scrH[:, o_:o_ + DKH].rearrange("p (k n) -> p k n", n=128); o_ += DKH
        kd = scrH[:, o_:o_ + DKH].rearrange("p (k n) -> p k n", n=128); o_ += DKH
        sT = scrH[:, o_:o_ + 128]; o_ += 128
        Sb = scrH[:, o_:o_ + KH * DVH].rearrange("p (k n) -> p k n", n=DVH); o_ += KH * DVH
        wt_mix = list(range(len(wt)))
        for i in range(2):
            if FC * T + (i + 1) * KG * 128 <= BIGN:
                wt.append(big[:, FC * T + i * KG * 128:FC * T + (i + 1) * KG * 128].rearrange("p (k c) -> p k c", c=128))
        if NHH >= KG * 128:
            wt.append(scrH[:, 0:KG * 128].rearrange("p (k c) -> p k c", c=128))
        wt_ffn = list(range(len(wt)))
        wt_act = [wt_mix]
        alT = stat[:, 0, :]
        erun = small[:, 0:KH]
        acoef = small[:, 8:8 + KH]
        dsg = small[:, 16:16 + NH * KH]
        dall = small[:, 32:32 + 3 * 8].rearrange("p (j n) -> p j n", n=8) if NH * KH <= 8 else None
        sm = small[:, 56:64]

        psi = [0]

        def next_ps():
            psi[0] = (psi[0] + 1) % 8
            return psi[0]

        tmi = [0]

        def next_tmp():
            tmi[0] = (tmi[0] + 1) % 4
            return tmi[0]

        def dma(q, out_ap, in_ap, reads, writes):
            S.op(q, lambda e: e.dma_start(out=out_ap, in_=in_ap), reads, writes, kind="dma")

        def mmr(out_ap, pi, lhsT, rhs, start, stop, reads):
            S.op("pe", lambda e: e.matmul(out_ap, lhsT, rhs, start=start, stop=stop), reads, [("ps", pi)])

        def mm(pi, sl, lhsT, rhs, start, stop, reads):
            mmr(ps[pi][sl[0], sl[1]], pi, lhsT, rhs, start, stop, reads)

        def act(out_ap, in_ap, func, reads, writes, bias=None, scale=None):
            kw = {}
            if bias is not None:
                kw["bias"] = bias
            if scale is not None:
                kw["scale"] = scale
            S.op("act", lambda e: e.activation(out=out_ap, in_=in_ap, func=func, **kw), reads, writes)

        def tt(out_ap, a, b, op, reads, writes, eng="dve"):
            S.op(eng, lambda e: e.tensor_tensor(out=out_ap, in0=a, in1=b, op=op), reads, writes)

        def ts(out_ap, a, s1, s2, op0, op1, reads, writes, eng="dve"):
            if s2 is None:
                S.op(eng, lambda e: e.tensor_scalar(out=out_ap, in0=a, scalar1=s1, scalar2=None, op0=op0), reads, writes)
            else:
                S.op(eng, lambda e: e.tensor_scalar(out=out_ap, in0=a, scalar1=s1, scalar2=s2, op0=op0, op1=op1), reads, writes)

        def stt(out_ap, a, s, b, op0, op1, reads, writes, eng="dve"):
            S.op(eng, lambda e: e.scalar_tensor_tensor(out=out_ap, in0=a, scalar=s, in1=b, op0=op0, op1=op1), reads, writes)

        def cp(out_ap, in_ap, reads, writes, eng="dve"):
            if eng == "act":
                S.op(eng, lambda e: e.copy(out=out_ap, in_=in_ap), reads, writes)
            else:
                S.op(eng, lambda e: e.tensor_copy(out=out_ap, in_=in_ap), reads, writes)

        def memset(ap, v, writes, eng="dve"):
            S.op(eng, lambda e: e.memset(ap, v), [], writes)

        def recip(out_ap, in_ap, reads, writes):
            S.op("dve", lambda e: e.reciprocal(out=out_ap, in_=in_ap), reads, writes)

        def barrier():
            S.barrier(lambda e: e.memset(small[:, 72:73], 0.0))

        dma("sp", vec[:], vecs.ap(), [], ["vec"])
        dma("sp", gn[:], gnorm.ap(), [], ["gn"])
        dma("sp", cst[:], consts.ap(), [], ["cst"])
        dma("sp", fl[:], flags.ap(), [], ["fl"])
        cp(idb[:], cst[:, 0, :], ["cst"], ["idb"])
        IDN, TRI, UPP, ONES, CM = 0, 1, 2, 3, 4

        gq = [name for name, _, _, _ in wspec]
        gpos = [0]

        def gather_through(pred):
            last = max([i for i, n in enumerate(gq) if pred(n)], default=-1)
            while gpos[0] <= last:
                name = gq[gpos[0]]
                gpos[0] += 1
                for p in range(win[name].shape[0]):
                    dma("pool", wbn[name][p], win[name][p], [], [("wb", name, p)])
                    S.op("pool", lambda e, a=wbn[name][p], b=wg[name][p]: e.collective_compute(
                        "AllGather", ALU.bypass, replica_groups=GROUPS, ins=[a], outs=[b], **GQOS),
                        [("wb", name, p)], [("wg", name)], kind="cc")

        wl_of = {name: wl for name, _, _, wl in wspec}

        def first_part(n, lmax):
            if wl_of[n] < lmax:
                return True
            if wl_of[n] > lmax:
                return False
            return not (n.startswith("moe") and int(n.split("_")[2]) >= NE // 2)

        def gather_after_exchange(l):
            if l >= DEPTH - 2:
                gather_through(lambda n: True)
            elif (l + 1) % 2 == 1:
                gather_through(lambda n: first_part(n, l + 1))
            else:
                gather_through(lambda n: first_part(n, min(l + 3, DEPTH - 1)))

        wbi = [0]

        def load_w(name, k0, nk, j, ncol=128):
            i = wt_act[0][wbi[0] % len(wt_act[0])]
            wbi[0] += 1
            dma("sp", wt[i][:, 0:nk, 0:ncol], wview[name][j, :, k0:k0 + nk, 0:ncol], [("wg", name)], [("wt", i)])
            return i

        HBK = [("hb", j) for j in range(KC)]

        def proj(name, jc, ncol, blocks, epi, rhs=None, rkeys=None, rpos=None):
            rhs = hb if rhs is None else rhs
            rkeys = HBK if rkeys is None else rkeys
            rpos = rpos or (lambda t0: t0)
            wi = load_w(name, 0, KC, jc, ncol)
            for (t0, tn) in blocks:
                pi = next_ps()
                r0 = rpos(t0)
                for k in range(KC):
                    mm(pi, (slice(0, ncol), slice(0, tn)), wt[wi][:, k, 0:ncol], rhs[:, k, r0:r0 + tn], k == 0, k == KC - 1,
                       [("wt", wi)] + rkeys)
                epi(t0, tn, pi)

        def ln_accum(pss, pqq, j, zap, zkey):
            for bi, (t0, tn) in enumerate(TB):
                ti = next_tmp()
                act(tmp[ti][:, 0:tn], zap[:, t0:t0 + tn], AF.Square, [zkey], [("tmp", ti)])
                mm(pss[bi], (slice(0, 128), slice(0, tn)), cst[:, ONES, :], zap[:, t0:t0 + tn], j == 0, j == KC - 1, [zkey, "cst"])
                mm(pqq[bi], (slice(0, 128), slice(0, tn)), cst[:, ONES, :], tmp[ti][:, 0:tn], j == 0, j == KC - 1, [("tmp", ti), "cst"])

        def ln_finish(pss, pqq, n):
            for bi, (t0, tn) in enumerate(TB):
                sl = slice(t0, t0 + tn)
                ta, tb_ = next_tmp(), next_tmp()
                ts(stat[:, 0, sl], ps[pss[bi]][:, 0:tn], 1.0 / n, None, ALU.mult, None, [("ps", pss[bi])], [("stat", 0, bi)])
                ts(tmp[ta][:, 0:tn], ps[pqq[bi]][:, 0:tn], 1.0 / n, None, ALU.mult, None, [("ps", pqq[bi])], [("tmp", ta)])
                tt(tmp[tb_][:, 0:tn], stat[:, 0, sl], stat[:, 0, sl], ALU.mult, [("stat", 0, bi)], [("tmp", tb_)])
                tt(tmp[ta][:, 0:tn], tmp[ta][:, 0:tn], tmp[tb_][:, 0:tn], ALU.subtract, [("tmp", ta), ("tmp", tb_)], [("tmp", ta)])
                ts(tmp[ta][:, 0:tn], tmp[ta][:, 0:tn], EPS, None, ALU.add, None, [("tmp", ta)], [("tmp", ta)])
                act(tmp[tb_][:, 0:tn], tmp[ta][:, 0:tn], AF.Sqrt, [("tmp", ta)], [("tmp", tb_)])
                recip(stat[:, 1, sl], tmp[tb_][:, 0:tn], [("tmp", tb_)], [("stat", 1, bi)])

        SK = [("stat", r, b) for r in (0, 1) for b in range(NB)]

        def ln_from_dram(src):
            pss, pqq = [next_ps() for _ in TB], [next_ps() for _ in TB]
            for j in range(KC):
                si = j % 3
                dma("sp", stg[si][:], src[:, j, :], [("zres", j)], [("stg", si)])
                ln_accum(pss, pqq, j, stg[si], ("stg", si))
            ln_finish(pss, pqq, D)

        def ln_apply(src_dram, gi, bi_, write_out=None):
            for j in range(KC):
                si = j % 3
                dma("sp", stg[si][:], src_dram[:, j, :], [("zres", j)], [("stg", si)])
                tt(stg[si][:], stg[si][:], stat[:, 0, :], ALU.subtract, [("stg", si)] + SK, [("stg", si)])
                tt(stg[si][:], stg[si][:], stat[:, 1, :], ALU.mult, [("stg", si)] + SK, [("stg", si)])
                ts(stg[si][:], stg[si][:], vec[:, gi, j:j + 1], vec[:, bi_, j:j + 1], ALU.mult, ALU.add, [("stg", si), "vec"], [("stg", si)])
                cp(hb[:, j, :], stg[si][:], [("stg", si)], [("hb", j)], eng="act")
                dma("act", hres[:, j, :], stg[si][:], [("stg", si)], [("hres", j)])
                if write_out is not None:
                    dma("sp", write_out[:, j, :], stg[si][:, NM:T], [("stg", si)], [("out", j)])

        def ffn(w1n, w3n, w2n, first, gated):
            for f in range(FC):
                wa = load_w(w1n, 0, KC, f)
                wgk = load_w(w3n, 0, KC, f)
                for (t0, tn) in TB:
                    pa, pg = next_ps(), next_ps()
                    for k in range(KC):
                        mm(pa, (slice(0, 128), slice(0, tn)), wt[wa][:, k, 0:128], hb[:, k, t0:t0 + tn], k == 0, k == KC - 1, [("wt", wa)] + HBK)
                    for k in range(KC):
                        mm(pg, (slice(0, 128), slice(0, tn)), wt[wgk][:, k, 0:128], hb[:, k, t0:t0 + tn], k == 0, k == KC - 1, [("wt", wgk)] + HBK)
                    ti = next_tmp()
                    act(tmp[ti][:, 0:tn], ps[pa][:, 0:tn], AF.Silu, [("ps", pa)], [("tmp", ti)])
                    tt(HH[:, f, t0:t0 + tn], ps[pg][:, 0:tn], tmp[ti][:, 0:tn], ALU.mult, [("ps", pg), ("tmp", ti)], [("H", f)])
            HK = [("H", f) for f in range(FC)]
            for j in range(KC):
                grp = []
                for k0 in range(0, FC, KG):
                    nk = min(KG, FC - k0)
                    grp.append((k0, nk, load_w(w2n, k0, nk, j)))
                si = j % 3
                if first:
                    dma("act", stg[si][:], hres[:, j, :], [("hres", j)], [("stg", si)])
                    ts(stg[si][:], stg[si][:], ALPHA, None, ALU.mult, None, [("stg", si)], [("stg", si)])
                else:
                    dma("act", stg[si][:], zres[:, j, :], [("zres", j)], [("stg", si)])
                for (t0, tn) in TB:
                    pa = next_ps()
                    for (k0, nk, wi) in grp:
                        for k in range(nk):
                            mm(pa, (slice(0, 128), slice(0, tn)), wt[wi][:, k, 0:128], HH[:, k0 + k, t0:t0 + tn],
                               k0 + k == 0, k0 + k == FC - 1, [("wt", wi)] + HK)
                    if gated:
                        ti = next_tmp()
                        tt(tmp[ti][:, 0:tn], ps[pa][:, 0:tn], Gb[:, t0:t0 + tn], ALU.mult, [("ps", pa), "Gb"], [("tmp", ti)])
                        tt(stg[si][:, t0:t0 + tn], stg[si][:, t0:t0 + tn], tmp[ti][:, 0:tn], ALU.add, [("stg", si), ("tmp", ti)], [("stg", si)])
                    else:
                        tt(stg[si][:, t0:t0 + tn], stg[si][:, t0:t0 + tn], ps[pa][:, 0:tn], ALU.add, [("stg", si), ("ps", pa)], [("stg", si)])
                dma("act", zres[:, j, :], stg[si][:], [("stg", si)], [("zres", j)])

        def router(li):
            dma("sp", rtw[:], rtr[li], [], ["rtw"])
            dma("sp", sm[0:1, 0:NE], rtb[li], [], ["rtbias"])
            for (t0, nt) in TILES:
                pl = next_ps()
                for g0 in range(0, KC, 4):
                    gi = next_tmp()
                    ng = min(4, KC - g0)
                    hv = tmp[gi][:, 0:ng * 128].rearrange("p (k n) -> p k n", n=128)
                    dma("sp", hv[:, :, 0:nt], hres[:, g0:g0 + ng, t0:t0 + nt], [("hres", j) for j in range(g0, g0 + ng)], [("tmp", gi)])
                    for k in range(ng):
                        mm(pl, (slice(0, nt), slice(0, NE)), hv[:, k, 0:nt], rtw[:, g0 + k, :], g0 + k == 0, False, [("tmp", gi), "rtw"])
                mm(pl, (slice(0, nt), slice(0, NE)), cst[0:1, ONES, 0:nt], sm[0:1, 0:NE], False, True, ["cst", "rtbias"])
                L0 = lg[0:nt, 0:NE]
                L1 = lg[0:nt, 8:8 + NE]
                K1 = lg[0:nt, 16:16 + NE]
                K2 = lg[0:nt, 24:24 + NE]
                GG = lg[0:nt, 32:32 + NE]
                m1, m2, dd, g1, g2 = (lg[0:nt, 40 + i:41 + i] for i in range(5))
                LK = ["lg"]
                cp(L0, ps[pl][0:nt, 0:NE], [("ps", pl)], LK)
                S.op("dve", lambda e, o=m1, i=L0: e.reduce_max(out=o, in_=i, axis=mybir.AxisListType.X), LK, LK)
                ts(K1, L0, m1, None, ALU.is_equal, None, LK, LK)
                stt(L1, K1, -1e30, L0, ALU.mult, ALU.add, LK, LK)
                S.op("dve", lambda e, o=m2, i=L1: e.reduce_max(out=o, in_=i, axis=mybir.AxisListType.X), LK, LK)
                ts(K2, L1, m2, None, ALU.is_equal, None, LK, LK)
                tt(dd, m2, m1, ALU.subtract, LK, LK)
                act(dd, dd, AF.Exp, LK, LK)
                ts(g1, dd, 1.0, None, ALU.add, None, LK, LK)
                recip(g1, g1, LK, LK)
                tt(g2, dd, g1, ALU.mult, LK, LK)
                ts(GG, K1, g1, None, ALU.mult, None, LK, LK)
                stt(GG, K2, g2, GG, ALU.mult, ALU.add, LK, LK)
                pt = next_ps()
                mm(pt, (slice(0, NE), slice(0, nt)), GG, cst[0:nt, IDN, 0:nt], True, True, LK + ["cst"])
                cp(GT[0:NE, t0:t0 + nt], ps[pt][0:NE, 0:nt], [("ps", pt)], ["GT"], eng="act")

        def gate_bcast(e):
            ts(Gsel[0:NE, :], GT[0:NE, :], cst[0:NE, IDN, e:e + 1], None, ALU.mult, None, ["GT", "cst"], ["Gsel"])
            for (t0, tn) in TB:
                pi = next_ps()
                mm(pi, (slice(0, 128), slice(0, tn)), cst[0:NE, ONES, :], Gsel[0:NE, t0:t0 + tn], True, True, ["Gsel", "cst"])
                cp(Gb[:, t0:t0 + tn], ps[pi][:, 0:tn], [("ps", pi)], ["Gb"], eng="act")

        def gla_head(l, h, wn):
            RAK = [("RA", h * VH + v) for v in range(VH)]
            QK = [("RM", h * KH + k) for k in range(KH)]
            KK = [("RM", NH * KH + k) for k in range(KH)]
            dma("sp", auh[0:R1, 0:DKH], alup[l, h], [], ["auh"])
            for kh in range(KH):
                proj(wn, cQ + h * KH + kh, 128, TB,
                     lambda t0, tn, pi, kh=kh: cp(RM[:, h * KH + kh, t0:t0 + tn], ps[pi][:, 0:tn], [("ps", pi)], [("RM", h * KH + kh)], eng="act"))
                proj(wn, cK + h * KH + kh, 128, TB,
                     lambda t0, tn, pi, kh=kh: cp(RM[:, NH * KH + kh, t0:t0 + tn], ps[pi][:, 0:tn], [("ps", pi)], [("RM", NH * KH + kh)]))
            for vv in range(VH):
                proj(wn, cV + h * VH + vv, 128, TB,
                     lambda t0, tn, pi, vv=vv: cp(RA[:, h * VH + vv, t0:t0 + tn], ps[pi][:, 0:tn], [("ps", pi)], [("RA", h * VH + vv)],
                                                  eng="act" if vv % 2 else "dve"))
            memset(Sst[:, :, :], 0.0, ["Sst"])
            memset(Sb[:, :, :], 0.0, ["Sb"])
            memset(erun, 1.0, ["erun"])
            for (t0, nt) in TILES:
                pre = t0 < NM
                chunks = [(0, nt)] if nt <= 64 else [(0, 64), (64, 64)]
                pa_ = next_ps()
                mm(pa_, (slice(0, nt), slice(0, DKH)), alT[0:R1, t0:t0 + nt], auh[0:R1, 0:DKH], True, True, ["alT", "auh"])
                act(e1[0:nt, :], ps[pa_][0:nt, 0:DKH], AF.Exp, [("ps", pa_)], ["e1"], scale=-1.0)
                act(ltok[0:nt, :], e1[0:nt, :], AF.Ln, ["e1"], ["ltok"], bias=1.0)
                pe_ = next_ps()
                mm(pe_, (slice(0, nt), slice(0, DKH)), cst[0:nt, UPP, 0:nt], ltok[0:nt, :], True, True, ["ltok", "cst"])
                act(ef[0:nt, :], ps[pe_][0:nt, 0:DKH], AF.Exp, [("ps", pe_)], ["ef"])
                pb_ = next_ps()
                for kh in range(KH):
                    mm(pb_, (slice(0, 128), slice(kh * 128, kh * 128 + nt)), ltok[0:nt, kh * 128:(kh + 1) * 128], cst[0:nt, TRI, 0:nt],
                       True, True, ["ltok", "cst"])
                pbv = ps[pb_][:, 0:KH * 128].rearrange("p (k n) -> p k n", n=128)[:, :, 0:nt]
                act(eB[:, :, 0:nt], pbv, AF.Exp, [("ps", pb_)], ["eB"])
                act(enB[:, :, 0:nt], pbv, AF.Exp, [("ps", pb_)], ["enB"], scale=-1.0)
                pk_ = next_ps()
                for kh in range(KH):
                    mm(pk_, (slice(0, nt), slice(kh * 128, (kh + 1) * 128)), RM[:, NH * KH + kh, t0:t0 + nt], idb[:, :], True, True, KK + ["idb"])
                tt(kte[0:nt, :], ps[pk_][0:nt, 0:DKH], ef[0:nt, :], ALU.mult, [("ps", pk_), "ef"], ["kte"])
                if pre:
                    ts(kte[0:nt, :], kte[0:nt, :], fl[0:nt, 0:1], None, ALU.mult, None, ["kte", "fl"], ["kte"])
                pv_ = next_ps()
                for vv in range(VH):
                    mm(pv_, (slice(0, nt), slice(vv * 128, (vv + 1) * 128)), RA[:, h * VH + vv, t0:t0 + nt], idb[:, :], True, True, RAK + ["idb"])
                cp(vt[0:nt, :], ps[pv_][0:nt, 0:DVH], [("ps", pv_)], ["vt"], eng="act")
                stt(qd[:, :, 0:nt], RM[:, h * KH:(h + 1) * KH, t0:t0 + nt], QSCALE, eB[:, :, 0:nt], ALU.mult, ALU.mult, QK + ["eB"], ["qd"])
                tt(kd[:, :, 0:nt], RM[:, NH * KH:NH * KH + KH, t0:t0 + nt], enB[:, :, 0:nt], ALU.mult, KK + ["enB"], ["kd"])
                ps_ = next_ps()
                for kh in range(KH):
                    mm(ps_, (slice(0, nt), slice(0, nt)), kd[:, kh, 0:nt], qd[:, kh, 0:nt], kh == 0, kh == KH - 1, ["kd", "qd"])
                tt(sT[0:nt, 0:nt], ps[ps_][0:nt, 0:nt], cst[0:nt, CM, 0:nt], ALU.mult, [("ps", ps_), "cst"], ["sT"])
                for (c0, cn) in chunks:
                    cs = slice(c0, c0 + cn)
                    po = next_ps()
                    for vv in range(VH):
                        osl = (slice(0, 128), slice(vv * 64, vv * 64 + cn))
                        mm(po, osl, vt[cs, vv * 128:(vv + 1) * 128], sT[cs, cs], True, False, ["vt", "sT"])
                        for kh in range(KH):
                            mm(po, osl, Sb[:, kh, vv * 128:(vv + 1) * 128], qd[:, kh, cs], False, kh == KH - 1, ["Sb", "qd"])
                    for kh in range(KH):
                        ts(RM[:, h * KH + kh, t0 + c0:t0 + c0 + cn], qd[:, kh, cs], erun[:, kh:kh + 1], None, ALU.mult, None,
                           ["qd", "erun"], [("RM", h * KH + kh)])
                    pov = ps[po][:, 0:VH * 64].rearrange("p (v n) -> p v n", n=64)[:, :, 0:cn]
                    cp(RA[:, h * VH:(h + 1) * VH, t0 + c0:t0 + c0 + cn], pov, [("ps", po)], RAK, eng="act")
                    for kh in range(KH):
                        pS = next_ps()
                        mm(pS, (slice(0, 128), slice(0, DVH)), kte[cs, kh * 128:(kh + 1) * 128], vt[cs, :], True, True, ["kte", "vt"])
                        dec = eB[:, kh, c0 + cn - 1:c0 + cn]
                        stt(Sst[:, kh, :], Sst[:, kh, :], dec, ps[pS][:, 0:DVH], ALU.mult, ALU.add, ["Sst", "eB", ("ps", pS)], ["Sst"])
                        cp(Sb[:, kh, :], Sst[:, kh, :], ["Sst"], ["Sb"], eng="act")
                        if not pre:
                            tt(erun[:, kh:kh + 1], erun[:, kh:kh + 1], dec, ALU.mult, ["erun", "eB"], ["erun"])
            dma("sp", xsrc[h][:, 0:KH * DVH], Sstf, ["Sst"], [("xsrc", h)])
            cp(dsg[:, h * KH:(h + 1) * KH], erun, ["erun"], ["dsg"])
            S.op("pool", lambda e, a=xsrc[h], b=xdst[h]: e.collective_compute(
                "AllGather", ALU.bypass, replica_groups=GROUPS, ins=[a], outs=[b]), [("xsrc", h)], [("xdst", h)], kind="cc")

        def gla_finish_head(l, h, wn):
            RAK = [("RA", h * VH + v) for v in range(VH)]
            memset(Sst[:, :, :], 0.0, ["Sst"])
            for j in range(3):
                dma("sp", Sx[:, :], xdst[h][j * 128:(j + 1) * 128, 0:KH * DVH], [("xdst", h)], ["Sx"])
                ts(acoef, dall[:, j, h * KH:(h + 1) * KH], -1.0, fl[:, 1 + j:2 + j], ALU.add, ALU.mult, ["dall", "fl"], ["acoef"])
                ts(acoef, acoef, 1.0, None, ALU.add, None, ["acoef"], ["acoef"])
                ts(Sx[:, :], Sx[:, :], fl[:, 1 + j:2 + j], None, ALU.mult, None, ["Sx", "fl"], ["Sx"])
                for kh in range(KH):
                    stt(Sst[:, kh, :], Sst[:, kh, :], acoef[:, kh:kh + 1], Sx[:, kh * DVH:(kh + 1) * DVH], ALU.mult, ALU.add,
                        ["Sst", "acoef", "Sx"], ["Sst"])
            cp(Sb[:, :, :], Sst[:, :, :], ["Sst"], ["Sb"], eng="act")
            for vv in range(VH):
                for (t0, tn) in TB:
                    pi = next_ps()
                    for kh in range(KH):
                        mm(pi, (slice(0, 128), slice(0, tn)), Sb[:, kh, vv * 128:(vv + 1) * 128], RM[:, h * KH + kh, t0:t0 + tn],
                           kh == 0, kh == KH - 1, ["Sb", ("RM", h * KH + kh)])
                    tt(RA[:, h * VH + vv, t0:t0 + tn], RA[:, h * VH + vv, t0:t0 + tn], ps[pi][:, 0:tn], ALU.add,
                       [("ps", pi), ("RA", h * VH + vv)], [("RA", h * VH + vv)])
            for bi, (t0, tn) in enumerate(TB):
                pr = next_ps()
                for vv in range(VH):
                    ti = next_tmp()
                    act(tmp[ti][:, 0:tn], RA[:, h * VH + vv, t0:t0 + tn], AF.Square, [("RA", h * VH + vv)], [("tmp", ti)])
                    mm(pr, (slice(0, 128), slice(0, tn)), cst[:, ONES, :], tmp[ti][:, 0:tn], vv == 0, vv == VH - 1, [("tmp", ti), "cst"])
                ta, tb_ = next_tmp(), next_tmp()
                ts(tmp[ta][:, 0:tn], ps[pr][:, 0:tn], 1.0 / DVH, EPS, ALU.mult, ALU.add, [("ps", pr)], [("tmp", ta)])
                act(tmp[tb_][:, 0:tn], tmp[ta][:, 0:tn], AF.Sqrt, [("tmp", ta)], [("tmp", tb_)])
                recip(stat[:, 1, t0:t0 + tn], tmp[tb_][:, 0:tn], [("tmp", tb_)], [("stat", 1, bi)])
            for vv in range(VH):
                jj = h * VH + vv

                def epi_r(t0, tn, pi, jj=jj):
                    ti = next_tmp()
                    act(tmp[ti][:, 0:tn], ps[pi][:, 0:tn], AF.Silu, [("ps", pi)], [("tmp", ti)])
                    tt(tmp[ti][:, 0:tn], tmp[ti][:, 0:tn], stat[:, 1, t0:t0 + tn], ALU.mult, [("tmp", ti)] + SK, [("tmp", ti)])
                    stt(RA[:, jj, t0:t0 + tn], RA[:, jj, t0:t0 + tn], gn[:, l, jj:jj + 1], tmp[ti][:, 0:tn], ALU.mult, ALU.mult,
                        [("RA", jj), "gn", ("tmp", ti)], [("RA", jj)])
                proj(wn, cR + jj, 128, TB, epi_r)

        gather_through(lambda n: wl_of[n] == 0)
        pss, pqq = [next_ps() for _ in TB], [next_ps() for _ in TB]
        for j in range(KC):
            si = j % 3
            dma("sp", stg[si][:], xT[:, j, :], [], [("stg", si)])
            ln_accum(pss, pqq, j, stg[si], ("stg", si))
            dma("sp", zres[:, j, :], stg[si][:], [("stg", si)], [("zres", j)])
        ln_finish(pss, pqq, D)
        ln_apply(zres, 0, 1)

        for l in range(DEPTH):
            vb = 2 + l * 9
            V_CB, V_CNG, V_CNB, V_MG, V_MB, V_FG, V_FB = [vb + i for i in range(7)]
            wn = f"w_in{l}"
            barrier()
            wt_act[0] = wt_mix
            dma("sp", cw[:], convw[l], [], ["cw"])
            if STOP != "noGLA":
                memset(alT[0:32, :], 1.0, ["alT"])
                proj(wn, cA, LOWR, TB, lambda t0, tn, pi: cp(alT[0:LOWR, t0:t0 + tn], ps[pi][0:LOWR, 0:tn], [("ps", pi)], ["alT"]))
                for h in range(NH):
                    gla_head(l, h, wn)
                dma("sp", xsrc[NH][:, 0:NH * KH], dsg, ["dsg"], [("xsrc", NH)])
                S.op("pool", lambda e, a=xsrc[NH], b=xdst[NH]: e.collective_compute(
                    "AllGather", ALU.bypass, replica_groups=GROUPS, ins=[a], outs=[b]), [("xsrc", NH)], [("xdst", NH)], kind="cc")
            memset(Ue[:, :, 0:HALO], 0.0, [("Ue", j) for j in range(KC)])
            for j in range(KC):
                wa = load_w(wn, 0, KC, cG + j)
                wgk = load_w(wn, 0, KC, cG + KC + j)
                for (t0, tn) in TB:
                    pa, pg = next_ps(), next_ps()
                    for k in range(KC):
                        mm(pa, (slice(0, 128), slice(0, tn)), wt[wa][:, k, 0:128], hb[:, k, t0:t0 + tn], k == 0, k == KC - 1, [("wt", wa)] + HBK)
                    for k in range(KC):
                        mm(pg, (slice(0, 128), slice(0, tn)), wt[wgk][:, k, 0:128], hb[:, k, t0:t0 + tn], k == 0, k == KC - 1, [("wt", wgk)] + HBK)
                    ti = next_tmp()
                    act(tmp[ti][:, 0:tn], ps[pg][:, 0:tn], AF.Sigmoid, [("ps", pg)], [("tmp", ti)])
                    u0 = upos(t0)
                    tt(Ue[:, j, u0:u0 + tn], ps[pa][:, 0:tn], tmp[ti][:, 0:tn], ALU.mult, [("ps", pa), ("tmp", ti)], [("Ue", j)])
            for j in range(KC):
                cp(hal[:, j * HALO:(j + 1) * HALO], Ue[:, j, UW - HALO:UW], [("Ue", j)], ["hal"])
            dma("sp", usrc[:, 0:KC * HALO], hal[:], ["hal"], ["usrc"])
            S.op("pool", lambda e: e.collective_compute("AllGather", ALU.bypass, replica_groups=GROUPS,
                                                        ins=[usrc.ap()], outs=[udst.ap()]), ["usrc"], ["udst"], kind="cc")
            gather_after_exchange(l)
            if STOP != "noGLA":
                for j in range(3):
                    dma("sp", dall[:, j, 0:NH * KH], xdst[NH][j * 128:(j + 1) * 128, 0:NH * KH], [("xdst", NH)], ["dall"])
                for h in range(NH):
                    gla_finish_head(l, h, wn)
            barrier()
            ts(hal[:], hal[:], 0.0, None, ALU.mult, None, ["hal", "usrc"], ["hal"])
            for jj in range(3):
                dma("sp", halg[:, :], udst[jj * 128:(jj + 1) * 128, 0:KC * HALO], ["udst"], ["halg"])
                stt(hal[:], halg[:, :], fl[:, 4 + jj:5 + jj], hal[:], ALU.mult, ALU.add, ["halg", "fl", "hal"], ["hal"])
            W = NM + HALO + TL
            pss, pqq = [next_ps() for _ in TB], [next_ps() for _ in TB]
            for j in range(KC):
                hj = hal[:, j * HALO:(j + 1) * HALO]
                stt(hj[:, HALO - NM:HALO], Ue[:, j, HALO:HALO + NM], fl[:, 0:1], hj[:, HALO - NM:HALO], ALU.mult, ALU.add,
                    [("Ue", j), "fl", "hal"], ["hal"])
                cp(Ue[:, j, HALO + NM:HALO + NM + HALO], hj, ["hal"], [("Ue", j)])
                ts(cacc[:, :], Ue[:, j, 0:W], cw[:, j, 0:1], vec[:, V_CB, j:j + 1], ALU.mult, ALU.add, [("Ue", j), "cw", "vec"], ["cacc"])
                for k in range(1, CW):
                    stt(cacc[:, :], Ue[:, j, k:k + W], cw[:, j, k:k + 1], cacc[:, :], ALU.mult, ALU.add, [("Ue", j), "cw", "cacc"], ["cacc"])
                si = j % 3
                cp(stg[si][:, 0:NM], cacc[:, 0:NM], ["cacc"], [("stg", si)])
                cp(stg[si][:, NM:T], cacc[:, NM + HALO:W], ["cacc"], [("stg", si)], eng="act")
                ln_accum(pss, pqq, j, stg[si], ("stg", si))
                cp(Ue[:, j, HALO:HALO + NM], stg[si][:, 0:NM], [("stg", si)], [("Ue", j)])
                cp(Ue[:, j, HALO + NM + HALO:UW], stg[si][:, NM:T], [("stg", si)], [("Ue", j)], eng="act")
            ln_finish(pss, pqq, D)
            for j in range(KC):
                si = j % 3
                cp(stg[si][:, 0:NM], Ue[:, j, HALO:HALO + NM], [("Ue", j)], [("stg", si)])
                cp(stg[si][:, NM:T], Ue[:, j, HALO + NM + HALO:UW], [("Ue", j)], [("stg", si)])
                tt(stg[si][:], stg[si][:], stat[:, 0, :], ALU.subtract, [("stg", si)] + SK, [("stg", si)])
                tt(stg[si][:], stg[si][:], stat[:, 1, :], ALU.mult, [("stg", si)] + SK, [("stg", si)])
                ts(stg[si][:], stg[si][:], vec[:, V_CNG, j:j + 1], vec[:, V_CNB, j:j + 1], ALU.mult, ALU.add, [("stg", si), "vec"], [("stg", si)])
                act(Ue[:, j, HALO:HALO + NM], stg[si][:, 0:NM], AF.Silu, [("stg", si)], [("Ue", j)])
                act(Ue[:, j, HALO + NM + HALO:UW], stg[si][:, NM:T], AF.Silu, [("stg", si)], [("Ue", j)])
            UK = [("Ue", j) for j in range(KC)]
            for j in range(KC):
                wa = load_w(f"w_convo{l}", 0, KC, j)
                wgk = load_w(wn, 0, KC, cGB + j)
                for (t0, tn) in TB:
                    pa, pg = next_ps(), next_ps()
                    u0 = upos(t0)
                    for k in range(KC):
                        mm(pa, (slice(0, 128), slice(0, tn)), wt[wa][:, k, 0:128], Ue[:, k, u0:u0 + tn], k == 0, k == KC - 1, [("wt", wa)] + UK)
                    for k in range(KC):
                        mm(pg, (slice(0, 128), slice(0, tn)), wt[wgk][:, k, 0:128], hb[:, k, t0:t0 + tn], k == 0, k == KC - 1, [("wt", wgk)] + HBK)
                    ti = next_tmp()
                    act(tmp[ti][:, 0:tn], ps[pg][:, 0:tn], AF.Sigmoid, [("ps", pg)], [("tmp", ti)])
                    tt(RM[:, j, t0:t0 + tn], ps[pa][:, 0:tn], tmp[ti][:, 0:tn], ALU.mult, [("ps", pa), ("tmp", ti)], [("RM", j)])
            if STOP != "noGLA":
                RAALL = [("RA", j) for j in range(KC)]
                for j in range(KC):
                    wa = load_w(f"w_glao{l}", 0, KC, j)
                    wgk = load_w(wn, 0, KC, cGA + j)
                    for (t0, tn) in TB:
                        pa, pg = next_ps(), next_ps()
                        for k in range(KC):
                            mm(pa, (slice(0, 128), slice(0, tn)), wt[wa][:, k, 0:128], RA[:, k, t0:t0 + tn], k == 0, k == KC - 1, [("wt", wa)] + RAALL)
                        for k in range(KC):
                            mm(pg, (slice(0, 128), slice(0, tn)), wt[wgk][:, k, 0:128], hb[:, k, t0:t0 + tn], k == 0, k == KC - 1, [("wt", wgk)] + HBK)
                        ti = next_tmp()
                        act(tmp[ti][:, 0:tn], ps[pg][:, 0:tn], AF.Sigmoid, [("ps", pg)], [("tmp", ti)])
                        tt(tmp[ti][:, 0:tn], ps[pa][:, 0:tn], tmp[ti][:, 0:tn], ALU.mult, [("ps", pa), ("tmp", ti)], [("tmp", ti)])
                        tt(RM[:, j, t0:t0 + tn], RM[:, j, t0:t0 + tn], tmp[ti][:, 0:tn], ALU.add, [("RM", j), ("tmp", ti)], [("RM", j)])
            MK = [("RM", j) for j in range(KC)]
            for j in range(KC):
                wa = load_w(f"w_out{l}", 0, KC, j)
                si = j % 3
                dma("sp", stg[si][:], hres[:, j, :], [("hres", j)], [("stg", si)])
                for (t0, tn) in TB:
                    pa = next_ps()
                    for k in range(KC):
                        mm(pa, (slice(0, 128), slice(0, tn)), wt[wa][:, k, 0:128], RM[:, k, t0:t0 + tn], k == 0, k == KC - 1, [("wt", wa)] + MK)
                    stt(stg[si][:, t0:t0 + tn], stg[si][:, t0:t0 + tn], ALPHA, ps[pa][:, 0:tn], ALU.mult, ALU.add, [("stg", si), ("ps", pa)], [("stg", si)])
                dma("sp", zres[:, j, :], stg[si][:], [("stg", si)], [("zres", j)])
            ln_from_dram(zres)
            ln_apply(zres, V_MG, V_MB)
            barrier()
            wt_act[0] = wt_ffn
            last_out = out if l == DEPTH - 1 else None
            if l % 2 == 0:
                ffn(f"ffn1_{l}", f"ffn3_{l}", f"ffn2_{l}", True, False)
            else:
                router(l // 2)
                for e in range(NE):
                    gate_bcast(e)
                    ffn(f"moe1_{l}_{e}", f"moe3_{l}_{e}", f"moe2_{l}_{e}", e == 0, True)
            ln_from_dram(zres)
            ln_apply(zres, V_FG, V_FB, write_out=last_out)
        S.emit(nc, sems, dma_sems, cc_sem)
    return nc


def make_inputs(cfg, inp):
    D, SEQ, DEPTH, DFF, NE, NM, LOWR, CW = (cfg[k] for k in ("D", "SEQ", "DEPTH", "DFF", "NE", "NMETA", "LOWR", "CW"))
    KC = D // 128
    TL = SEQ // 4
    T = NM + TL
    f32 = lambda a: np.ascontiguousarray(np.asarray(a), dtype=np.float32)
    fm = lambda v: f32(v).reshape(KC, 128).T
    x = f32(inp["x"])
    meta = f32(inp["meta_tokens"])
    nv = 2 + DEPTH * 9
    vecs = np.zeros((128, nv, KC), np.float32)
    vecs[:, 0] = fm(inp["ln_in_g"]); vecs[:, 1] = fm(inp["ln_in_b"])
    for l in range(DEPTH):
        b = 2 + l * 9
        for i, k in enumerate(("conv_b", "conv_norm_g", "conv_norm_b", "ln_mix_g", "ln_mix_b", "ln_ffn_g", "ln_ffn_b")):
            vecs[:, b + i] = fm(f32(inp[k])[l])
    convw = np.ascontiguousarray(f32(inp["conv_w"]).reshape(DEPTH, CW, KC, 128).transpose(0, 3, 2, 1))
    gnorm = np.ascontiguousarray(f32(inp["gla_norm_g"]).reshape(DEPTH, KC, 128).transpose(2, 0, 1))
    NH = 4
    DK = D // 2
    DKH = DK // NH
    al = np.concatenate([f32(inp["w_alpha_up"]), f32(inp["b_alpha"])[:, None, :]], 1)
    alup = np.ascontiguousarray(al.reshape(DEPTH, LOWR + 1, NH, DKH).transpose(0, 2, 1, 3))
    NMOE = max(1, DEPTH // 2)
    rtr = np.zeros((NMOE, 128, KC, NE), np.float32)
    rtb = np.zeros((NMOE, 1, NE), np.float32)
    if DEPTH // 2:
        rtr[:] = f32(inp["router_w"]).reshape(DEPTH // 2, KC, 128, NE).transpose(0, 2, 1, 3)
        rtb[:, 0] = f32(inp["router_b"])
    consts = np.zeros((128, 5, 128), np.float32)
    ii = np.arange(128)
    same = (ii[:, None] // 64) == (ii[None, :] // 64)
    consts[:, 0] = np.eye(128)
    consts[:, 1] = (same & (ii[:, None] <= ii[None, :])) * (-1.0 / 16.0)
    consts[:, 2] = (same & (ii[:, None] > ii[None, :])) * (-1.0 / 16.0)
    consts[:, 3] = 1.0
    consts[:, 4] = (same & (ii[:, None] <= ii[None, :])) * 1.0
    shared = dict(vecs=vecs, convw=convw, gnorm=gnorm, alup=alup, rtr=rtr, rtb=rtb, consts=consts)
    wsrc = {}
    for l in range(DEPTH):
        wsrc[f"w_in{l}"] = inp["w_in"][l]; wsrc[f"w_glao{l}"] = inp["w_gla_o"][l]
        wsrc[f"w_convo{l}"] = inp["w_conv_o"][l]; wsrc[f"w_out{l}"] = inp["w_out"][l]
        if l % 2 == 0:
            wsrc[f"ffn1_{l}"] = inp["ffn_w1"][l // 2]; wsrc[f"ffn3_{l}"] = inp["ffn_w3"][l // 2]; wsrc[f"ffn2_{l}"] = inp["ffn_w2"][l // 2]
        else:
            for e in range(NE):
                wsrc[f"moe1_{l}_{e}"] = inp["moe_w1"][l // 2][e]; wsrc[f"moe3_{l}_{e}"] = inp["moe_w3"][l // 2][e]
                wsrc[f"moe2_{l}_{e}"] = inp["moe_w2"][l // 2][e]
    DK_ = D // 2
    oA_ = 2 * DK_ + 2 * D

    def tile_major(n, w):
        w = f32(w)
        if n.startswith("w_in"):
            w = np.concatenate([w[:, :oA_], w[:, oA_ + LOWR:], w[:, oA_:oA_ + LOWR], np.zeros((w.shape[0], 128 - LOWR), np.float32)], 1)
        K_, N_ = w.shape
        return np.ascontiguousarray(w.reshape(K_ // 128, 128, N_ // 128, 128).transpose(2, 1, 0, 3))

    wsh = [dict() for _ in range(4)]
    for n, w in wsrc.items():
        wtm = tile_major(n, w)
        for r in range(4):
            wsh[r][n] = shard_weight(wtm, r)
        del wtm
    maps = []
    for core in range(8):
        b, c = core // 4, core % 4
        tok = np.concatenate([meta, x[b, c * TL:(c + 1) * TL]], 0)
        xT = np.ascontiguousarray(tok.T.reshape(KC, 128, T).transpose(1, 0, 2))
        fl = np.zeros((128, 8), np.float32)
        fl[:, 0] = 1.0 if c == 0 else 0.0
        for j in range(3):
            fl[:, 1 + j] = 1.0 if j < c else 0.0
            fl[:, 4 + j] = 1.0 if j == c - 1 else 0.0
        m = dict(shared)
        m.update(xT=xT, flags=fl)
        m.update(wsh[c])
        maps.append(m)
    return maps


def run(cfg, inp):
    nc = build(cfg)
    maps = make_inputs(cfg, inp)
    res = run_bass_kernel_spmd(nc, maps, core_ids=list(range(8)))
    D, SEQ = cfg["D"], cfg["SEQ"]
    TL = SEQ // 4
    B = 2
    o = np.zeros((B, SEQ, D), np.float32)
    for core in range(8):
        b, c = core // 4, core % 4
        y = res.results[core]["out"]
        o[b, c * TL:(c + 1) * TL] = y.transpose(2, 1, 0).reshape(TL, D)
    return o


def kernel(**inputs):
    return run(dict(CFG_FULL), inputs)
```

```python
import numpy as np
import concourse.bass as bass
import concourse.mybir as mybir
from concourse.bass_utils import run_bass_kernel_spmd

F32 = mybir.dt.float32
BF16 = mybir.dt.bfloat16
AF = mybir.ActivationFunctionType
ALU = mybir.AluOpType

CFG_FULL = dict(D=2048, SEQ=4096, DEPTH=4, DFF=5632, NE=8, NMETA=16, LOWR=16, CW=31)
PR, PC = 256, 2048
PIECE = PR * PC
GROUPS = [[0, 1, 2, 3], [4, 5, 6, 7]]
GQOS = {"dma_qos": "P2"}


class Op:
    __slots__ = ("eng", "fn", "deps", "kind", "inc", "sem", "val", "idx")


class Sched:
    def __init__(self):
        self.ops = []
        self.lastw = {}
        self.readers = {}
        self.last_barrier = None
        self.last_eng = {}
        self.recent_sp = []

    def op(self, eng, fn, reads=(), writes=(), kind="c"):
        o = Op()
        o.eng, o.fn, o.kind, o.inc, o.idx = eng, fn, kind, False, len(self.ops)
        deps = set()
        for b in reads:
            w = self.lastw.get(b)
            if w is not None:
                deps.add(w)
        for b in writes:
            w = self.lastw.get(b)
            if w is not None:
                deps.add(w)
            for r in self.readers.get(b, ()):
                deps.add(r)
        deps.discard(o.idx)
        if self.last_barrier is not None and eng != "pool":
            deps.add(self.last_barrier)
        o.deps = deps
        if kind == "dma" and eng in ("sp", "act"):
            self.recent_sp = (self.recent_sp + [o.idx])[-16:]
        elif kind == "c":
            self.last_eng[eng] = o.idx
        for b in reads:
            lst = self.readers.setdefault(b, [])
            if kind == "c":
                lst[:] = [r for r in lst if not (self.ops[r].eng == eng and self.ops[r].kind == "c")]
            lst.append(o.idx)
        for b in writes:
            self.lastw[b] = o.idx
            self.readers[b] = []
        self.ops.append(o)
        return o

    def barrier(self, fn):
        o = self.op("dve", fn, [], [])
        o.deps |= set(self.last_eng.values()) | set(self.recent_sp)
        o.deps.discard(o.idx)
        self.last_barrier = o.idx
        return o

    def emit(self, nc, sems, dma_sems, cc_sem):
        ops = self.ops
        for o in ops:
            for d in o.deps:
                p = ops[d]
                if p.eng == "pe" and o.eng == "pe" and p.kind == "c":
                    continue
                p.inc = True
        cnt = {e: 0 for e in sems}
        dcnt = {}
        rr = {"sp": 0, "pool": 0, "act": 0}
        ccn = 0
        prev_on_sem = {}
        for o in ops:
            if o.kind == "dma":
                lst = dma_sems[o.eng]
                s = lst[rr[o.eng] % len(lst)]
                rr[o.eng] += 1
                pv = prev_on_sem.get(id(s))
                if pv is not None:
                    o.deps.add(pv)
                prev_on_sem[id(s)] = o.idx
                dcnt[id(s)] = dcnt.get(id(s), 0) + 16
                o.sem, o.val, o.inc = s, dcnt[id(s)], True
            elif o.kind == "cc":
                ccn += 1
                o.sem, o.val, o.inc = cc_sem, ccn, True
            elif o.inc:
                cnt[o.eng] += 1
                o.sem, o.val = sems[o.eng], cnt[o.eng]
        per = {"pe": [], "act": [], "dve": [], "pool": [], "sp": []}
        waited = {e: {} for e in per}
        for o in ops:
            ws = {}
            for d in o.deps:
                p = ops[d]
                if not p.inc:
                    continue
                if p.eng == "pe" and o.eng == "pe" and p.kind == "c":
                    continue
                k = id(p.sem)
                if waited[o.eng].get(k, 0) >= p.val:
                    continue
                if k not in ws or ws[k][1] < p.val:
                    ws[k] = (p.sem, p.val)
            for k, (s, v) in ws.items():
                waited[o.eng][k] = v
            per[o.eng].append((list(ws.values()), o))
        engs = {"pe": "tensor", "act": "scalar", "dve": "vector", "pool": "gpsimd", "sp": "sync"}
        with nc.Block() as block:
            for en, lst in per.items():
                def body(e, lst=lst):
                    for ws, o in lst:
                        for s, v in ws:
                            e.wait_ge(s, v)
                        ins = o.fn(e)
                        if o.inc:
                            if o.kind == "dma":
                                ins.then_inc(o.sem, 16)
                            elif o.kind == "cc":
                                ins.then_inc(o.sem)
                            else:
                                ins.then_inc(o.sem, 1)
                    if en in ("sp", "pool", "act"):
                        for s in dma_sems[en]:
                            v = dcnt.get(id(s), 0)
                            if v:
                                e.wait_ge(s, v)
                getattr(block, engs[en])(body)


def n_pieces(numel):
    return -(-numel // (4 * PIECE))


def shard_weight(w, r):
    flat = np.ascontiguousarray(w, dtype=np.float32).reshape(-1)
    npc = n_pieces(flat.size)
    pad = npc * 4 * PIECE - flat.size
    if pad:
        flat = np.concatenate([flat, np.zeros(pad, np.float32)])
    return np.ascontiguousarray(flat.reshape(npc, 4, PR, PC)[:, r])


def build(cfg):
    D, SEQ, DEPTH, DFF, NE = cfg["D"], cfg["SEQ"], cfg["DEPTH"], cfg["DFF"], cfg["NE"]
    NM, LOWR, CW = cfg["NMETA"], cfg["LOWR"], cfg["CW"]
    STOP = cfg.get("STOP")
    KC = D // 128
    DK = D // 2
    NH = 4
    DKH = DK // NH
    DVH = D // NH
    KH = DKH // 128
    VH = DVH // 128
    FC = DFF // 128
    TL = SEQ // 4
    T = NM + TL
    DIN = DK + DK + D + D + LOWR + 2 * D + D + D
    oQ, oK, oV, oR, oA = 0, DK, 2 * DK, 2 * DK + D, 2 * DK + 2 * D
    oG = oA + LOWR
    oGA = oG + 2 * D
    oGB = oGA + D
    cQ, cK, cV = 0, DK // 128, 2 * DK // 128
    cR = cV + KC
    cG = cR + KC
    cGA = cG + 2 * KC
    cGB = cGA + KC
    cA = cGB + KC
    DINP = (cA + 1) * 128
    ALPHA = (2.0 * DEPTH) ** 0.25
    QSCALE = DKH ** -0.5
    EPS = 1e-5
    HALO = CW - 1
    UW = HALO + NM + HALO + TL
    R1 = LOWR + 1
    NMOE = max(1, DEPTH // 2)
    TB = [(0, NM)] + [(NM + i, min(512, TL - i)) for i in range(0, TL, 512)]
    NB = len(TB)
    TILES = [(0, NM)] + [(NM + i, 128) for i in range(0, TL, 128)]

    def upos(t0):
        return HALO + t0 if t0 < NM else HALO + NM + HALO + (t0 - NM)

    nc = bass.Bass("TRN2", target_bir_lowering=False, num_devices=8)
    S = Sched()

    def dt_in(name, shape):
        return nc.dram_tensor(name, list(shape), F32, kind="ExternalInput")

    xT = dt_in("xT", [128, KC, T])
    vecs = dt_in("vecs", [128, 2 + DEPTH * 9, KC])
    convw = dt_in("convw", [DEPTH, 128, KC, CW])
    gnorm = dt_in("gnorm", [128, DEPTH, KC])
    alup = dt_in("alup", [DEPTH, NH, R1, DKH])
    rtr = dt_in("rtr", [NMOE, 128, KC, NE])
    rtb = dt_in("rtb", [NMOE, 1, NE])
    consts = dt_in("consts", [128, 5, 128])
    flags = dt_in("flags", [128, 8])
    out = nc.dram_tensor("out", [128, KC, TL], F32, kind="ExternalOutput")

    wspec = []
    for l in range(DEPTH):
        wspec += [(f"w_in{l}", D, DINP, l), (f"w_glao{l}", D, D, l), (f"w_convo{l}", D, D, l), (f"w_out{l}", D, D, l)]
        if l % 2 == 0:
            wspec += [(f"ffn1_{l}", D, DFF, l), (f"ffn3_{l}", D, DFF, l), (f"ffn2_{l}", DFF, D, l)]
        else:
            for e in range(NE):
                wspec += [(f"moe1_{l}_{e}", D, DFF, l), (f"moe3_{l}_{e}", D, DFF, l), (f"moe2_{l}_{e}", DFF, D, l)]
    win, wbn, wg, wview = {}, {}, {}, {}
    for name, K, N, _ in wspec:
        npc = n_pieces(K * N)
        win[name] = dt_in(name, [npc, PR, PC])
        wbn[name] = nc.dram_tensor(name + "_b", [npc, PR, PC], BF16)
        wg[name] = nc.dram_tensor(name + "_g", [npc, 4 * PR, PC], BF16)
        wview[name] = wg[name].ap().rearrange("a b c -> (a b c)")[0:K * N].rearrange("(j p k c) -> j p k c", p=128, k=K // 128, c=128)

    hres = nc.dram_tensor("hres", [128, KC, T], F32)
    zres = nc.dram_tensor("zres", [128, KC, T], F32)
    xsrc = nc.dram_tensor("xsrc", [NH + 1, 128, 1024], F32)
    xdst = nc.dram_tensor("xdst", [NH + 1, 512, 1024], F32)
    usrc = nc.dram_tensor("usrc", [128, 1024], F32)
    udst = nc.dram_tensor("udst", [512, 1024], F32)

    import contextlib
    es = contextlib.ExitStack()
    sb = lambda name, shape, dt=F32: es.enter_context(nc.sbuf_tensor(name, list(shape), dt))
    with es:
        hb = sb("hb", [128, KC, T], BF16)
        BIGN = KC * (2 * T + UW)
        assert FC * T <= BIGN
        big = sb("big", [128, BIGN], BF16)
        RA = big[:, 0:KC * T].rearrange("p (c t) -> p c t", t=T)
        Ue = big[:, KC * T:KC * T + KC * UW].rearrange("p (c t) -> p c t", t=UW)
        RM = big[:, KC * (T + UW):BIGN].rearrange("p (c t) -> p c t", t=T)
        HH = big[:, 0:FC * T].rearrange("p (c t) -> p c t", t=T)
        stat = sb("stat", [128, 2, T])
        vec = sb("vec", [128, 2 + DEPTH * 9, KC])
        cw = sb("cw", [128, KC, CW])
        gn = sb("gn", [128, DEPTH, KC])
        cst = sb("cst", [128, 5, 128])
        idb = sb("idb", [128, 128], BF16)
        dg = sb("dg", [128, 4, 128], BF16)
        fl = sb("fl", [128, 8])
        small = sb("small", [128, 80])
        KG = max(KC, 11)
        wt = [sb(f"wt{i}", [128, KG, 128], BF16) for i in range(4)]
        stg = [sb(f"stg{i}", [128, T]) for i in range(3)]
        tmp = [sb(f"tmp{i}", [128, 512]) for i in range(4)]
        hal = sb("hal", [128, KC * HALO])
        NF = max(6 * DKH + 2 * KH * DVH, KC * HALO + NM + HALO + TL, KC * NE + 3 * T + 64)
        scrF = sb("scrF", [128, NF])
        NHH = 3 * DKH + DVH + 128 + KH * DVH
        scrH = sb("scrH", [128, NHH], BF16)
        ps = [es.enter_context(nc.psum_tensor(f"ps{i}", [128, 512], F32)) for i in range(8)]
        sems = {e: es.enter_context(nc.semaphore(f"s_{e}")) for e in ("pe", "act", "dve")}
        dma_sems = {q: [es.enter_context(nc.semaphore(f"d_{q}{i}")) for i in range(8)] for q in ("sp", "pool")}
        dma_sems["act"] = [es.enter_context(nc.semaphore(f"d_act{i}")) for i in range(4)]
        cc_sem = es.enter_context(nc.semaphore("cc"))

        o_ = 0
        auh = scrF[:, o_:o_ + DKH]; o_ += DKH
        e1 = scrF[:, o_:o_ + DKH]; o_ += DKH
        ltok = scrF[:, o_:o_ + DKH]; o_ += DKH
        ef = scrF[:, o_:o_ + DKH]; o_ += DKH
        eB = scrF[:, o_:o_ + DKH].rearrange("p (k n) -> p k n", n=128); o_ += DKH
        enB = scrF[:, o_:o_ + DKH].rearrange("p (k n) -> p k n", n=128); o_ += DKH
        Sstf = scrF[:, o_:o_ + KH * DVH]
        Sst = Sstf.rearrange("p (k n) -> p k n", n=DVH); o_ += KH * DVH
        Sx = scrF[:, o_:o_ + KH * DVH]; o_ += KH * DVH
        halg = scrF[:, 0:KC * HALO]
        cacc = scrF[:, KC * HALO:KC * HALO + NM + HALO + TL]
        rtw = scrF[:, 0:KC * NE].rearrange("p (k n) -> p k n", n=NE)
        GT = scrF[:, KC * NE:KC * NE + T]
        Gsel = scrF[:, KC * NE + T:KC * NE + 2 * T]
        Gb = scrF[:, KC * NE + 2 * T:KC * NE + 3 * T]
        lg = scrF[:, KC * NE + 3 * T:KC * NE + 3 * T + 64]
        o_ = 0
        kte = scrH[:, o_:o_ + DKH]; o_ += DKH
        vt = scrH[:, o_:o_ + DVH]; o_ += DVH
        qd = scrH[:, o_:o_ + DKH].rearrange("p (k n) -> p k n", n=128); o_ += DKH
        kd = scrH[:, o_:o_ + DKH].rearrange("p (k n) -> p k n", n=128); o_ += DKH
        sT = scrH[:, o_:o_ + 128]; o_ += 128
        Sb = scrH[:, o_:o_ + KH * DVH].rearrange("p (k n) -> p k n", n=DVH); o_ += KH * DVH
        wt_mix = list(range(len(wt)))
        for i in range(2):
            if FC * T + (i + 1) * KG * 128 <= BIGN:
                wt.append(big[:, FC * T + i * KG * 128:FC * T + (i + 1) * KG * 128].rearrange("p (k c) -> p k c", c=128))
        if NHH >= KG * 128:
            wt.append(scrH[:, 0:KG * 128].rearrange("p (k c) -> p k c", c=128))
        wt_ffn = list(range(len(wt)))
        wt_act = [wt_mix]
        alT = stat[:, 0, :]
        erun = small[:, 0:KH]
        acoef = small[:, 8:8 + KH]
        dsg = small[:, 16:16 + NH * KH]
        dall = small[:, 32:32 + 3 * 8].rearrange("p (j n) -> p j n", n=8) if NH * KH <= 8 else None
        sm = small[:, 56:64]

        psi = [0]

        def next_ps():
            psi[0] = (psi[0] + 1) % 8
            return psi[0]

        tmi = [0]

        def next_tmp():
            tmi[0] = (tmi[0] + 1) % 4
            return tmi[0]

        def dma(q, out_ap, in_ap, reads, writes):
            S.op(q, lambda e: e.dma_start(out=out_ap, in_=in_ap), reads, writes, kind="dma")

        def mmr(out_ap, pi, lhsT, rhs, start, stop, reads):
            S.op("pe", lambda e: e.matmul(out_ap, lhsT, rhs, start=start, stop=stop), reads, [("ps", pi)])

        def mm(pi, sl, lhsT, rhs, start, stop, reads):
            mmr(ps[pi][sl[0], sl[1]], pi, lhsT, rhs, start, stop, reads)

        def act(out_ap, in_ap, func, reads, writes, bias=None, scale=None):
            kw = {}
            if bias is not None:
                kw["bias"] = bias
            if scale is not None:
                kw["scale"] = scale
            S.op("act", lambda e: e.activation(out=out_ap, in_=in_ap, func=func, **kw), reads, writes)

        def tt(out_ap, a, b, op, reads, writes, eng="dve"):
            S.op(eng, lambda e: e.tensor_tensor(out=out_ap, in0=a, in1=b, op=op), reads, writes)

        def ts(out_ap, a, s1, s2, op0, op1, reads, writes, eng="dve"):
            if s2 is None:
                S.op(eng, lambda e: e.tensor_scalar(out=out_ap, in0=a, scalar1=s1, scalar2=None, op0=op0), reads, writes)
            else:
                S.op(eng, lambda e: e.tensor_scalar(out=out_ap, in0=a, scalar1=s1, scalar2=s2, op0=op0, op1=op1), reads, writes)

        def stt(out_ap, a, s, b, op0, op1, reads, writes, eng="dve"):
            S.op(eng, lambda e: e.scalar_tensor_tensor(out=out_ap, in0=a, scalar=s, in1=b, op0=op0, op1=op1), reads, writes)

        def cp(out_ap, in_ap, reads, writes, eng="dve"):
            if eng == "act":
                S.op(eng, lambda e: e.copy(out=out_ap, in_=in_ap), reads, writes)
            else:
                S.op(eng, lambda e: e.tensor_copy(out=out_ap, in_=in_ap), reads, writes)

        def memset(ap, v, writes, eng="dve"):
            S.op(eng, lambda e: e.memset(ap, v), [], writes)

        def recip(out_ap, in_ap, reads, writes):
            S.op("dve", lambda e: e.reciprocal(out=out_ap, in_=in_ap), reads, writes)

        def barrier():
            S.barrier(lambda e: e.memset(small[:, 72:73], 0.0))

        dma("sp", vec[:], vecs.ap(), [], ["vec"])
        dma("sp", gn[:], gnorm.ap(), [], ["gn"])
        dma("sp", cst[:], consts.ap(), [], ["cst"])
        dma("sp", fl[:], flags.ap(), [], ["fl"])
        cp(idb[:], cst[:, 0, :], ["cst"], ["idb"])
        IDN, TRI, UPP, ONES, CM = 0, 1, 2, 3, 4

        gq = [name for name, _, _, _ in wspec]
        gpos = [0]

        def gather_through(pred):
            last = max([i for i, n in enumerate(gq) if pred(n)], default=-1)
            while gpos[0] <= last:
                name = gq[gpos[0]]
                gpos[0] += 1
                for p in range(win[name].shape[0]):
                    dma("pool", wbn[name][p], win[name][p], [], [("wb", name, p)])
                    S.op("pool", lambda e, a=wbn[name][p], b=wg[name][p]: e.collective_compute(
                        "AllGather", ALU.bypass, replica_groups=GROUPS, ins=[a], outs=[b], **GQOS),
                        [("wb", name, p)], [("wg", name)], kind="cc")

        wl_of = {name: wl for name, _, _, wl in wspec}

        def first_part(n, lmax):
            if wl_of[n] < lmax:
                return True
            if wl_of[n] > lmax:
                return False
            return not (n.startswith("moe") and int(n.split("_")[2]) >= NE // 2)

        def gather_after_exchange(l):
            if l >= DEPTH - 2:
                gather_through(lambda n: True)
            elif (l + 1) % 2 == 1:
                gather_through(lambda n: first_part(n, l + 1))
            else:
                gather_through(lambda n: first_part(n, min(l + 3, DEPTH - 1)))

        wbi = [0]

        def load_w(name, k0, nk, j, ncol=128):
            i = wt_act[0][wbi[0] % len(wt_act[0])]
            wbi[0] += 1
            dma("sp", wt[i][:, 0:nk, 0:ncol], wview[name][j, :, k0:k0 + nk, 0:ncol], [("wg", name)], [("wt", i)])
            return i

        HBK = [("hb", j) for j in range(KC)]

        def proj(name, jc, ncol, blocks, epi, rhs=None, rkeys=None, rpos=None):
            rhs = hb if rhs is None else rhs
            rkeys = HBK if rkeys is None else rkeys
            rpos = rpos or (lambda t0: t0)
            wi = load_w(name, 0, KC, jc, ncol)
            for (t0, tn) in blocks:
                pi = next_ps()
                r0 = rpos(t0)
                for k in range(KC):
                    mm(pi, (slice(0, ncol), slice(0, tn)), wt[wi][:, k, 0:ncol], rhs[:, k, r0:r0 + tn], k == 0, k == KC - 1,
                       [("wt", wi)] + rkeys)
                epi(t0, tn, pi)

        def ln_accum(pss, pqq, j, zap, zkey):
            for bi, (t0, tn) in enumerate(TB):
                ti = next_tmp()
                act(tmp[ti][:, 0:tn], zap[:, t0:t0 + tn], AF.Square, [zkey], [("tmp", ti)])
                mm(pss[bi], (slice(0, 128), slice(0, tn)), cst[:, ONES, :], zap[:, t0:t0 + tn], j == 0, j == KC - 1, [zkey, "cst"])
                mm(pqq[bi], (slice(0, 128), slice(0, tn)), cst[:, ONES, :], tmp[ti][:, 0:tn], j == 0, j == KC - 1, [("tmp", ti), "cst"])

        def ln_finish(pss, pqq, n):
            for bi, (t0, tn) in enumerate(TB):
                sl = slice(t0, t0 + tn)
                ta, tb_ = next_tmp(), next_tmp()
                ts(stat[:, 0, sl], ps[pss[bi]][:, 0:tn], 1.0 / n, None, ALU.mult, None, [("ps", pss[bi])], [("stat", 0, bi)])
                ts(tmp[ta][:, 0:tn], ps[pqq[bi]][:, 0:tn], 1.0 / n, None, ALU.mult, None, [("ps", pqq[bi])], [("tmp", ta)])
                tt(tmp[tb_][:, 0:tn], stat[:, 0, sl], stat[:, 0, sl], ALU.mult, [("stat", 0, bi)], [("tmp", tb_)])
                tt(tmp[ta][:, 0:tn], tmp[ta][:, 0:tn], tmp[tb_][:, 0:tn], ALU.subtract, [("tmp", ta), ("tmp", tb_)], [("tmp", ta)])
                ts(tmp[ta][:, 0:tn], tmp[ta][:, 0:tn], EPS, None, ALU.add, None, [("tmp", ta)], [("tmp", ta)])
                act(tmp[tb_][:, 0:tn], tmp[ta][:, 0:tn], AF.Sqrt, [("tmp", ta)], [("tmp", tb_)])
                recip(stat[:, 1, sl], tmp[tb_][:, 0:tn], [("tmp", tb_)], [("stat", 1, bi)])

        SK = [("stat", r, b) for r in (0, 1) for b in range(NB)]

        def ln_from_dram(src):
            pss, pqq = [next_ps() for _ in TB], [next_ps() for _ in TB]
            for j in range(KC):
                si = j % 3
                dma("sp", stg[si][:], src[:, j, :], [("zres", j)], [("stg", si)])
                ln_accum(pss, pqq, j, stg[si], ("stg", si))
            ln_finish(pss, pqq, D)

        def ln_apply(src_dram, gi, bi_, write_out=None):
            for j in range(KC):
                si = j % 3
                dma("sp", stg[si][:], src_dram[:, j, :], [("zres", j)], [("stg", si)])
                tt(stg[si][:], stg[si][:], stat[:, 0, :], ALU.subtract, [("stg", si)] + SK, [("stg", si)])
                tt(stg[si][:], stg[si][:], stat[:, 1, :], ALU.mult, [("stg", si)] + SK, [("stg", si)])
                ts(stg[si][:], stg[si][:], vec[:, gi, j:j + 1], vec[:, bi_, j:j + 1], ALU.mult, ALU.add, [("stg", si), "vec"], [("stg", si)])
                cp(hb[:, j, :], stg[si][:], [("stg", si)], [("hb", j)], eng="act")
                dma("act", hres[:, j, :], stg[si][:], [("stg", si)], [("hres", j)])
                if write_out is not None:
                    dma("sp", write_out[:, j, :], stg[si][:, NM:T], [("stg", si)], [("out", j)])

        def ffn(w1n, w3n, w2n, first, gated):
            for f in range(FC):
                wa = load_w(w1n, 0, KC, f)
                wgk = load_w(w3n, 0, KC, f)
                for (t0, tn) in TB:
                    pa, pg = next_ps(), next_ps()
                    for k in range(KC):
                        mm(pa, (slice(0, 128), slice(0, tn)), wt[wa][:, k, 0:128], hb[:, k, t0:t0 + tn], k == 0, k == KC - 1, [("wt", wa)] + HBK)
                    for k in range(KC):
                        mm(pg, (slice(0, 128), slice(0, tn)), wt[wgk][:, k, 0:128], hb[:, k, t0:t0 + tn], k == 0, k == KC - 1, [("wt", wgk)] + HBK)
                    ti = next_tmp()
                    act(tmp[ti][:, 0:tn], ps[pa][:, 0:tn], AF.Silu, [("ps", pa)], [("tmp", ti)])
                    tt(HH[:, f, t0:t0 + tn], ps[pg][:, 0:tn], tmp[ti][:, 0:tn], ALU.mult, [("ps", pg), ("tmp", ti)], [("H", f)])
            HK = [("H", f) for f in range(FC)]
            for j in range(KC):
                grp = []
                for k0 in range(0, FC, KG):
                    nk = min(KG, FC - k0)
                    grp.append((k0, nk, load_w(w2n, k0, nk, j)))
                si = j % 3
                if first:
                    dma("act", stg[si][:], hres[:, j, :], [("hres", j)], [("stg", si)])
                    ts(stg[si][:], stg[si][:], ALPHA, None, ALU.mult, None, [("stg", si)], [("stg", si)])
                else:
                    dma("act", stg[si][:], zres[:, j, :], [("zres", j)], [("stg", si)])
                for (t0, tn) in TB:
                    pa = next_ps()
                    for (k0, nk, wi) in grp:
                        for k in range(nk):
                            mm(pa, (slice(0, 128), slice(0, tn)), wt[wi][:, k, 0:128], HH[:, k0 + k, t0:t0 + tn],
                               k0 + k == 0, k0 + k == FC - 1, [("wt", wi)] + HK)
                    if gated:
                        ti = next_tmp()
                        tt(tmp[ti][:, 0:tn], ps[pa][:, 0:tn], Gb[:, t0:t0 + tn], ALU.mult, [("ps", pa), "Gb"], [("tmp", ti)])
                        tt(stg[si][:, t0:t0 + tn], stg[si][:, t0:t0 + tn], tmp[ti][:, 0:tn], ALU.add, [("stg", si), ("tmp", ti)], [("stg", si)])
                    else:
                        tt(stg[si][:, t0:t0 + tn], stg[si][:, t0:t0 + tn], ps[pa][:, 0:tn], ALU.add, [("stg", si), ("ps", pa)], [("stg", si)])
                dma("act", zres[:, j, :], stg[si][:], [("stg", si)], [("zres", j)])

        def router(li):
            dma("sp", rtw[:], rtr[li], [], ["rtw"])
            dma("sp", sm[0:1, 0:NE], rtb[li], [], ["rtbias"])
            for (t0, nt) in TILES:
                pl = next_ps()
                for g0 in range(0, KC, 4):
                    gi = next_tmp()
                    ng = min(4, KC - g0)
                    hv = tmp[gi][:, 0:ng * 128].rearrange("p (k n) -> p k n", n=128)
                    dma("sp", hv[:, :, 0:nt], hres[:, g0:g0 + ng, t0:t0 + nt], [("hres", j) for j in range(g0, g0 + ng)], [("tmp", gi)])
                    for k in range(ng):
                        mm(pl, (slice(0, nt), slice(0, NE)), hv[:, k, 0:nt], rtw[:, g0 + k, :], g0 + k == 0, False, [("tmp", gi), "rtw"])
                mm(pl, (slice(0, nt), slice(0, NE)), cst[0:1, ONES, 0:nt], sm[0:1, 0:NE], False, True, ["cst", "rtbias"])
                L0 = lg[0:nt, 0:NE]
                L1 = lg[0:nt, 8:8 + NE]
                K1 = lg[0:nt, 16:16 + NE]
                K2 = lg[0:nt, 24:24 + NE]
                GG = lg[0:nt, 32:32 + NE]
                m1, m2, dd, g1, g2 = (lg[0:nt, 40 + i:41 + i] for i in range(5))
                LK = ["lg"]
                cp(L0, ps[pl][0:nt, 0:NE], [("ps", pl)], LK)
                S.op("dve", lambda e, o=m1, i=L0: e.reduce_max(out=o, in_=i, axis=mybir.AxisListType.X), LK, LK)
                ts(K1, L0, m1, None, ALU.is_equal, None, LK, LK)
                stt(L1, K1, -1e30, L0, ALU.mult, ALU.add, LK, LK)
                S.op("dve", lambda e, o=m2, i=L1: e.reduce_max(out=o, in_=i, axis=mybir.AxisListType.X), LK, LK)
                ts(K2, L1, m2, None, ALU.is_equal, None, LK, LK)
                tt(dd, m2, m1, ALU.subtract, LK, LK)
                act(dd, dd, AF.Exp, LK, LK)
                ts(g1, dd, 1.0, None, ALU.add, None, LK, LK)
                recip(g1, g1, LK, LK)
                tt(g2, dd, g1, ALU.mult, LK, LK)
                ts(GG, K1, g1, None, ALU.mult, None, LK, LK)
                stt(GG, K2, g2, GG, ALU.mult, ALU.add, LK, LK)
                pt = next_ps()
                mm(pt, (slice(0, NE), slice(0, nt)), GG, cst[0:nt, IDN, 0:nt], True, True, LK + ["cst"])
                cp(GT[0:NE, t0:t0 + nt], ps[pt][0:NE, 0:nt], [("ps", pt)], ["GT"], eng="act")

        def gate_bcast(e):
            ts(Gsel[0:NE, :], GT[0:NE, :], cst[0:NE, IDN, e:e + 1], None, ALU.mult, None, ["GT", "cst"], ["Gsel"])
            for (t0, tn) in TB:
                pi = next_ps()
                mm(pi, (slice(0, 128), slice(0, tn)), cst[0:NE, ONES, :], Gsel[0:NE, t0:t0 + tn], True, True, ["Gsel", "cst"])
                cp(Gb[:, t0:t0 + tn], ps[pi][:, 0:tn], [("ps", pi)], ["Gb"], eng="act")

        def gla_head(l, h, wn):
            RAK = [("RA", h * VH + v) for v in range(VH)]
            QK = [("RM", h * KH + k) for k in range(KH)]
            KK = [("RM", NH * KH + k) for k in range(KH)]
            dma("sp", auh[0:R1, 0:DKH], alup[l, h], [], ["auh"])
            for kh in range(KH):
                proj(wn, cQ + h * KH + kh, 128, TB,
                     lambda t0, tn, pi, kh=kh: cp(RM[:, h * KH + kh, t0:t0 + tn], ps[pi][:, 0:tn], [("ps", pi)], [("RM", h * KH + kh)], eng="act"))
                proj(wn, cK + h * KH + kh, 128, TB,
                     lambda t0, tn, pi, kh=kh: cp(RM[:, NH * KH + kh, t0:t0 + tn], ps[pi][:, 0:tn], [("ps", pi)], [("RM", NH * KH + kh)]))
            for vv in range(VH):
                proj(wn, cV + h * VH + vv, 128, TB,
                     lambda t0, tn, pi, vv=vv: cp(RA[:, h * VH + vv, t0:t0 + tn], ps[pi][:, 0:tn], [("ps", pi)], [("RA", h * VH + vv)],
                                                  eng="act" if vv % 2 else "dve"))
            memset(Sst[:, :, :], 0.0, ["Sst"])
            memset(Sb[:, :, :], 0.0, ["Sb"])
            memset(erun, 1.0, ["erun"])
            for (t0, nt) in TILES:
                pre = t0 < NM
                chunks = [(0, nt)] if nt <= 64 else [(0, 64), (64, 64)]
                pa_ = next_ps()
                mm(pa_, (slice(0, nt), slice(0, DKH)), alT[0:R1, t0:t0 + nt], auh[0:R1, 0:DKH], True, True, ["alT", "auh"])
                act(e1[0:nt, :], ps[pa_][0:nt, 0:DKH], AF.Exp, [("ps", pa_)], ["e1"], scale=-1.0)
                act(ltok[0:nt, :], e1[0:nt, :], AF.Ln, ["e1"], ["ltok"], bias=1.0)
                pe_ = next_ps()
                mm(pe_, (slice(0, nt), slice(0, DKH)), cst[0:nt, UPP, 0:nt], ltok[0:nt, :], True, True, ["ltok", "cst"])
                act(ef[0:nt, :], ps[pe_][0:nt, 0:DKH], AF.Exp, [("ps", pe_)], ["ef"])
                pb_ = next_ps()
                for kh in range(KH):
                    mm(pb_, (slice(0, 128), slice(kh * 128, kh * 128 + nt)), ltok[0:nt, kh * 128:(kh + 1) * 128], cst[0:nt, TRI, 0:nt],
                       True, True, ["ltok", "cst"])
                pbv = ps[pb_][:, 0:KH * 128].rearrange("p (k n) -> p k n", n=128)[:, :, 0:nt]
                act(eB[:, :, 0:nt], pbv, AF.Exp, [("ps", pb_)], ["eB"])
                act(enB[:, :, 0:nt], pbv, AF.Exp, [("ps", pb_)], ["enB"], scale=-1.0)
                pk_ = next_ps()
                for kh in range(KH):
                    mm(pk_, (slice(0, nt), slice(kh * 128, (kh + 1) * 128)), RM[:, NH * KH + kh, t0:t0 + nt], idb[:, :], True, True, KK + ["idb"])
                tt(kte[0:nt, :], ps[pk_][0:nt, 0:DKH], ef[0:nt, :], ALU.mult, [("ps", pk_), "ef"], ["kte"])
                if pre:
                    ts(kte[0:nt, :], kte[0:nt, :], fl[0:nt, 0:1], None, ALU.mult, None, ["kte", "fl"], ["kte"])
                pv_ = next_ps()
                for vv in range(VH):
                    mm(pv_, (slice(0, nt), slice(vv * 128, (vv + 1) * 128)), RA[:, h * VH + vv, t0:t0 + nt], idb[:, :], True, True, RAK + ["idb"])
                cp(vt[0:nt, :], ps[pv_][0:nt, 0:DVH], [("ps", pv_)], ["vt"], eng="act")
                stt(qd[:, :, 0:nt], RM[:, h * KH:(h + 1) * KH, t0:t0 + nt], QSCALE, eB[:, :, 0:nt], ALU.mult, ALU.mult, QK + ["eB"], ["qd"])
                tt(kd[:, :, 0:nt], RM[:, NH * KH:NH * KH + KH, t0:t0 + nt], enB[:, :, 0:nt], ALU.mult, KK + ["enB"], ["kd"])
                ps_ = next_ps()
                for kh in range(KH):
                    mm(ps_, (slice(0, nt), slice(0, nt)), kd[:, kh, 0:nt], qd[:, kh, 0:nt], kh == 0, kh == KH - 1, ["kd", "qd"])
                tt(sT[0:nt, 0:nt], ps[ps_][0:nt, 0:nt], cst[0:nt, CM, 0:nt], ALU.mult, [("ps", ps_), "cst"], ["sT"])
                for (c0, cn) in chunks:
                    cs = slice(c0, c0 + cn)
                    po = next_ps()
                    for vv in range(VH):
                        osl = (slice(0, 128), slice(vv * 64, vv * 64 + cn))
                        mm(po, osl, vt[cs, vv * 128:(vv + 1) * 128], sT[cs, cs], True, False, ["vt", "sT"])
                        for kh in range(KH):
                            mm(po, osl, Sb[:, kh, vv * 128:(vv + 1) * 128], qd[:, kh, cs], False, kh == KH - 1, ["Sb", "qd"])
                    for kh in range(KH):
                        ts(RM[:, h * KH + kh, t0 + c0:t0 + c0 + cn], qd[:, kh, cs], erun[:, kh:kh + 1], None, ALU.mult, None,
                           ["qd", "erun"], [("RM", h * KH + kh)])
                    pov = ps[po][:, 0:VH * 64].rearrange("p (v n) -> p v n", n=64)[:, :, 0:cn]
                    cp(RA[:, h * VH:(h + 1) * VH, t0 + c0:t0 + c0 + cn], pov, [("ps", po)], RAK, eng="act")
                    for kh in range(KH):
                        pS = next_ps()
                        mm(pS, (slice(0, 128), slice(0, DVH)), kte[cs, kh * 128:(kh + 1) * 128], vt[cs, :], True, True, ["kte", "vt"])
                        dec = eB[:, kh, c0 + cn - 1:c0 + cn]
                        stt(Sst[:, kh, :], Sst[:, kh, :], dec, ps[pS][:, 0:DVH], ALU.mult, ALU.add, ["Sst", "eB", ("ps", pS)], ["Sst"])
                        cp(Sb[:, kh, :], Sst[:, kh, :], ["Sst"], ["Sb"], eng="act")
                        if not pre:
                            tt(erun[:, kh:kh + 1], erun[:, kh:kh + 1], dec, ALU.mult, ["erun", "eB"], ["erun"])
            dma("sp", xsrc[h][:, 0:KH * DVH], Sstf, ["Sst"], [("xsrc", h)])
            cp(dsg[:, h * KH:(h + 1) * KH], erun, ["erun"], ["dsg"])
            S.op("pool", lambda e, a=xsrc[h], b=xdst[h]: e.collective_compute(
                "AllGather", ALU.bypass, replica_groups=GROUPS, ins=[a], outs=[b]), [("xsrc", h)], [("xdst", h)], kind="cc")

        def gla_finish_head(l, h, wn):
            RAK = [("RA", h * VH + v) for v in range(VH)]
            memset(Sst[:, :, :], 0.0, ["Sst"])
            for j in range(3):
                dma("sp", Sx[:, :], xdst[h][j * 128:(j + 1) * 128, 0:KH * DVH], [("xdst", h)], ["Sx"])
                ts(acoef, dall[:, j, h * KH:(h + 1) * KH], -1.0, fl[:, 1 + j:2 + j], ALU.add, ALU.mult, ["dall", "fl"], ["acoef"])
                ts(acoef, acoef, 1.0, None, ALU.add, None, ["acoef"], ["acoef"])
                ts(Sx[:, :], Sx[:, :], fl[:, 1 + j:2 + j], None, ALU.mult, None, ["Sx", "fl"], ["Sx"])
                for kh in range(KH):
                    stt(Sst[:, kh, :], Sst[:, kh, :], acoef[:, kh:kh + 1], Sx[:, kh * DVH:(kh + 1) * DVH], ALU.mult, ALU.add,
                        ["Sst", "acoef", "Sx"], ["Sst"])
            cp(Sb[:, :, :], Sst[:, :, :], ["Sst"], ["Sb"], eng="act")
            for vv in range(VH):
                for (t0, tn) in TB:
                    pi = next_ps()
                    for kh in range(KH):
                        mm(pi, (slice(0, 128), slice(0, tn)), Sb[:, kh, vv * 128:(vv + 1) * 128], RM[:, h * KH + kh, t0:t0 + tn],
                           kh == 0, kh == KH - 1, ["Sb", ("RM", h * KH + kh)])
                    tt(RA[:, h * VH + vv, t0:t0 + tn], RA[:, h * VH + vv, t0:t0 + tn], ps[pi][:, 0:tn], ALU.add,
                       [("ps", pi), ("RA", h * VH + vv)], [("RA", h * VH + vv)])
            for bi, (t0, tn) in enumerate(TB):
                pr = next_ps()
                for vv in range(VH):
                    ti = next_tmp()
                    act(tmp[ti][:, 0:tn], RA[:, h * VH + vv, t0:t0 + tn], AF.Square, [("RA", h * VH + vv)], [("tmp", ti)])
                    mm(pr, (slice(0, 128), slice(0, tn)), cst[:, ONES, :], tmp[ti][:, 0:tn], vv == 0, vv == VH - 1, [("tmp", ti), "cst"])
                ta, tb_ = next_tmp(), next_tmp()
                ts(tmp[ta][:, 0:tn], ps[pr][:, 0:tn], 1.0 / DVH, EPS, ALU.mult, ALU.add, [("ps", pr)], [("tmp", ta)])
                act(tmp[tb_][:, 0:tn], tmp[ta][:, 0:tn], AF.Sqrt, [("tmp", ta)], [("tmp", tb_)])
                recip(stat[:, 1, t0:t0 + tn], tmp[tb_][:, 0:tn], [("tmp", tb_)], [("stat", 1, bi)])
            for vv in range(VH):
                jj = h * VH + vv

                def epi_r(t0, tn, pi, jj=jj):
                    ti = next_tmp()
                    act(tmp[ti][:, 0:tn], ps[pi][:, 0:tn], AF.Silu, [("ps", pi)], [("tmp", ti)])
                    tt(tmp[ti][:, 0:tn], tmp[ti][:, 0:tn], stat[:, 1, t0:t0 + tn], ALU.mult, [("tmp", ti)] + SK, [("tmp", ti)])
                    stt(RA[:, jj, t0:t0 + tn], RA[:, jj, t0:t0 + tn], gn[:, l, jj:jj + 1], tmp[ti][:, 0:tn], ALU.mult, ALU.mult,
                        [("RA", jj), "gn", ("tmp", ti)], [("RA", jj)])
                proj(wn, cR + jj, 128, TB, epi_r)

        gather_through(lambda n: wl_of[n] == 0)
        pss, pqq = [next_ps() for _ in TB], [next_ps() for _ in TB]
        for j in range(KC):
            si = j % 3
            dma("sp", stg[si][:], xT[:, j, :], [], [("stg", si)])
            ln_accum(pss, pqq, j, stg[si], ("stg", si))
            dma("sp", zres[:, j, :], stg[si][:], [("stg", si)], [("zres", j)])
        ln_finish(pss, pqq, D)
        ln_apply(zres, 0, 1)

        for l in range(DEPTH):
            vb = 2 + l * 9
            V_CB, V_CNG, V_CNB, V_MG, V_MB, V_FG, V_FB = [vb + i for i in range(7)]
            wn = f"w_in{l}"
            barrier()
            wt_act[0] = wt_mix
            dma("sp", cw[:], convw[l], [], ["cw"])
            if STOP != "noGLA":
                memset(alT[0:32, :], 1.0, ["alT"])
                proj(wn, cA, LOWR, TB, lambda t0, tn, pi: cp(alT[0:LOWR, t0:t0 + tn], ps[pi][0:LOWR, 0:tn], [("ps", pi)], ["alT"]))
                for h in range(NH):
                    gla_head(l, h, wn)
                dma("sp", xsrc[NH][:, 0:NH * KH], dsg, ["dsg"], [("xsrc", NH)])
                S.op("pool", lambda e, a=xsrc[NH], b=xdst[NH]: e.collective_compute(
                    "AllGather", ALU.bypass, replica_groups=GROUPS, ins=[a], outs=[b]), [("xsrc", NH)], [("xdst", NH)], kind="cc")
            memset(Ue[:, :, 0:HALO], 0.0, [("Ue", j) for j in range(KC)])
            for j in range(KC):
                wa = load_w(wn, 0, KC, cG + j)
                wgk = load_w(wn, 0, KC, cG + KC + j)
                for (t0, tn) in TB:
                    pa, pg = next_ps(), next_ps()
                    for k in range(KC):
                        mm(pa, (slice(0, 128), slice(0, tn)), wt[wa][:, k, 0:128], hb[:, k, t0:t0 + tn], k == 0, k == KC - 1, [("wt", wa)] + HBK)
                    for k in range(KC):
                        mm(pg, (slice(0, 128), slice(0, tn)), wt[wgk][:, k, 0:128], hb[:, k, t0:t0 + tn], k == 0, k == KC - 1, [("wt", wgk)] + HBK)
                    ti = next_tmp()
                    act(tmp[ti][:, 0:tn], ps[pg][:, 0:tn], AF.Sigmoid, [("ps", pg)], [("tmp", ti)])
                    u0 = upos(t0)
                    tt(Ue[:, j, u0:u0 + tn], ps[pa][:, 0:tn], tmp[ti][:, 0:tn], ALU.mult, [("ps", pa), ("tmp", ti)], [("Ue", j)])
            for j in range(KC):
                cp(hal[:, j * HALO:(j + 1) * HALO], Ue[:, j, UW - HALO:UW], [("Ue", j)], ["hal"])
            dma("sp", usrc[:, 0:KC * HALO], hal[:], ["hal"], ["usrc"])
            S.op("pool", lambda e: e.collective_compute("AllGather", ALU.bypass, replica_groups=GROUPS,
                                                        ins=[usrc.ap()], outs=[udst.ap()]), ["usrc"], ["udst"], kind="cc")
            gather_after_exchange(l)
            if STOP != "noGLA":
                for j in range(3):
                    dma("sp", dall[:, j, 0:NH * KH], xdst[NH][j * 128:(j + 1) * 128, 0:NH * KH], [("xdst", NH)], ["dall"])
                for h in range(NH):
                    gla_finish_head(l, h, wn)
            barrier()
            ts(hal[:], hal[:], 0.0, None, ALU.mult, None, ["hal", "usrc"], ["hal"])
            for jj in range(3):
                dma("sp", halg[:, :], udst[jj * 128:(jj + 1) * 128, 0:KC * HALO], ["udst"], ["halg"])
                stt(hal[:], halg[:, :], fl[:, 4 + jj:5 + jj], hal[:], ALU.mult, ALU.add, ["halg", "fl", "hal"], ["hal"])
            W = NM + HALO + TL
            pss, pqq = list(range(NB)), list(range(NB, 2 * NB))
            cbanks = list(range(2 * NB, 8))
            cblocks = [(c0, min(512, W - c0)) for c0 in range(0, W, 512)]
            cbi = 0
            dgi = 0
            for j in range(KC):
                hj = hal[:, j * HALO:(j + 1) * HALO]
                stt(hj[:, HALO - NM:HALO], Ue[:, j, HALO:HALO + NM], fl[:, 0:1], hj[:, HALO - NM:HALO], ALU.mult, ALU.add,
                    [("Ue", j), "fl", "hal"], ["hal"])
                cp(Ue[:, j, HALO + NM:HALO + NM + HALO], hj, ["hal"], [("Ue", j)])
                si = j % 3
                for (c0, cn) in cblocks:
                    pc = cbanks[cbi % len(cbanks)]
                    cbi += 1
                    for k in range(CW):
                        di = dgi % 4
                        dgi += 1
                        ts(dg[:, di, :], idb[:, :], cw[:, j, k:k + 1], None, ALU.mult, None, ["idb", "cw"], [("dg", di)])
                        mm(pc, (slice(0, 128), slice(0, cn)), dg[:, di, :], Ue[:, j, c0 + k:c0 + k + cn], k == 0, k == CW - 1,
                           [("dg", di), ("Ue", j)])
                    a0, a1 = c0, min(c0 + cn, NM)
                    if a1 > a0:
                        act(stg[si][:, a0:a1], ps[pc][:, a0 - c0:a1 - c0], AF.Identity, [("ps", pc), "vec"], [("stg", si)],
                            bias=vec[:, V_CB, j:j + 1])
                    b0, b1 = max(c0, NM + HALO), min(c0 + cn, W)
                    if b1 > b0:
                        act(stg[si][:, b0 - HALO:b1 - HALO], ps[pc][:, b0 - c0:b1 - c0], AF.Identity, [("ps", pc), "vec"], [("stg", si)],
                            bias=vec[:, V_CB, j:j + 1])
                ln_accum(pss, pqq, j, stg[si], ("stg", si))
                cp(Ue[:, j, HALO:HALO + NM], stg[si][:, 0:NM], [("stg", si)], [("Ue", j)])
                cp(Ue[:, j, HALO + NM + HALO:UW], stg[si][:, NM:T], [("stg", si)], [("Ue", j)])
            ln_finish(pss, pqq, D)
            for j in range(KC):
                si = j % 3
                cp(stg[si][:, 0:NM], Ue[:, j, HALO:HALO + NM], [("Ue", j)], [("stg", si)])
                cp(stg[si][:, NM:T], Ue[:, j, HALO + NM + HALO:UW], [("Ue", j)], [("stg", si)])
                tt(stg[si][:], stg[si][:], stat[:, 0, :], ALU.subtract, [("stg", si)] + SK, [("stg", si)])
                tt(stg[si][:], stg[si][:], stat[:, 1, :], ALU.mult, [("stg", si)] + SK, [("stg", si)])
                ts(stg[si][:], stg[si][:], vec[:, V_CNG, j:j + 1], vec[:, V_CNB, j:j + 1], ALU.mult, ALU.add, [("stg", si), "vec"], [("stg", si)])
                act(Ue[:, j, HALO:HALO + NM], stg[si][:, 0:NM], AF.Silu, [("stg", si)], [("Ue", j)])
                act(Ue[:, j, HALO + NM + HALO:UW], stg[si][:, NM:T], AF.Silu, [("stg", si)], [("Ue", j)])
            UK = [("Ue", j) for j in range(KC)]
            for j in range(KC):
                wa = load_w(f"w_convo{l}", 0, KC, j)
                wgk = load_w(wn, 0, KC, cGB + j)
                for (t0, tn) in TB:
                    pa, pg = next_ps(), next_ps()
                    u0 = upos(t0)
                    for k in range(KC):
                        mm(pa, (slice(0, 128), slice(0, tn)), wt[wa][:, k, 0:128], Ue[:, k, u0:u0 + tn], k == 0, k == KC - 1, [("wt", wa)] + UK)
                    for k in range(KC):
                        mm(pg, (slice(0, 128), slice(0, tn)), wt[wgk][:, k, 0:128], hb[:, k, t0:t0 + tn], k == 0, k == KC - 1, [("wt", wgk)] + HBK)
                    ti = next_tmp()
                    act(tmp[ti][:, 0:tn], ps[pg][:, 0:tn], AF.Sigmoid, [("ps", pg)], [("tmp", ti)])
                    tt(RM[:, j, t0:t0 + tn], ps[pa][:, 0:tn], tmp[ti][:, 0:tn], ALU.mult, [("ps", pa), ("tmp", ti)], [("RM", j)])
            if STOP != "noGLA":
                RAALL = [("RA", j) for j in range(KC)]
                for j in range(KC):
                    wa = load_w(f"w_glao{l}", 0, KC, j)
                    wgk = load_w(wn, 0, KC, cGA + j)
                    for (t0, tn) in TB:
                        pa, pg = next_ps(), next_ps()
                        for k in range(KC):
                            mm(pa, (slice(0, 128), slice(0, tn)), wt[wa][:, k, 0:128], RA[:, k, t0:t0 + tn], k == 0, k == KC - 1, [("wt", wa)] + RAALL)
                        for k in range(KC):
                            mm(pg, (slice(0, 128), slice(0, tn)), wt[wgk][:, k, 0:128], hb[:, k, t0:t0 + tn], k == 0, k == KC - 1, [("wt", wgk)] + HBK)
                        ti = next_tmp()
                        act(tmp[ti][:, 0:tn], ps[pg][:, 0:tn], AF.Sigmoid, [("ps", pg)], [("tmp", ti)])
                        tt(tmp[ti][:, 0:tn], ps[pa][:, 0:tn], tmp[ti][:, 0:tn], ALU.mult, [("ps", pa), ("tmp", ti)], [("tmp", ti)])
                        tt(RM[:, j, t0:t0 + tn], RM[:, j, t0:t0 + tn], tmp[ti][:, 0:tn], ALU.add, [("RM", j), ("tmp", ti)], [("RM", j)])
            MK = [("RM", j) for j in range(KC)]
            for j in range(KC):
                wa = load_w(f"w_out{l}", 0, KC, j)
                si = j % 3
                dma("act", stg[si][:], hres[:, j, :], [("hres", j)], [("stg", si)])
                for (t0, tn) in TB:
                    pa = next_ps()
                    for k in range(KC):
                        mm(pa, (slice(0, 128), slice(0, tn)), wt[wa][:, k, 0:128], RM[:, k, t0:t0 + tn], k == 0, k == KC - 1, [("wt", wa)] + MK)
                    stt(stg[si][:, t0:t0 + tn], stg[si][:, t0:t0 + tn], ALPHA, ps[pa][:, 0:tn], ALU.mult, ALU.add, [("stg", si), ("ps", pa)], [("stg", si)])
                dma("act", zres[:, j, :], stg[si][:], [("stg", si)], [("zres", j)])
            ln_from_dram(zres)
            ln_apply(zres, V_MG, V_MB)
            barrier()
            wt_act[0] = wt_ffn
            last_out = out if l == DEPTH - 1 else None
            if l % 2 == 0:
                ffn(f"ffn1_{l}", f"ffn3_{l}", f"ffn2_{l}", True, False)
            else:
                router(l // 2)
                for e in range(NE):
                    gate_bcast(e)
                    ffn(f"moe1_{l}_{e}", f"moe3_{l}_{e}", f"moe2_{l}_{e}", e == 0, True)
            ln_from_dram(zres)
            ln_apply(zres, V_FG, V_FB, write_out=last_out)
        S.emit(nc, sems, dma_sems, cc_sem)
    return nc


def make_inputs(cfg, inp):
    D, SEQ, DEPTH, DFF, NE, NM, LOWR, CW = (cfg[k] for k in ("D", "SEQ", "DEPTH", "DFF", "NE", "NMETA", "LOWR", "CW"))
    KC = D // 128
    TL = SEQ // 4
    T = NM + TL
    f32 = lambda a: np.ascontiguousarray(np.asarray(a), dtype=np.float32)
    fm = lambda v: f32(v).reshape(KC, 128).T
    x = f32(inp["x"])
    meta = f32(inp["meta_tokens"])
    nv = 2 + DEPTH * 9
    vecs = np.zeros((128, nv, KC), np.float32)
    vecs[:, 0] = fm(inp["ln_in_g"]); vecs[:, 1] = fm(inp["ln_in_b"])
    for l in range(DEPTH):
        b = 2 + l * 9
        for i, k in enumerate(("conv_b", "conv_norm_g", "conv_norm_b", "ln_mix_g", "ln_mix_b", "ln_ffn_g", "ln_ffn_b")):
            vecs[:, b + i] = fm(f32(inp[k])[l])
    convw = np.ascontiguousarray(f32(inp["conv_w"]).reshape(DEPTH, CW, KC, 128).transpose(0, 3, 2, 1))
    gnorm = np.ascontiguousarray(f32(inp["gla_norm_g"]).reshape(DEPTH, KC, 128).transpose(2, 0, 1))
    NH = 4
    DK = D // 2
    DKH = DK // NH
    al = np.concatenate([f32(inp["w_alpha_up"]), f32(inp["b_alpha"])[:, None, :]], 1)
    alup = np.ascontiguousarray(al.reshape(DEPTH, LOWR + 1, NH, DKH).transpose(0, 2, 1, 3))
    NMOE = max(1, DEPTH // 2)
    rtr = np.zeros((NMOE, 128, KC, NE), np.float32)
    rtb = np.zeros((NMOE, 1, NE), np.float32)
    if DEPTH // 2:
        rtr[:] = f32(inp["router_w"]).reshape(DEPTH // 2, KC, 128, NE).transpose(0, 2, 1, 3)
        rtb[:, 0] = f32(inp["router_b"])
    consts = np.zeros((128, 5, 128), np.float32)
    ii = np.arange(128)
    same = (ii[:, None] // 64) == (ii[None, :] // 64)
    consts[:, 0] = np.eye(128)
    consts[:, 1] = (same & (ii[:, None] <= ii[None, :])) * (-1.0 / 16.0)
    consts[:, 2] = (same & (ii[:, None] > ii[None, :])) * (-1.0 / 16.0)
    consts[:, 3] = 1.0
    consts[:, 4] = (same & (ii[:, None] <= ii[None, :])) * 1.0
    shared = dict(vecs=vecs, convw=convw, gnorm=gnorm, alup=alup, rtr=rtr, rtb=rtb, consts=consts)
    wsrc = {}
    for l in range(DEPTH):
        wsrc[f"w_in{l}"] = inp["w_in"][l]; wsrc[f"w_glao{l}"] = inp["w_gla_o"][l]
        wsrc[f"w_convo{l}"] = inp["w_conv_o"][l]; wsrc[f"w_out{l}"] = inp["w_out"][l]
        if l % 2 == 0:
            wsrc[f"ffn1_{l}"] = inp["ffn_w1"][l // 2]; wsrc[f"ffn3_{l}"] = inp["ffn_w3"][l // 2]; wsrc[f"ffn2_{l}"] = inp["ffn_w2"][l // 2]
        else:
            for e in range(NE):
                wsrc[f"moe1_{l}_{e}"] = inp["moe_w1"][l // 2][e]; wsrc[f"moe3_{l}_{e}"] = inp["moe_w3"][l // 2][e]
                wsrc[f"moe2_{l}_{e}"] = inp["moe_w2"][l // 2][e]
    DK_ = D // 2
    oA_ = 2 * DK_ + 2 * D

    def tile_major(n, w):
        w = f32(w)
        if n.startswith("w_in"):
            w = np.concatenate([w[:, :oA_], w[:, oA_ + LOWR:], w[:, oA_:oA_ + LOWR], np.zeros((w.shape[0], 128 - LOWR), np.float32)], 1)
        K_, N_ = w.shape
        return np.ascontiguousarray(w.reshape(K_ // 128, 128, N_ // 128, 128).transpose(2, 1, 0, 3))

    wsh = [dict() for _ in range(4)]
    for n, w in wsrc.items():
        wtm = tile_major(n, w)
        for r in range(4):
            wsh[r][n] = shard_weight(wtm, r)
        del wtm
    maps = []
    for core in range(8):
        b, c = core // 4, core % 4
        tok = np.concatenate([meta, x[b, c * TL:(c + 1) * TL]], 0)
        xT = np.ascontiguousarray(tok.T.reshape(KC, 128, T).transpose(1, 0, 2))
        fl = np.zeros((128, 8), np.float32)
        fl[:, 0] = 1.0 if c == 0 else 0.0
        for j in range(3):
            fl[:, 1 + j] = 1.0 if j < c else 0.0
            fl[:, 4 + j] = 1.0 if j == c - 1 else 0.0
        m = dict(shared)
        m.update(xT=xT, flags=fl)
        m.update(wsh[c])
        maps.append(m)
    return maps


def run(cfg, inp):
    nc = build(cfg)
    maps = make_inputs(cfg, inp)
    res = run_bass_kernel_spmd(nc, maps, core_ids=list(range(8)))
    D, SEQ = cfg["D"], cfg["SEQ"]
    TL = SEQ // 4
    B = 2
    o = np.zeros((B, SEQ, D), np.float32)
    for core in range(8):
        b, c = core // 4, core % 4
        y = res.results[core]["out"]
        o[b, c * TL:(c + 1) * TL] = y.transpose(2, 1, 0).reshape(TL, D)
    return o


def kernel(**inputs):
    return run(dict(CFG_FULL), inputs)
```

```python
import numpy as np
import concourse.bass as bass
import concourse.mybir as mybir
from concourse.bass_utils import run_bass_kernel_spmd

F32 = mybir.dt.float32
BF16 = mybir.dt.bfloat16
AF = mybir.ActivationFunctionType
ALU = mybir.AluOpType

CFG_FULL = dict(D=2048, SEQ=4096, DEPTH=4, DFF=5632, NE=8, NMETA=16, LOWR=16, CW=31)
PR, PC = 256, 2048
PIECE = PR * PC
GROUPS = [[0, 1, 2, 3], [4, 5, 6, 7]]
GQOS = {"dma_qos": "P2"}


class Op:
    __slots__ = ("eng", "fn", "deps", "kind", "inc", "sem", "val", "idx")


class Sched:
    def __init__(self):
        self.ops = []
        self.lastw = {}
        self.readers = {}
        self.last_barrier = None
        self.last_eng = {}
        self.recent_sp = []

    def op(self, eng, fn, reads=(), writes=(), kind="c"):
        o = Op()
        o.eng, o.fn, o.kind, o.inc, o.idx = eng, fn, kind, False, len(self.ops)
        deps = set()
        for b in reads:
            w = self.lastw.get(b)
            if w is not None:
                deps.add(w)
        for b in writes:
            w = self.lastw.get(b)
            if w is not None:
                deps.add(w)
            for r in self.readers.get(b, ()):
                deps.add(r)
        deps.discard(o.idx)
        if self.last_barrier is not None and eng != "pool":
            deps.add(self.last_barrier)
        o.deps = deps
        if kind == "dma" and eng in ("sp", "act"):
            self.recent_sp = (self.recent_sp + [o.idx])[-16:]
        elif kind == "c":
            self.last_eng[eng] = o.idx
        for b in reads:
            lst = self.readers.setdefault(b, [])
            if kind == "c":
                lst[:] = [r for r in lst if not (self.ops[r].eng == eng and self.ops[r].kind == "c")]
            lst.append(o.idx)
        for b in writes:
            self.lastw[b] = o.idx
            self.readers[b] = []
        self.ops.append(o)
        return o

    def barrier(self, fn):
        o = self.op("dve", fn, [], [])
        o.deps |= set(self.last_eng.values()) | set(self.recent_sp)
        o.deps.discard(o.idx)
        self.last_barrier = o.idx
        return o

    def emit(self, nc, sems, dma_sems, cc_sem):
        ops = self.ops
        for o in ops:
            for d in o.deps:
                p = ops[d]
                if p.eng == "pe" and o.eng == "pe" and p.kind == "c":
                    continue
                p.inc = True
        cnt = {e: 0 for e in sems}
        dcnt = {}
        rr = {"sp": 0, "pool": 0, "act": 0}
        ccn = 0
        prev_on_sem = {}
        for o in ops:
            if o.kind == "dma":
                lst = dma_sems[o.eng]
                s = lst[rr[o.eng] % len(lst)]
                rr[o.eng] += 1
                pv = prev_on_sem.get(id(s))
                if pv is not None:
                    o.deps.add(pv)
                prev_on_sem[id(s)] = o.idx
                dcnt[id(s)] = dcnt.get(id(s), 0) + 16
                o.sem, o.val, o.inc = s, dcnt[id(s)], True
            elif o.kind == "cc":
                ccn += 1
                o.sem, o.val, o.inc = cc_sem, ccn, True
            elif o.inc:
                cnt[o.eng] += 1
                o.sem, o.val = sems[o.eng], cnt[o.eng]
        per = {"pe": [], "act": [], "dve": [], "pool": [], "sp": []}
        waited = {e: {} for e in per}
        for o in ops:
            ws = {}
            for d in o.deps:
                p = ops[d]
                if not p.inc:
                    continue
                if p.eng == "pe" and o.eng == "pe" and p.kind == "c":
                    continue
                k = id(p.sem)
                if waited[o.eng].get(k, 0) >= p.val:
                    continue
                if k not in ws or ws[k][1] < p.val:
                    ws[k] = (p.sem, p.val)
            for k, (s, v) in ws.items():
                waited[o.eng][k] = v
            per[o.eng].append((list(ws.values()), o))
        engs = {"pe": "tensor", "act": "scalar", "dve": "vector", "pool": "gpsimd", "sp": "sync"}
        with nc.Block() as block:
            for en, lst in per.items():
                def body(e, lst=lst):
                    for ws, o in lst:
                        for s, v in ws:
                            e.wait_ge(s, v)
                        ins = o.fn(e)
                        if o.inc:
                            if o.kind == "dma":
                                ins.then_inc(o.sem, 16)
                            elif o.kind == "cc":
                                ins.then_inc(o.sem)
                            else:
                                ins.then_inc(o.sem, 1)
                    if en in ("sp", "pool", "act"):
                        for s in dma_sems[en]:
                            v = dcnt.get(id(s), 0)
                            if v:
                                e.wait_ge(s, v)
                getattr(block, engs[en])(body)


def n_pieces(numel):
    return -(-numel // (4 * PIECE))


def shard_weight(w, r):
    flat = np.ascontiguousarray(w, dtype=np.float32).reshape(-1)
    npc = n_pieces(flat.size)
    pad = npc * 4 * PIECE - flat.size
    if pad:
        flat = np.concatenate([flat, np.zeros(pad, np.float32)])
    return np.ascontiguousarray(flat.reshape(npc, 4, PR, PC)[:, r])


def build(cfg):
    D, SEQ, DEPTH, DFF, NE = cfg["D"], cfg["SEQ"], cfg["DEPTH"], cfg["DFF"], cfg["NE"]
    NM, LOWR, CW = cfg["NMETA"], cfg["LOWR"], cfg["CW"]
    STOP = cfg.get("STOP")
    KC = D // 128
    DK = D // 2
    NH = 4
    DKH = DK // NH
    DVH = D // NH
    KH = DKH // 128
    VH = DVH // 128
    FC = DFF // 128
    TL = SEQ // 4
    T = NM + TL
    DIN = DK + DK + D + D + LOWR + 2 * D + D + D
    oQ, oK, oV, oR, oA = 0, DK, 2 * DK, 2 * DK + D, 2 * DK + 2 * D
    oG = oA + LOWR
    oGA = oG + 2 * D
    oGB = oGA + D
    cA = 0
    cQ, cK, cV = 1, 1 + DK // 128, 1 + 2 * DK // 128
    cR = cV + KC
    cG = cR + KC
    cGA = cG + 2 * KC
    cGB = cGA + KC
    DINP = (cGB + KC) * 128
    ALPHA = (2.0 * DEPTH) ** 0.25
    QSCALE = DKH ** -0.5
    EPS = 1e-5
    HALO = CW - 1
    UW = HALO + NM + HALO + TL
    R1 = LOWR + 1
    NMOE = max(1, DEPTH // 2)
    TB = [(0, NM)] + [(NM + i, min(512, TL - i)) for i in range(0, TL, 512)]
    NB = len(TB)
    TILES = [(0, NM)] + [(NM + i, 128) for i in range(0, TL, 128)]

    def upos(t0):
        return HALO + t0 if t0 < NM else HALO + NM + HALO + (t0 - NM)

    nc = bass.Bass("TRN2", target_bir_lowering=False, num_devices=8)
    S = Sched()

    def dt_in(name, shape):
        return nc.dram_tensor(name, list(shape), F32, kind="ExternalInput")

    xT = dt_in("xT", [128, KC, T])
    vecs = dt_in("vecs", [128, 2 + DEPTH * 9, KC])
    convw = dt_in("convw", [DEPTH, 128, KC, CW])
    gnorm = dt_in("gnorm", [128, DEPTH, KC])
    alup = dt_in("alup", [DEPTH, NH, R1, DKH])
    rtr = dt_in("rtr", [NMOE, 128, KC, NE])
    rtb = dt_in("rtb", [NMOE, 1, NE])
    consts = dt_in("consts", [128, 5, 128])
    flags = dt_in("flags", [128, 8])
    out = nc.dram_tensor("out", [128, KC, TL], F32, kind="ExternalOutput")

    wspec = []
    for l in range(DEPTH):
        wspec += [(f"w_in{l}", D, DINP, l), (f"w_glao{l}", D, D, l), (f"w_convo{l}", D, D, l), (f"w_out{l}", D, D, l)]
        if l % 2 == 0:
            wspec += [(f"ffn1_{l}", D, DFF, l), (f"ffn3_{l}", D, DFF, l), (f"ffn2_{l}", DFF, D, l)]
        else:
            for e in range(NE):
                wspec += [(f"moe1_{l}_{e}", D, DFF, l), (f"moe3_{l}_{e}", D, DFF, l), (f"moe2_{l}_{e}", DFF, D, l)]
    win, wbn, wg, wview = {}, {}, {}, {}
    for name, K, N, _ in wspec:
        npc = n_pieces(K * N)
        win[name] = dt_in(name, [npc, PR, PC])
        wbn[name] = nc.dram_tensor(name + "_b", [npc, PR, PC], BF16)
        wg[name] = nc.dram_tensor(name + "_g", [npc, 4 * PR, PC], BF16)
        wview[name] = wg[name].ap().rearrange("a b c -> (a b c)")[0:K * N].rearrange("(j p k c) -> j p k c", p=128, k=K // 128, c=128)

    hres = nc.dram_tensor("hres", [128, KC, T], F32)
    zres = nc.dram_tensor("zres", [128, KC, T], F32)
    xsrc = nc.dram_tensor("xsrc", [NH + 1, 128, 1024], F32)
    xdst = nc.dram_tensor("xdst", [NH + 1, 512, 1024], F32)
    usrc = nc.dram_tensor("usrc", [128, 1024], F32)
    udst = nc.dram_tensor("udst", [512, 1024], F32)

    import contextlib
    es = contextlib.ExitStack()
    sb = lambda name, shape, dt=F32: es.enter_context(nc.sbuf_tensor(name, list(shape), dt))
    with es:
        hb = sb("hb", [128, KC, T], BF16)
        BIGN = KC * (2 * T + UW)
        assert FC * T <= BIGN
        big = sb("big", [128, BIGN], BF16)
        RA = big[:, 0:KC * T].rearrange("p (c t) -> p c t", t=T)
        Ue = big[:, KC * T:KC * T + KC * UW].rearrange("p (c t) -> p c t", t=UW)
        RM = big[:, KC * (T + UW):BIGN].rearrange("p (c t) -> p c t", t=T)
        HH = big[:, 0:FC * T].rearrange("p (c t) -> p c t", t=T)
        stat = sb("stat", [128, 2, T])
        vec = sb("vec", [128, 2 + DEPTH * 9, KC])
        cw = sb("cw", [128, KC, CW])
        gn = sb("gn", [128, DEPTH, KC])
        cst = sb("cst", [128, 5, 128])
        idb = sb("idb", [128, 128], BF16)
        dg = sb("dg", [128, 4, 128], BF16)
        fl = sb("fl", [128, 8])
        small = sb("small", [128, 80])
        KG = max(KC, 11)
        wt = [sb(f"wt{i}", [128, KG, 128], BF16) for i in range(4)]
        stg = [sb(f"stg{i}", [128, T]) for i in range(3)]
        tmp = [sb(f"tmp{i}", [128, 512]) for i in range(4)]
        hal = sb("hal", [128, KC * HALO])
        NF = max(6 * DKH + 2 * KH * DVH, KC * HALO + NM + HALO + TL, KC * NE + 3 * T + 64)
        scrF = sb("scrF", [128, NF])
        NHH = 3 * DKH + DVH + 128 + KH * DVH
        scrH = sb("scrH", [128, NHH], BF16)
        ps = [es.enter_context(nc.psum_tensor(f"ps{i}", [128, 512], F32)) for i in range(8)]
        sems = {e: es.enter_context(nc.semaphore(f"s_{e}")) for e in ("pe", "act", "dve")}
        dma_sems = {q: [es.enter_context(nc.semaphore(f"d_{q}{i}")) for i in range(8)] for q in ("sp", "pool")}
        dma_sems["act"] = [es.enter_context(nc.semaphore(f"d_act{i}")) for i in range(4)]
        cc_sem = es.enter_context(nc.semaphore("cc"))

        o_ = 0
        auh = scrF[:, o_:o_ + DKH]; o_ += DKH
        e1 = scrF[:, o_:o_ + DKH]; o_ += DKH
        ltok = scrF[:, o_:o_ + DKH]; o_ += DKH
        ef = scrF[:, o_:o_ + DKH]; o_ += DKH
        eB = scrF[:, o_:o_ + DKH].rearrange("p (k n) -> p k n", n=128); o_ += DKH
        enB = scrF[:, o_:o_ + DKH].rearrange("p (k n) -> p k n", n=128); o_ += DKH
        Sstf = scrF[:, o_:o_ + KH * DVH]
        Sst = Sstf.rearrange("p (k n) -> p k n", n=DVH); o_ += KH * DVH
        Sx = scrF[:, o_:o_ + KH * DVH]; o_ += KH * DVH
        halg = scrF[:, 0:KC * HALO]
        cacc = scrF[:, KC * HALO:KC * HALO + NM + HALO + TL]
        rtw = scrF[:, 0:KC * NE].rearrange("p (k n) -> p k n", n=NE)
        GT = scrF[:, KC * NE:KC * NE + T]
        Gsel = scrF[:, KC * NE + T:KC * NE + 2 * T]
        Gb = scrF[:, KC * NE + 2 * T:KC * NE + 3 * T]
        lg = scrF[:, KC * NE + 3 * T:KC * NE + 3 * T + 64]
        o_ = 0
        kte = scrH[:, o_:o_ + DKH]; o_ += DKH
        vt = scrH[:, o_:o_ + DVH]; o_ += DVH
        qd = scrH[:, o_:o_ + DKH].rearrange("p (k n) -> p k n", n=128); o_ += DKH
        kd = scrH[:, o_:o_ + DKH].rearrange("p (k n) -> p k n", n=128); o_ += DKH
        sT = scrH[:, o_:o_ + 128]; o_ += 128
        Sb = scrH[:, o_:o_ + KH * DVH].rearrange("p (k n) -> p k n", n=DVH); o_ += KH * DVH
        wt_mix = list(range(len(wt)))
        for i in range(2):
            if FC * T + (i + 1) * KG * 128 <= BIGN:
                wt.append(big[:, FC * T + i * KG * 128:FC * T + (i + 1) * KG * 128].rearrange("p (k c) -> p k c", c=128))
        if NHH >= KG * 128:
            wt.append(scrH[:, 0:KG * 128].rearrange("p (k c) -> p k c", c=128))
        wt_ffn = list(range(len(wt)))
        wt_act = [wt_mix]
        alT = stat[:, 0, :]
        erun = small[:, 0:KH]
        acoef = small[:, 8:8 + KH]
        dsg = small[:, 16:16 + NH * KH]
        dall = small[:, 32:32 + 3 * 8].rearrange("p (j n) -> p j n", n=8) if NH * KH <= 8 else None
        sm = small[:, 56:64]

        psi = [0]

        def next_ps():
            psi[0] = (psi[0] + 1) % 8
            return psi[0]

        tmi = [0]

        def next_tmp():
            tmi[0] = (tmi[0] + 1) % 4
            return tmi[0]

        def dma(q, out_ap, in_ap, reads, writes):
            S.op(q, lambda e: e.dma_start(out=out_ap, in_=in_ap), reads, writes, kind="dma")

        def mmr(out_ap, pi, lhsT, rhs, start, stop, reads):
            S.op("pe", lambda e: e.matmul(out_ap, lhsT, rhs, start=start, stop=stop), reads, [("ps", pi)])

        def mm(pi, sl, lhsT, rhs, start, stop, reads):
            mmr(ps[pi][sl[0], sl[1]], pi, lhsT, rhs, start, stop, reads)

        def act(out_ap, in_ap, func, reads, writes, bias=None, scale=None):
            kw = {}
            if bias is not None:
                kw["bias"] = bias
            if scale is not None:
                kw["scale"] = scale
            S.op("act", lambda e: e.activation(out=out_ap, in_=in_ap, func=func, **kw), reads, writes)

        def tt(out_ap, a, b, op, reads, writes, eng="dve"):
            S.op(eng, lambda e: e.tensor_tensor(out=out_ap, in0=a, in1=b, op=op), reads, writes)

        def ts(out_ap, a, s1, s2, op0, op1, reads, writes, eng="dve"):
            if s2 is None:
                S.op(eng, lambda e: e.tensor_scalar(out=out_ap, in0=a, scalar1=s1, scalar2=None, op0=op0), reads, writes)
            else:
                S.op(eng, lambda e: e.tensor_scalar(out=out_ap, in0=a, scalar1=s1, scalar2=s2, op0=op0, op1=op1), reads, writes)

        def stt(out_ap, a, s, b, op0, op1, reads, writes, eng="dve"):
            S.op(eng, lambda e: e.scalar_tensor_tensor(out=out_ap, in0=a, scalar=s, in1=b, op0=op0, op1=op1), reads, writes)

        def cp(out_ap, in_ap, reads, writes, eng="dve"):
            if eng == "act":
                S.op(eng, lambda e: e.copy(out=out_ap, in_=in_ap), reads, writes)
            else:
                S.op(eng, lambda e: e.tensor_copy(out=out_ap, in_=in_ap), reads, writes)

        def memset(ap, v, writes, eng="dve"):
            S.op(eng, lambda e: e.memset(ap, v), [], writes)

        def recip(out_ap, in_ap, reads, writes):
            S.op("dve", lambda e: e.reciprocal(out=out_ap, in_=in_ap), reads, writes)

        def barrier():
            S.barrier(lambda e: e.memset(small[:, 72:73], 0.0))

        dma("sp", vec[:], vecs.ap(), [], ["vec"])
        dma("sp", gn[:], gnorm.ap(), [], ["gn"])
        dma("sp", cst[:], consts.ap(), [], ["cst"])
        dma("sp", fl[:], flags.ap(), [], ["fl"])
        cp(idb[:], cst[:, 0, :], ["cst"], ["idb"])
        IDN, TRI, UPP, ONES, CM = 0, 1, 2, 3, 4

        gq = [name for name, _, _, _ in wspec]
        gpos = [0]

        def gather_through(pred):
            last = max([i for i, n in enumerate(gq) if pred(n)], default=-1)
            while gpos[0] <= last:
                name = gq[gpos[0]]
                gpos[0] += 1
                for p in range(win[name].shape[0]):
                    dma("pool", wbn[name][p], win[name][p], [], [("wb", name, p)])
                    S.op("pool", lambda e, a=wbn[name][p], b=wg[name][p]: e.collective_compute(
                        "AllGather", ALU.bypass, replica_groups=GROUPS, ins=[a], outs=[b], **GQOS),
                        [("wb", name, p)], [("wg", name, p)], kind="cc")

        wl_of = {name: wl for name, _, _, wl in wspec}

        def first_part(n, lmax):
            if wl_of[n] < lmax:
                return True
            if wl_of[n] > lmax:
                return False
            return not (n.startswith("moe") and int(n.split("_")[2]) >= NE // 2)

        def gather_after_exchange(l):
            if l >= DEPTH - 2:
                gather_through(lambda n: True)
            elif (l + 1) % 2 == 1:
                gather_through(lambda n: first_part(n, l + 1))
            else:
                gather_through(lambda n: first_part(n, min(l + 3, DEPTH - 1)))

        wbi = [0]

        def load_w(name, k0, nk, j, ncol=128):
            i = wt_act[0][wbi[0] % len(wt_act[0])]
            wbi[0] += 1
            ch = 128 * wview[name].shape[2] * 128
            pcs = range((j * ch) // (4 * PIECE), ((j + 1) * ch - 1) // (4 * PIECE) + 1)
            dma("sp", wt[i][:, 0:nk, 0:ncol], wview[name][j, :, k0:k0 + nk, 0:ncol], [("wg", name, p) for p in pcs], [("wt", i)])
            return i

        HBK = [("hb", j) for j in range(KC)]

        def proj(name, jc, ncol, blocks, epi, rhs=None, rkeys=None, rpos=None):
            rhs = hb if rhs is None else rhs
            rkeys = HBK if rkeys is None else rkeys
            rpos = rpos or (lambda t0: t0)
            wi = load_w(name, 0, KC, jc, ncol)
            for (t0, tn) in blocks:
                pi = next_ps()
                r0 = rpos(t0)
                for k in range(KC):
                    mm(pi, (slice(0, ncol), slice(0, tn)), wt[wi][:, k, 0:ncol], rhs[:, k, r0:r0 + tn], k == 0, k == KC - 1,
                       [("wt", wi)] + rkeys)
                epi(t0, tn, pi)

        def ln_accum(pss, pqq, j, zap, zkey):
            for bi, (t0, tn) in enumerate(TB):
                ti = next_tmp()
                act(tmp[ti][:, 0:tn], zap[:, t0:t0 + tn], AF.Square, [zkey], [("tmp", ti)])
                mm(pss[bi], (slice(0, 128), slice(0, tn)), cst[:, ONES, :], zap[:, t0:t0 + tn], j == 0, j == KC - 1, [zkey, "cst"])
                mm(pqq[bi], (slice(0, 128), slice(0, tn)), cst[:, ONES, :], tmp[ti][:, 0:tn], j == 0, j == KC - 1, [("tmp", ti), "cst"])

        def ln_finish(pss, pqq, n):
            for bi, (t0, tn) in enumerate(TB):
                sl = slice(t0, t0 + tn)
                ta, tb_ = next_tmp(), next_tmp()
                ts(stat[:, 0, sl], ps[pss[bi]][:, 0:tn], 1.0 / n, None, ALU.mult, None, [("ps", pss[bi])], [("stat", 0, bi)])
                ts(tmp[ta][:, 0:tn], ps[pqq[bi]][:, 0:tn], 1.0 / n, None, ALU.mult, None, [("ps", pqq[bi])], [("tmp", ta)])
                tt(tmp[tb_][:, 0:tn], stat[:, 0, sl], stat[:, 0, sl], ALU.mult, [("stat", 0, bi)], [("tmp", tb_)])
                tt(tmp[ta][:, 0:tn], tmp[ta][:, 0:tn], tmp[tb_][:, 0:tn], ALU.subtract, [("tmp", ta), ("tmp", tb_)], [("tmp", ta)])
                ts(tmp[ta][:, 0:tn], tmp[ta][:, 0:tn], EPS, None, ALU.add, None, [("tmp", ta)], [("tmp", ta)])
                act(tmp[tb_][:, 0:tn], tmp[ta][:, 0:tn], AF.Sqrt, [("tmp", ta)], [("tmp", tb_)])
                recip(stat[:, 1, sl], tmp[tb_][:, 0:tn], [("tmp", tb_)], [("stat", 1, bi)])

        SK = [("stat", r, b) for r in (0, 1) for b in range(NB)]

        def ln_from_dram(src):
            pss, pqq = [next_ps() for _ in TB], [next_ps() for _ in TB]
            for j in range(KC):
                si = j % 3
                dma("sp", stg[si][:], src[:, j, :], [("zres", j)], [("stg", si)])
                ln_accum(pss, pqq, j, stg[si], ("stg", si))
            ln_finish(pss, pqq, D)

        def ln_apply(src_dram, gi, bi_, write_out=None):
            for j in range(KC):
                si = j % 3
                dma("sp", stg[si][:], src_dram[:, j, :], [("zres", j)], [("stg", si)])
                tt(stg[si][:], stg[si][:], stat[:, 0, :], ALU.subtract, [("stg", si)] + SK, [("stg", si)])
                tt(stg[si][:], stg[si][:], stat[:, 1, :], ALU.mult, [("stg", si)] + SK, [("stg", si)])
                ts(stg[si][:], stg[si][:], vec[:, gi, j:j + 1], vec[:, bi_, j:j + 1], ALU.mult, ALU.add, [("stg", si), "vec"], [("stg", si)])
                cp(hb[:, j, :], stg[si][:], [("stg", si)], [("hb", j)], eng="act")
                dma("act", hres[:, j, :], stg[si][:], [("stg", si)], [("hres", j)])
                if write_out is not None:
                    dma("sp", write_out[:, j, :], stg[si][:, NM:T], [("stg", si)], [("out", j)])

        def ffn(w1n, w3n, w2n, first, gated):
            for f in range(FC):
                wa = load_w(w1n, 0, KC, f)
                wgk = load_w(w3n, 0, KC, f)
                for (t0, tn) in TB:
                    pa, pg = next_ps(), next_ps()
                    for k in range(KC):
                        mm(pa, (slice(0, 128), slice(0, tn)), wt[wa][:, k, 0:128], hb[:, k, t0:t0 + tn], k == 0, k == KC - 1, [("wt", wa)] + HBK)
                    for k in range(KC):
                        mm(pg, (slice(0, 128), slice(0, tn)), wt[wgk][:, k, 0:128], hb[:, k, t0:t0 + tn], k == 0, k == KC - 1, [("wt", wgk)] + HBK)
                    ti = next_tmp()
                    act(tmp[ti][:, 0:tn], ps[pa][:, 0:tn], AF.Silu, [("ps", pa)], [("tmp", ti)])
                    tt(HH[:, f, t0:t0 + tn], ps[pg][:, 0:tn], tmp[ti][:, 0:tn], ALU.mult, [("ps", pg), ("tmp", ti)], [("H", f)])
            HK = [("H", f) for f in range(FC)]
            for j in range(KC):
                grp = []
                for k0 in range(0, FC, KG):
                    nk = min(KG, FC - k0)
                    grp.append((k0, nk, load_w(w2n, k0, nk, j)))
                si = j % 3
                if first:
                    dma("act", stg[si][:], hres[:, j, :], [("hres", j)], [("stg", si)])
                    ts(stg[si][:], stg[si][:], ALPHA, None, ALU.mult, None, [("stg", si)], [("stg", si)])
                else:
                    dma("act", stg[si][:], zres[:, j, :], [("zres", j)], [("stg", si)])
                for (t0, tn) in TB:
                    pa = next_ps()
                    for (k0, nk, wi) in grp:
                        for k in range(nk):
                            mm(pa, (slice(0, 128), slice(0, tn)), wt[wi][:, k, 0:128], HH[:, k0 + k, t0:t0 + tn],
                               k0 + k == 0, k0 + k == FC - 1, [("wt", wi)] + HK)
                    if gated:
                        ti = next_tmp()
                        tt(tmp[ti][:, 0:tn], ps[pa][:, 0:tn], Gb[:, t0:t0 + tn], ALU.mult, [("ps", pa), "Gb"], [("tmp", ti)])
                        tt(stg[si][:, t0:t0 + tn], stg[si][:, t0:t0 + tn], tmp[ti][:, 0:tn], ALU.add, [("stg", si), ("tmp", ti)], [("stg", si)])
                    else:
                        tt(stg[si][:, t0:t0 + tn], stg[si][:, t0:t0 + tn], ps[pa][:, 0:tn], ALU.add, [("stg", si), ("ps", pa)], [("stg", si)])
                dma("act", zres[:, j, :], stg[si][:], [("stg", si)], [("zres", j)])

        def router(li):
            dma("sp", rtw[:], rtr[li], [], ["rtw"])
            dma("sp", sm[0:1, 0:NE], rtb[li], [], ["rtbias"])
            for (t0, nt) in TILES:
                pl = next_ps()
                for g0 in range(0, KC, 4):
                    gi = next_tmp()
                    ng = min(4, KC - g0)
                    hv = tmp[gi][:, 0:ng * 128].rearrange("p (k n) -> p k n", n=128)
                    dma("sp", hv[:, :, 0:nt], hres[:, g0:g0 + ng, t0:t0 + nt], [("hres", j) for j in range(g0, g0 + ng)], [("tmp", gi)])
                    for k in range(ng):
                        mm(pl, (slice(0, nt), slice(0, NE)), hv[:, k, 0:nt], rtw[:, g0 + k, :], g0 + k == 0, False, [("tmp", gi), "rtw"])
                mm(pl, (slice(0, nt), slice(0, NE)), cst[0:1, ONES, 0:nt], sm[0:1, 0:NE], False, True, ["cst", "rtbias"])
                L0 = lg[0:nt, 0:NE]
                L1 = lg[0:nt, 8:8 + NE]
                K1 = lg[0:nt, 16:16 + NE]
                K2 = lg[0:nt, 24:24 + NE]
                GG = lg[0:nt, 32:32 + NE]
                m1, m2, dd, g1, g2 = (lg[0:nt, 40 + i:41 + i] for i in range(5))
                LK = ["lg"]
                cp(L0, ps[pl][0:nt, 0:NE], [("ps", pl)], LK)
                S.op("dve", lambda e, o=m1, i=L0: e.reduce_max(out=o, in_=i, axis=mybir.AxisListType.X), LK, LK)
                ts(K1, L0, m1, None, ALU.is_equal, None, LK, LK)
                stt(L1, K1, -1e30, L0, ALU.mult, ALU.add, LK, LK)
                S.op("dve", lambda e, o=m2, i=L1: e.reduce_max(out=o, in_=i, axis=mybir.AxisListType.X), LK, LK)
                ts(K2, L1, m2, None, ALU.is_equal, None, LK, LK)
                tt(dd, m2, m1, ALU.subtract, LK, LK)
                act(dd, dd, AF.Exp, LK, LK)
                ts(g1, dd, 1.0, None, ALU.add, None, LK, LK)
                recip(g1, g1, LK, LK)
                tt(g2, dd, g1, ALU.mult, LK, LK)
                ts(GG, K1, g1, None, ALU.mult, None, LK, LK)
                stt(GG, K2, g2, GG, ALU.mult, ALU.add, LK, LK)
                pt = next_ps()
                mm(pt, (slice(0, NE), slice(0, nt)), GG, cst[0:nt, IDN, 0:nt], True, True, LK + ["cst"])
                cp(GT[0:NE, t0:t0 + nt], ps[pt][0:NE, 0:nt], [("ps", pt)], ["GT"], eng="act")

        def gate_bcast(e):
            ts(Gsel[0:NE, :], GT[0:NE, :], cst[0:NE, IDN, e:e + 1], None, ALU.mult, None, ["GT", "cst"], ["Gsel"])
            for (t0, tn) in TB:
                pi = next_ps()
                mm(pi, (slice(0, 128), slice(0, tn)), cst[0:NE, ONES, :], Gsel[0:NE, t0:t0 + tn], True, True, ["Gsel", "cst"])
                cp(Gb[:, t0:t0 + tn], ps[pi][:, 0:tn], [("ps", pi)], ["Gb"], eng="act")

        def gla_head(l, h, wn):
            RAK = [("RA", h * VH + v) for v in range(VH)]
            QK = [("RM", h * KH + k) for k in range(KH)]
            KK = [("RM", NH * KH + k) for k in range(KH)]
            dma("sp", auh[0:R1, 0:DKH], alup[l, h], [], ["auh"])
            for kh in range(KH):
                proj(wn, cQ + h * KH + kh, 128, TB,
                     lambda t0, tn, pi, kh=kh: cp(RM[:, h * KH + kh, t0:t0 + tn], ps[pi][:, 0:tn], [("ps", pi)], [("RM", h * KH + kh)], eng="act"))
                proj(wn, cK + h * KH + kh, 128, TB,
                     lambda t0, tn, pi, kh=kh: cp(RM[:, NH * KH + kh, t0:t0 + tn], ps[pi][:, 0:tn], [("ps", pi)], [("RM", NH * KH + kh)]))
            for vv in range(VH):
                proj(wn, cV + h * VH + vv, 128, TB,
                     lambda t0, tn, pi, vv=vv: cp(RA[:, h * VH + vv, t0:t0 + tn], ps[pi][:, 0:tn], [("ps", pi)], [("RA", h * VH + vv)],
                                                  eng="act" if vv % 2 else "dve"))
            memset(Sst[:, :, :], 0.0, ["Sst"])
            memset(Sb[:, :, :], 0.0, ["Sb"])
            memset(erun, 1.0, ["erun"])
            for (t0, nt) in TILES:
                pre = t0 < NM
                chunks = [(0, nt)] if nt <= 64 else [(0, 64), (64, 64)]
                pa_ = next_ps()
                mm(pa_, (slice(0, nt), slice(0, DKH)), alT[0:R1, t0:t0 + nt], auh[0:R1, 0:DKH], True, True, ["alT", "auh"])
                act(e1[0:nt, :], ps[pa_][0:nt, 0:DKH], AF.Exp, [("ps", pa_)], ["e1"], scale=-1.0)
                act(ltok[0:nt, :], e1[0:nt, :], AF.Ln, ["e1"], ["ltok"], bias=1.0)
                pe_ = next_ps()
                mm(pe_, (slice(0, nt), slice(0, DKH)), cst[0:nt, UPP, 0:nt], ltok[0:nt, :], True, True, ["ltok", "cst"])
                act(ef[0:nt, :], ps[pe_][0:nt, 0:DKH], AF.Exp, [("ps", pe_)], ["ef"])
                pb_ = next_ps()
                for kh in range(KH):
                    mm(pb_, (slice(0, 128), slice(kh * 128, kh * 128 + nt)), ltok[0:nt, kh * 128:(kh + 1) * 128], cst[0:nt, TRI, 0:nt],
                       True, True, ["ltok", "cst"])
                pbv = ps[pb_][:, 0:KH * 128].rearrange("p (k n) -> p k n", n=128)[:, :, 0:nt]
                act(eB[:, :, 0:nt], pbv, AF.Exp, [("ps", pb_)], ["eB"])
                act(enB[:, :, 0:nt], pbv, AF.Exp, [("ps", pb_)], ["enB"], scale=-1.0)
                pk_ = next_ps()
                for kh in range(KH):
                    mm(pk_, (slice(0, nt), slice(kh * 128, (kh + 1) * 128)), RM[:, NH * KH + kh, t0:t0 + nt], idb[:, :], True, True, KK + ["idb"])
                tt(kte[0:nt, :], ps[pk_][0:nt, 0:DKH], ef[0:nt, :], ALU.mult, [("ps", pk_), "ef"], ["kte"])
                if pre:
                    ts(kte[0:nt, :], kte[0:nt, :], fl[0:nt, 0:1], None, ALU.mult, None, ["kte", "fl"], ["kte"])
                pv_ = next_ps()
                for vv in range(VH):
                    mm(pv_, (slice(0, nt), slice(vv * 128, (vv + 1) * 128)), RA[:, h * VH + vv, t0:t0 + nt], idb[:, :], True, True, RAK + ["idb"])
                cp(vt[0:nt, :], ps[pv_][0:nt, 0:DVH], [("ps", pv_)], ["vt"], eng="act")
                stt(qd[:, :, 0:nt], RM[:, h * KH:(h + 1) * KH, t0:t0 + nt], QSCALE, eB[:, :, 0:nt], ALU.mult, ALU.mult, QK + ["eB"], ["qd"])
                tt(kd[:, :, 0:nt], RM[:, NH * KH:NH * KH + KH, t0:t0 + nt], enB[:, :, 0:nt], ALU.mult, KK + ["enB"], ["kd"])
                ps_ = next_ps()
                for kh in range(KH):
                    mm(ps_, (slice(0, nt), slice(0, nt)), kd[:, kh, 0:nt], qd[:, kh, 0:nt], kh == 0, kh == KH - 1, ["kd", "qd"])
                tt(sT[0:nt, 0:nt], ps[ps_][0:nt, 0:nt], cst[0:nt, CM, 0:nt], ALU.mult, [("ps", ps_), "cst"], ["sT"])
                for (c0, cn) in chunks:
                    cs = slice(c0, c0 + cn)
                    po = next_ps()
                    for vv in range(VH):
                        osl = (slice(0, 128), slice(vv * 64, vv * 64 + cn))
                        mm(po, osl, vt[cs, vv * 128:(vv + 1) * 128], sT[cs, cs], True, False, ["vt", "sT"])
                        for kh in range(KH):
                            mm(po, osl, Sb[:, kh, vv * 128:(vv + 1) * 128], qd[:, kh, cs], False, kh == KH - 1, ["Sb", "qd"])
                    for kh in range(KH):
                        ts(RM[:, h * KH + kh, t0 + c0:t0 + c0 + cn], qd[:, kh, cs], erun[:, kh:kh + 1], None, ALU.mult, None,
                           ["qd", "erun"], [("RM", h * KH + kh)])
                    pov = ps[po][:, 0:VH * 64].rearrange("p (v n) -> p v n", n=64)[:, :, 0:cn]
                    cp(RA[:, h * VH:(h + 1) * VH, t0 + c0:t0 + c0 + cn], pov, [("ps", po)], RAK, eng="act")
                    for kh in range(KH):
                        pS = next_ps()
                        mm(pS, (slice(0, 128), slice(0, DVH)), kte[cs, kh * 128:(kh + 1) * 128], vt[cs, :], True, True, ["kte", "vt"])
                        dec = eB[:, kh, c0 + cn - 1:c0 + cn]
                        stt(Sst[:, kh, :], Sst[:, kh, :], dec, ps[pS][:, 0:DVH], ALU.mult, ALU.add, ["Sst", "eB", ("ps", pS)], ["Sst"])
                        cp(Sb[:, kh, :], Sst[:, kh, :], ["Sst"], ["Sb"], eng="act")
                        if not pre:
                            tt(erun[:, kh:kh + 1], erun[:, kh:kh + 1], dec, ALU.mult, ["erun", "eB"], ["erun"])
            dma("sp", xsrc[h][:, 0:KH * DVH], Sstf, ["Sst"], [("xsrc", h)])
            cp(dsg[:, h * KH:(h + 1) * KH], erun, ["erun"], ["dsg"])
            S.op("pool", lambda e, a=xsrc[h], b=xdst[h]: e.collective_compute(
                "AllGather", ALU.bypass, replica_groups=GROUPS, ins=[a], outs=[b]), [("xsrc", h)], [("xdst", h)], kind="cc")

        def gla_finish_head(l, h, wn):
            RAK = [("RA", h * VH + v) for v in range(VH)]
            memset(Sst[:, :, :], 0.0, ["Sst"])
            for j in range(3):
                dma("sp", Sx[:, :], xdst[h][j * 128:(j + 1) * 128, 0:KH * DVH], [("xdst", h)], ["Sx"])
                ts(acoef, dall[:, j, h * KH:(h + 1) * KH], -1.0, fl[:, 1 + j:2 + j], ALU.add, ALU.mult, ["dall", "fl"], ["acoef"])
                ts(acoef, acoef, 1.0, None, ALU.add, None, ["acoef"], ["acoef"])
                ts(Sx[:, :], Sx[:, :], fl[:, 1 + j:2 + j], None, ALU.mult, None, ["Sx", "fl"], ["Sx"])
                for kh in range(KH):
                    stt(Sst[:, kh, :], Sst[:, kh, :], acoef[:, kh:kh + 1], Sx[:, kh * DVH:(kh + 1) * DVH], ALU.mult, ALU.add,
                        ["Sst", "acoef", "Sx"], ["Sst"])
            cp(Sb[:, :, :], Sst[:, :, :], ["Sst"], ["Sb"], eng="act")
            for vv in range(VH):
                for (t0, tn) in TB:
                    pi = next_ps()
                    for kh in range(KH):
                        mm(pi, (slice(0, 128), slice(0, tn)), Sb[:, kh, vv * 128:(vv + 1) * 128], RM[:, h * KH + kh, t0:t0 + tn],
                           kh == 0, kh == KH - 1, ["Sb", ("RM", h * KH + kh)])
                    tt(RA[:, h * VH + vv, t0:t0 + tn], RA[:, h * VH + vv, t0:t0 + tn], ps[pi][:, 0:tn], ALU.add,
                       [("ps", pi), ("RA", h * VH + vv)], [("RA", h * VH + vv)])
            for bi, (t0, tn) in enumerate(TB):
                pr = next_ps()
                for vv in range(VH):
                    ti = next_tmp()
                    act(tmp[ti][:, 0:tn], RA[:, h * VH + vv, t0:t0 + tn], AF.Square, [("RA", h * VH + vv)], [("tmp", ti)])
                    mm(pr, (slice(0, 128), slice(0, tn)), cst[:, ONES, :], tmp[ti][:, 0:tn], vv == 0, vv == VH - 1, [("tmp", ti), "cst"])
                ta, tb_ = next_tmp(), next_tmp()
                ts(tmp[ta][:, 0:tn], ps[pr][:, 0:tn], 1.0 / DVH, EPS, ALU.mult, ALU.add, [("ps", pr)], [("tmp", ta)])
                act(tmp[tb_][:, 0:tn], tmp[ta][:, 0:tn], AF.Sqrt, [("tmp", ta)], [("tmp", tb_)])
                recip(stat[:, 1, t0:t0 + tn], tmp[tb_][:, 0:tn], [("tmp", tb_)], [("stat", 1, bi)])
            for vv in range(VH):
                jj = h * VH + vv

                def epi_r(t0, tn, pi, jj=jj):
                    ti = next_tmp()
                    act(tmp[ti][:, 0:tn], ps[pi][:, 0:tn], AF.Silu, [("ps", pi)], [("tmp", ti)])
                    tt(tmp[ti][:, 0:tn], tmp[ti][:, 0:tn], stat[:, 1, t0:t0 + tn], ALU.mult, [("tmp", ti)] + SK, [("tmp", ti)])
                    stt(RA[:, jj, t0:t0 + tn], RA[:, jj, t0:t0 + tn], gn[:, l, jj:jj + 1], tmp[ti][:, 0:tn], ALU.mult, ALU.mult,
                        [("RA", jj), "gn", ("tmp", ti)], [("RA", jj)])
                proj(wn, cR + jj, 128, TB, epi_r)

        gather_through(lambda n: wl_of[n] == 0)
        pss, pqq = [next_ps() for _ in TB], [next_ps() for _ in TB]
        for j in range(KC):
            si = j % 3
            dma("sp", stg[si][:], xT[:, j, :], [], [("stg", si)])
            ln_accum(pss, pqq, j, stg[si], ("stg", si))
            dma("sp", zres[:, j, :], stg[si][:], [("stg", si)], [("zres", j)])
        ln_finish(pss, pqq, D)
        ln_apply(zres, 0, 1)

        for l in range(DEPTH):
            vb = 2 + l * 9
            V_CB, V_CNG, V_CNB, V_MG, V_MB, V_FG, V_FB = [vb + i for i in range(7)]
            wn = f"w_in{l}"
            barrier()
            wt_act[0] = wt_mix
            dma("sp", cw[:], convw[l], [], ["cw"])
            if STOP != "noGLA":
                memset(alT[0:32, :], 1.0, ["alT"])
                proj(wn, cA, LOWR, TB, lambda t0, tn, pi: cp(alT[0:LOWR, t0:t0 + tn], ps[pi][0:LOWR, 0:tn], [("ps", pi)], ["alT"]))
                for h in range(NH):
                    gla_head(l, h, wn)
                dma("sp", xsrc[NH][:, 0:NH * KH], dsg, ["dsg"], [("xsrc", NH)])
                S.op("pool", lambda e, a=xsrc[NH], b=xdst[NH]: e.collective_compute(
                    "AllGather", ALU.bypass, replica_groups=GROUPS, ins=[a], outs=[b]), [("xsrc", NH)], [("xdst", NH)], kind="cc")
            memset(Ue[:, :, 0:HALO], 0.0, [("Ue", j) for j in range(KC)])
            for j in range(KC):
                wa = load_w(wn, 0, KC, cG + j)
                wgk = load_w(wn, 0, KC, cG + KC + j)
                for (t0, tn) in TB:
                    pa, pg = next_ps(), next_ps()
                    for k in range(KC):
                        mm(pa, (slice(0, 128), slice(0, tn)), wt[wa][:, k, 0:128], hb[:, k, t0:t0 + tn], k == 0, k == KC - 1, [("wt", wa)] + HBK)
                    for k in range(KC):
                        mm(pg, (slice(0, 128), slice(0, tn)), wt[wgk][:, k, 0:128], hb[:, k, t0:t0 + tn], k == 0, k == KC - 1, [("wt", wgk)] + HBK)
                    ti = next_tmp()
                    act(tmp[ti][:, 0:tn], ps[pg][:, 0:tn], AF.Sigmoid, [("ps", pg)], [("tmp", ti)])
                    u0 = upos(t0)
                    tt(Ue[:, j, u0:u0 + tn], ps[pa][:, 0:tn], tmp[ti][:, 0:tn], ALU.mult, [("ps", pa), ("tmp", ti)], [("Ue", j)])
            for j in range(KC):
                cp(hal[:, j * HALO:(j + 1) * HALO], Ue[:, j, UW - HALO:UW], [("Ue", j)], ["hal"])
            dma("sp", usrc[:, 0:KC * HALO], hal[:], ["hal"], ["usrc"])
            S.op("pool", lambda e: e.collective_compute("AllGather", ALU.bypass, replica_groups=GROUPS,
                                                        ins=[usrc.ap()], outs=[udst.ap()]), ["usrc"], ["udst"], kind="cc")
            gather_after_exchange(l)
            if STOP != "noGLA":
                for j in range(3):
                    dma("sp", dall[:, j, 0:NH * KH], xdst[NH][j * 128:(j + 1) * 128, 0:NH * KH], [("xdst", NH)], ["dall"])
                for h in range(NH):
                    gla_finish_head(l, h, wn)
            barrier()
            ts(hal[:], hal[:], 0.0, None, ALU.mult, None, ["hal", "usrc"], ["hal"])
            for jj in range(3):
                dma("sp", halg[:, :], udst[jj * 128:(jj + 1) * 128, 0:KC * HALO], ["udst"], ["halg"])
                stt(hal[:], halg[:, :], fl[:, 4 + jj:5 + jj], hal[:], ALU.mult, ALU.add, ["halg", "fl", "hal"], ["hal"])
            W = NM + HALO + TL
            pss, pqq = list(range(NB)), list(range(NB, 2 * NB))
            cbanks = list(range(2 * NB, 8))
            cblocks = [(c0, min(512, W - c0)) for c0 in range(0, W, 512)]
            cbi = 0
            dgi = 0
            for j in range(KC):
                hj = hal[:, j * HALO:(j + 1) * HALO]
                stt(hj[:, HALO - NM:HALO], Ue[:, j, HALO:HALO + NM], fl[:, 0:1], hj[:, HALO - NM:HALO], ALU.mult, ALU.add,
                    [("Ue", j), "fl", "hal"], ["hal"])
                cp(Ue[:, j, HALO + NM:HALO + NM + HALO], hj, ["hal"], [("Ue", j)])
                si = j % 3
                for (c0, cn) in cblocks:
                    pc = cbanks[cbi % len(cbanks)]
                    cbi += 1
                    for k in range(CW):
                        di = dgi % 4
                        dgi += 1
                        ts(dg[:, di, :], idb[:, :], cw[:, j, k:k + 1], None, ALU.mult, None, ["idb", "cw"], [("dg", di)])
                        mm(pc, (slice(0, 128), slice(0, cn)), dg[:, di, :], Ue[:, j, c0 + k:c0 + k + cn], k == 0, k == CW - 1,
                           [("dg", di), ("Ue", j)])
                    a0, a1 = c0, min(c0 + cn, NM)
                    if a1 > a0:
                        act(stg[si][:, a0:a1], ps[pc][:, a0 - c0:a1 - c0], AF.Identity, [("ps", pc), "vec"], [("stg", si)],
                            bias=vec[:, V_CB, j:j + 1])
                    b0, b1 = max(c0, NM + HALO), min(c0 + cn, W)
                    if b1 > b0:
                        act(stg[si][:, b0 - HALO:b1 - HALO], ps[pc][:, b0 - c0:b1 - c0], AF.Identity, [("ps", pc), "vec"], [("stg", si)],
                            bias=vec[:, V_CB, j:j + 1])
                ln_accum(pss, pqq, j, stg[si], ("stg", si))
                cp(Ue[:, j, HALO:HALO + NM], stg[si][:, 0:NM], [("stg", si)], [("Ue", j)])
                cp(Ue[:, j, HALO + NM + HALO:UW], stg[si][:, NM:T], [("stg", si)], [("Ue", j)])
            ln_finish(pss, pqq, D)
            for j in range(KC):
                si = j % 3
                cp(stg[si][:, 0:NM], Ue[:, j, HALO:HALO + NM], [("Ue", j)], [("stg", si)])
                cp(stg[si][:, NM:T], Ue[:, j, HALO + NM + HALO:UW], [("Ue", j)], [("stg", si)])
                tt(stg[si][:], stg[si][:], stat[:, 0, :], ALU.subtract, [("stg", si)] + SK, [("stg", si)])
                tt(stg[si][:], stg[si][:], stat[:, 1, :], ALU.mult, [("stg", si)] + SK, [("stg", si)])
                ts(stg[si][:], stg[si][:], vec[:, V_CNG, j:j + 1], vec[:, V_CNB, j:j + 1], ALU.mult, ALU.add, [("stg", si), "vec"], [("stg", si)])
                act(Ue[:, j, HALO:HALO + NM], stg[si][:, 0:NM], AF.Silu, [("stg", si)], [("Ue", j)])
                act(Ue[:, j, HALO + NM + HALO:UW], stg[si][:, NM:T], AF.Silu, [("stg", si)], [("Ue", j)])
            UK = [("Ue", j) for j in range(KC)]
            for j in range(KC):
                wa = load_w(f"w_convo{l}", 0, KC, j)
                wgk = load_w(wn, 0, KC, cGB + j)
                for (t0, tn) in TB:
                    pa, pg = next_ps(), next_ps()
                    u0 = upos(t0)
                    for k in range(KC):
                        mm(pa, (slice(0, 128), slice(0, tn)), wt[wa][:, k, 0:128], Ue[:, k, u0:u0 + tn], k == 0, k == KC - 1, [("wt", wa)] + UK)
                    for k in range(KC):
                        mm(pg, (slice(0, 128), slice(0, tn)), wt[wgk][:, k, 0:128], hb[:, k, t0:t0 + tn], k == 0, k == KC - 1, [("wt", wgk)] + HBK)
                    ti = next_tmp()
                    act(tmp[ti][:, 0:tn], ps[pg][:, 0:tn], AF.Sigmoid, [("ps", pg)], [("tmp", ti)])
                    tt(RM[:, j, t0:t0 + tn], ps[pa][:, 0:tn], tmp[ti][:, 0:tn], ALU.mult, [("ps", pa), ("tmp", ti)], [("RM", j)])
            if STOP != "noGLA":
                RAALL = [("RA", j) for j in range(KC)]
                for j in range(KC):
                    wa = load_w(f"w_glao{l}", 0, KC, j)
                    wgk = load_w(wn, 0, KC, cGA + j)
                    for (t0, tn) in TB:
                        pa, pg = next_ps(), next_ps()
                        for k in range(KC):
                            mm(pa, (slice(0, 128), slice(0, tn)), wt[wa][:, k, 0:128], RA[:, k, t0:t0 + tn], k == 0, k == KC - 1, [("wt", wa)] + RAALL)
                        for k in range(KC):
                            mm(pg, (slice(0, 128), slice(0, tn)), wt[wgk][:, k, 0:128], hb[:, k, t0:t0 + tn], k == 0, k == KC - 1, [("wt", wgk)] + HBK)
                        ti = next_tmp()
                        act(tmp[ti][:, 0:tn], ps[pg][:, 0:tn], AF.Sigmoid, [("ps", pg)], [("tmp", ti)])
                        tt(tmp[ti][:, 0:tn], ps[pa][:, 0:tn], tmp[ti][:, 0:tn], ALU.mult, [("ps", pa), ("tmp", ti)], [("tmp", ti)])
                        tt(RM[:, j, t0:t0 + tn], RM[:, j, t0:t0 + tn], tmp[ti][:, 0:tn], ALU.add, [("RM", j), ("tmp", ti)], [("RM", j)])
            MK = [("RM", j) for j in range(KC)]
            for j in range(KC):
                wa = load_w(f"w_out{l}", 0, KC, j)
                si = j % 3
                dma("act", stg[si][:], hres[:, j, :], [("hres", j)], [("stg", si)])
                for (t0, tn) in TB:
                    pa = next_ps()
                    for k in range(KC):
                        mm(pa, (slice(0, 128), slice(0, tn)), wt[wa][:, k, 0:128], RM[:, k, t0:t0 + tn], k == 0, k == KC - 1, [("wt", wa)] + MK)
                    stt(stg[si][:, t0:t0 + tn], stg[si][:, t0:t0 + tn], ALPHA, ps[pa][:, 0:tn], ALU.mult, ALU.add, [("stg", si), ("ps", pa)], [("stg", si)])
                dma("act", zres[:, j, :], stg[si][:], [("stg", si)], [("zres", j)])
            ln_from_dram(zres)
            ln_apply(zres, V_MG, V_MB)
            barrier()
            wt_act[0] = wt_ffn
            last_out = out if l == DEPTH - 1 else None
            if l % 2 == 0:
                ffn(f"ffn1_{l}", f"ffn3_{l}", f"ffn2_{l}", True, False)
            else:
                router(l // 2)
                for e in range(NE):
                    gate_bcast(e)
                    ffn(f"moe1_{l}_{e}", f"moe3_{l}_{e}", f"moe2_{l}_{e}", e == 0, True)
            ln_from_dram(zres)
            ln_apply(zres, V_FG, V_FB, write_out=last_out)
        S.emit(nc, sems, dma_sems, cc_sem)
    return nc


def make_inputs(cfg, inp):
    D, SEQ, DEPTH, DFF, NE, NM, LOWR, CW = (cfg[k] for k in ("D", "SEQ", "DEPTH", "DFF", "NE", "NMETA", "LOWR", "CW"))
    KC = D // 128
    TL = SEQ // 4
    T = NM + TL
    f32 = lambda a: np.ascontiguousarray(np.asarray(a), dtype=np.float32)
    fm = lambda v: f32(v).reshape(KC, 128).T
    x = f32(inp["x"])
    meta = f32(inp["meta_tokens"])
    nv = 2 + DEPTH * 9
    vecs = np.zeros((128, nv, KC), np.float32)
    vecs[:, 0] = fm(inp["ln_in_g"]); vecs[:, 1] = fm(inp["ln_in_b"])
    for l in range(DEPTH):
        b = 2 + l * 9
        for i, k in enumerate(("conv_b", "conv_norm_g", "conv_norm_b", "ln_mix_g", "ln_mix_b", "ln_ffn_g", "ln_ffn_b")):
            vecs[:, b + i] = fm(f32(inp[k])[l])
    convw = np.ascontiguousarray(f32(inp["conv_w"]).reshape(DEPTH, CW, KC, 128).transpose(0, 3, 2, 1))
    gnorm = np.ascontiguousarray(f32(inp["gla_norm_g"]).reshape(DEPTH, KC, 128).transpose(2, 0, 1))
    NH = 4
    DK = D // 2
    DKH = DK // NH
    al = np.concatenate([f32(inp["w_alpha_up"]), f32(inp["b_alpha"])[:, None, :]], 1)
    alup = np.ascontiguousarray(al.reshape(DEPTH, LOWR + 1, NH, DKH).transpose(0, 2, 1, 3))
    NMOE = max(1, DEPTH // 2)
    rtr = np.zeros((NMOE, 128, KC, NE), np.float32)
    rtb = np.zeros((NMOE, 1, NE), np.float32)
    if DEPTH // 2:
        rtr[:] = f32(inp["router_w"]).reshape(DEPTH // 2, KC, 128, NE).transpose(0, 2, 1, 3)
        rtb[:, 0] = f32(inp["router_b"])
    consts = np.zeros((128, 5, 128), np.float32)
    ii = np.arange(128)
    same = (ii[:, None] // 64) == (ii[None, :] // 64)
    consts[:, 0] = np.eye(128)
    consts[:, 1] = (same & (ii[:, None] <= ii[None, :])) * (-1.0 / 16.0)
    consts[:, 2] = (same & (ii[:, None] > ii[None, :])) * (-1.0 / 16.0)
    consts[:, 3] = 1.0
    consts[:, 4] = (same & (ii[:, None] <= ii[None, :])) * 1.0
    shared = dict(vecs=vecs, convw=convw, gnorm=gnorm, alup=alup, rtr=rtr, rtb=rtb, consts=consts)
    wsrc = {}
    for l in range(DEPTH):
        wsrc[f"w_in{l}"] = inp["w_in"][l]; wsrc[f"w_glao{l}"] = inp["w_gla_o"][l]
        wsrc[f"w_convo{l}"] = inp["w_conv_o"][l]; wsrc[f"w_out{l}"] = inp["w_out"][l]
        if l % 2 == 0:
            wsrc[f"ffn1_{l}"] = inp["ffn_w1"][l // 2]; wsrc[f"ffn3_{l}"] = inp["ffn_w3"][l // 2]; wsrc[f"ffn2_{l}"] = inp["ffn_w2"][l // 2]
        else:
            for e in range(NE):
                wsrc[f"moe1_{l}_{e}"] = inp["moe_w1"][l // 2][e]; wsrc[f"moe3_{l}_{e}"] = inp["moe_w3"][l // 2][e]
                wsrc[f"moe2_{l}_{e}"] = inp["moe_w2"][l // 2][e]
    DK_ = D // 2
    oA_ = 2 * DK_ + 2 * D

    def tile_major(n, w):
        w = f32(w)
        if n.startswith("w_in"):
            w = np.concatenate([w[:, oA_:oA_ + LOWR], np.zeros((w.shape[0], 128 - LOWR), np.float32), w[:, :oA_], w[:, oA_ + LOWR:]], 1)
        K_, N_ = w.shape
        return np.ascontiguousarray(w.reshape(K_ // 128, 128, N_ // 128, 128).transpose(2, 1, 0, 3))

    wsh = [dict() for _ in range(4)]
    for n, w in wsrc.items():
        wtm = tile_major(n, w)
        for r in range(4):
            wsh[r][n] = shard_weight(wtm, r)
        del wtm
    maps = []
    for core in range(8):
        b, c = core // 4, core % 4
        tok = np.concatenate([meta, x[b, c * TL:(c + 1) * TL]], 0)
        xT = np.ascontiguousarray(tok.T.reshape(KC, 128, T).transpose(1, 0, 2))
        fl = np.zeros((128, 8), np.float32)
        fl[:, 0] = 1.0 if c == 0 else 0.0
        for j in range(3):
            fl[:, 1 + j] = 1.0 if j < c else 0.0
            fl[:, 4 + j] = 1.0 if j == c - 1 else 0.0
        m = dict(shared)
        m.update(xT=xT, flags=fl)
        m.update(wsh[c])
        maps.append(m)
    return maps


def run(cfg, inp):
    nc = build(cfg)
    maps = make_inputs(cfg, inp)
    res = run_bass_kernel_spmd(nc, maps, core_ids=list(range(8)))
    D, SEQ = cfg["D"], cfg["SEQ"]
    TL = SEQ // 4
    B = 2
    o = np.zeros((B, SEQ, D), np.float32)
    for core in range(8):
        b, c = core // 4, core % 4
        y = res.results[core]["out"]
        o[b, c * TL:(c + 1) * TL] = y.transpose(2, 1, 0).reshape(TL, D)
    return o


def kernel(**inputs):
    return run(dict(CFG_FULL), inputs)
```
